# Optimizing a Trainium2 kernel written in Bass

```python
import math
import jax
import jax.numpy as jnp
from jax import lax
import numpy as np

D_MODEL = 2048
BATCH = 4
SEQ = 4096
DEPTH = 2

GRID_W = 64
CTX_LEN = 256
HEAD_DIM = 128
ROPE_THETA = 10000.0
Q_BLOCK = 128
GQA_HEADS = 6
GQA_KV_HEADS = 2
GQA_GROUP = GQA_HEADS // GQA_KV_HEADS
MLA_HEADS = 5
MLA_Q_RANK = 512
MLA_KV_RANK = 256
MLA_NOPE_DIM = 128
MLA_ROPE_DIM = 64
MLA_V_DIM = 128
MLA_QK_DIM = MLA_NOPE_DIM + MLA_ROPE_DIM
NA_HEADS = 5
NA_KH = 8
NA_KW = 16
N_BRANCHES = 3
IN_SIZES = (GQA_HEADS * HEAD_DIM, GQA_KV_HEADS * HEAD_DIM, GQA_KV_HEADS * HEAD_DIM,
            MLA_Q_RANK, MLA_KV_RANK, MLA_ROPE_DIM,
            NA_HEADS * HEAD_DIM, NA_HEADS * HEAD_DIM, NA_HEADS * HEAD_DIM,
            N_BRANCHES * D_MODEL)
IN_WIDTH = sum(IN_SIZES)
N_GROUPS = 8
EXPERTS_PER_GROUP = 8
N_EXPERTS = N_GROUPS * EXPERTS_PER_GROUP
TOP_K = 2
D_EXPERT = 512
MOE_BLOCK = 128
LN_EPS = 1e-6
RMS_EPS = 1e-6
DEEPNORM_ALPHA = (2 * DEPTH) ** 0.25
DEEPNORM_BETA = (8 * DEPTH) ** -0.25

kernel_name = "hybrid_gqa_mla_natten_hmoe_diffusion_trunk"


def layer_norm(x, g, b):
    xf = x.astype(jnp.float32)
    mu = jnp.mean(xf, axis=-1, keepdims=True)
    var = jnp.mean(jnp.square(xf - mu), axis=-1, keepdims=True)
    return ((xf - mu) * lax.rsqrt(var + LN_EPS) * g.astype(jnp.float32) + b.astype(jnp.float32)).astype(x.dtype)


def rms_norm(x, g):
    xf = x.astype(jnp.float32)
    y = xf * lax.rsqrt(jnp.mean(jnp.square(xf), axis=-1, keepdims=True) + RMS_EPS)
    return (y * g.astype(jnp.float32)).astype(x.dtype)


def rope_tables(seq_len, dim):
    t = jnp.arange(seq_len, dtype=jnp.int32)
    row = (t // GRID_W).astype(jnp.float32)
    col = (t % GRID_W).astype(jnp.float32)
    quarter = dim // 4
    inv_freq = ROPE_THETA ** (-jnp.arange(quarter, dtype=jnp.float32) / quarter)
    ang_r = row[:, None] * inv_freq
    ang_c = col[:, None] * inv_freq
    ang = jnp.concatenate([ang_r, ang_r, ang_c, ang_c], axis=-1)
    return jnp.cos(ang), jnp.sin(ang)


def apply_rope(x, cos, sin):
    d = x.shape[-1]
    xr = x.reshape(x.shape[:-1] + (2, 2, d // 4))
    rot = jnp.stack([-xr[..., 1, :], xr[..., 0, :]], axis=-2).reshape(x.shape)
    return x * cos[:, None, :].astype(x.dtype) + rot * sin[:, None, :].astype(x.dtype)


def ada_modulation(cond, w_ada, b_ada):
    m = jax.nn.silu(cond) @ w_ada + b_ada
    return jnp.split(m, 6, axis=-1)


def modulate(x, shift, scale):
    return x * (1.0 + scale) + shift


def to_heads(t):
    return t.transpose(0, 2, 1, 3)


def heads_to_tokens(o):
    bsz, k, g, n, dv = o.shape
    return o.transpose(0, 3, 1, 2, 4).reshape(bsz, n, k * g * dv)


def project_mixers(u, w_in, gqa_qn, gqa_kn, mla_qn, mla_kvn, w_uq, w_ukv, rope):
    bsz, n, _ = u.shape
    split_pts = np.cumsum(IN_SIZES)[:-1].tolist()
    aq, ak, av, bq, bkv, bkr, cq, ck, cv, gate_logits = jnp.split(u @ w_in, split_pts, axis=-1)
    aq = rms_norm(aq.reshape(bsz, n, GQA_HEADS, HEAD_DIM), gqa_qn)
    ak = rms_norm(ak.reshape(bsz, n, GQA_KV_HEADS, HEAD_DIM), gqa_kn)
    av = av.reshape(bsz, n, GQA_KV_HEADS, HEAD_DIM)
    bq = (rms_norm(bq, mla_qn) @ w_uq).reshape(bsz, n, MLA_HEADS, MLA_QK_DIM)
    bkv = (rms_norm(bkv, mla_kvn) @ w_ukv).reshape(bsz, n, MLA_HEADS, MLA_NOPE_DIM + MLA_V_DIM)
    bq_nope, bq_rope = jnp.split(bq, [MLA_NOPE_DIM], axis=-1)
    bk_nope, bv = jnp.split(bkv, [MLA_NOPE_DIM], axis=-1)
    bkr = bkr[:, :, None, :]
    if rope is not None:
        (cos_a, sin_a), (cos_b, sin_b) = rope
        aq = apply_rope(aq, cos_a, sin_a)
        ak = apply_rope(ak, cos_a, sin_a)
        bq_rope = apply_rope(bq_rope, cos_b, sin_b)
        bkr = apply_rope(bkr, cos_b, sin_b)
    bq = jnp.concatenate([bq_nope, bq_rope], axis=-1)
    bk = jnp.concatenate([bk_nope, jnp.broadcast_to(bkr, (bsz, n, MLA_HEADS, MLA_ROPE_DIM))], axis=-1)
    cq = cq.reshape(bsz, n, NA_HEADS, HEAD_DIM)
    ck = ck.reshape(bsz, n, NA_HEADS, HEAD_DIM)
    cv = cv.reshape(bsz, n, NA_HEADS, HEAD_DIM)
    qa = aq.reshape(bsz, n, GQA_KV_HEADS, GQA_GROUP, HEAD_DIM).transpose(0, 2, 3, 1, 4)
    return (qa, to_heads(ak), to_heads(av),
            to_heads(bq)[:, :, None], to_heads(bk), to_heads(bv),
            to_heads(cq), to_heads(ck), to_heads(cv), gate_logits)


def dense_attention(q, k, v, scale):
    s = jnp.einsum("bkgqd,bksd->bkgqs", q, k, preferred_element_type=jnp.float32) * scale
    p = jax.nn.softmax(s, axis=-1).astype(v.dtype)
    return jnp.einsum("bkgqs,bksd->bkgqd", p, v)


def blocked_attention(q, k, v, scale):
    bsz, kh, g, s, dq = q.shape
    nb = s // Q_BLOCK
    qb = jnp.moveaxis(q.reshape(bsz, kh, g, nb, Q_BLOCK, dq), 3, 0)
    out = lax.map(lambda qi: dense_attention(qi, k, v, scale), qb)
    return jnp.moveaxis(out, 0, 3).reshape(bsz, kh, g, s, v.shape[-1])


def neighbourhood_attention(q, k, v, k_ctx, v_ctx, rpb, scale):
    bsz, h, s, d = q.shape
    rows = s // GRID_W
    kh = min(NA_KH, rows)
    qg = q.reshape(bsz, h, rows, GRID_W, d)
    kg = k.reshape(bsz, h, rows, GRID_W, d)
    vg = v.reshape(bsz, h, rows, GRID_W, d)
    col = jnp.arange(GRID_W, dtype=jnp.int32)
    col_start = jnp.clip(col - NA_KW // 2, 0, GRID_W - NA_KW)
    col_idx = col_start[:, None] + jnp.arange(NA_KW, dtype=jnp.int32)[None, :]
    col_off = col_idx - col[:, None] + (NA_KW - 1)
    n_win = kh * NA_KW

    def row_step(r):
        r_start = jnp.clip(r - kh // 2, 0, rows - kh)
        k_band = lax.dynamic_slice_in_dim(kg, r_start, kh, axis=2)
        v_band = lax.dynamic_slice_in_dim(vg, r_start, kh, axis=2)
        k_win = k_band[:, :, :, col_idx]
        v_win = v_band[:, :, :, col_idx]
        q_row = lax.dynamic_index_in_dim(qg, r, axis=2, keepdims=False)
        row_off = r_start + jnp.arange(kh, dtype=jnp.int32) - r + (NA_KH - 1)
        bias = rpb[:, row_off[None, :, None], col_off[:, None, :]].astype(jnp.float32)
        s_win = jnp.einsum("bhwd,bhiwjd->bhwij", q_row, k_win, preferred_element_type=jnp.float32) * scale + bias
        s_ctx = jnp.einsum("bhwd,bhcd->bhwc", q_row, k_ctx, preferred_element_type=jnp.float32) * scale
        p = jax.nn.softmax(jnp.concatenate([s_win.reshape(bsz, h, GRID_W, n_win), s_ctx], axis=-1), axis=-1).astype(v.dtype)
        p_win = p[..., :n_win].reshape(bsz, h, GRID_W, kh, NA_KW)
        return (jnp.einsum("bhwij,bhiwjd->bhwd", p_win, v_win)
                + jnp.einsum("bhwc,bhcd->bhwd", p[..., n_win:], v_ctx))

    out = lax.map(row_step, jnp.arange(rows, dtype=jnp.int32))
    return out.transpose(1, 0, 3, 2, 4).reshape(bsz, s, h * d)


def merge_branches(o_a, o_b, o_c, gate_logits, w_ba, w_bb, w_bc, w_out):
    g = jax.nn.sigmoid(gate_logits.astype(jnp.float32)).astype(o_a.dtype)
    g_a, g_b, g_c = jnp.split(g, N_BRANCHES, axis=-1)
    return (g_a * (o_a @ w_ba) + g_b * (o_b @ w_bb) + g_c * (o_c @ w_bc)) @ w_out


def routed_experts(u, expert_id, gates, w_gate, w_up, w_down):
    n, d = u.shape
    n_assign = n * TOP_K
    flat_e = expert_id.reshape(-1)
    flat_tok = jnp.arange(n_assign, dtype=jnp.int32) // TOP_K
    flat_w = gates.reshape(-1)
    order = jnp.argsort(flat_e)
    se, stok, sw = flat_e[order], flat_tok[order], flat_w[order]
    counts = jnp.bincount(flat_e, length=N_EXPERTS).astype(jnp.int32)
    starts = jnp.cumsum(counts) - counts
    padded = ((counts + MOE_BLOCK - 1) // MOE_BLOCK) * MOE_BLOCK
    pends = jnp.cumsum(padded)
    pstarts = pends - padded
    dest = pstarts[se] + jnp.arange(n_assign, dtype=jnp.int32) - starts[se]
    n_blocks = -(-n_assign // MOE_BLOCK) + N_EXPERTS
    n_slots = n_blocks * MOE_BLOCK
    slot_tok = jnp.full((n_slots,), n, jnp.int32).at[dest].set(stok)
    slot_w = jnp.zeros((n_slots,), u.dtype).at[dest].set(sw)
    block_start = jnp.arange(n_blocks, dtype=jnp.int32) * MOE_BLOCK
    block_e = jnp.clip(jnp.searchsorted(pends, block_start, side="right"), 0, N_EXPERTS - 1)
    u_pad = jnp.concatenate([u, jnp.zeros((1, d), u.dtype)], axis=0)

    def block_step(args):
        tok, e = args
        xb = u_pad[tok]
        hb = jax.nn.silu(xb @ w_gate[e]) * (xb @ w_up[e])
        return hb @ w_down[e]

    y = lax.map(block_step, (slot_tok.reshape(n_blocks, MOE_BLOCK), block_e)).reshape(n_slots, d)
    return jnp.zeros((n + 1, d), u.dtype).at[slot_tok].add(y * slot_w[:, None])[:n]


def hierarchical_moe(u, w_group, b_group, w_route, b_route, w_gate, w_up, w_down):
    n = u.shape[0]
    group_logits = jnp.matmul(u, w_group, preferred_element_type=jnp.float32) + b_group.astype(jnp.float32)
    group_prob = jax.nn.softmax(group_logits, axis=-1)
    group_sel = jnp.argmax(group_logits, axis=-1).astype(jnp.int32)
    group_gate = jnp.take_along_axis(group_prob, group_sel[:, None], axis=-1)
    exp_logits = (jnp.matmul(u, w_route, preferred_element_type=jnp.float32)
                  + b_route.astype(jnp.float32)).reshape(n, N_GROUPS, EXPERTS_PER_GROUP)
    exp_logits = jnp.take_along_axis(exp_logits, group_sel[:, None, None], axis=1)[:, 0]
    top_logits, top_idx = lax.top_k(exp_logits, TOP_K)
    gates = group_gate * jax.nn.softmax(top_logits, axis=-1)
    expert_id = group_sel[:, None] * EXPERTS_PER_GROUP + top_idx.astype(jnp.int32)
    return routed_experts(u, expert_id, gates.astype(u.dtype), w_gate, w_up, w_down)


def setup_inputs(seed: int = 0) -> dict:
    key = jax.random.key(seed)
    ks = iter(jax.random.split(key, 32))
    L, D = DEPTH, D_MODEL

    def nrm(shape, scale):
        return jax.random.normal(next(ks), shape, jnp.float32) * scale

    return {
        "x": nrm((BATCH, SEQ, D), 1.0),
        "c": nrm((BATCH, D), 1.0),
        "ctx": nrm((BATCH, CTX_LEN, D), 1.0),
        "c_ctx": nrm((D,), 1.0),
        "w_ada": nrm((L, D, 6 * D), 0.5 * D ** -0.5),
        "b_ada": nrm((L, 6 * D), 0.02),
        "w_in": nrm((L, D, IN_WIDTH), D ** -0.5),
        "gqa_q_norm": 1.0 + nrm((L, HEAD_DIM), 0.02),
        "gqa_k_norm": 1.0 + nrm((L, HEAD_DIM), 0.02),
        "mla_q_norm": 1.0 + nrm((L, MLA_Q_RANK), 0.02),
        "mla_kv_norm": 1.0 + nrm((L, MLA_KV_RANK), 0.02),
        "mla_w_uq": nrm((L, MLA_Q_RANK, MLA_HEADS * MLA_QK_DIM), MLA_Q_RANK ** -0.5),
        "mla_w_ukv": nrm((L, MLA_KV_RANK, MLA_HEADS * (MLA_NOPE_DIM + MLA_V_DIM)), MLA_KV_RANK ** -0.5),
        "na_rpb": nrm((L, NA_HEADS, 2 * NA_KH - 1, 2 * NA_KW - 1), 0.1),
        "w_branch_a": nrm((L, GQA_HEADS * HEAD_DIM, D), (GQA_HEADS * HEAD_DIM) ** -0.5),
        "w_branch_b": nrm((L, MLA_HEADS * MLA_V_DIM, D), (MLA_HEADS * MLA_V_DIM) ** -0.5),
        "w_branch_c": nrm((L, NA_HEADS * HEAD_DIM, D), (NA_HEADS * HEAD_DIM) ** -0.5),
        "w_out": nrm((L, D, D), DEEPNORM_BETA * D ** -0.5),
        "ln1_g": 1.0 + nrm((L, D), 0.02),
        "ln1_b": nrm((L, D), 0.02),
        "w_router_group": nrm((L, D, N_GROUPS), D ** -0.5),
        "b_router_group": nrm((L, N_GROUPS), 0.01),
        "w_router_expert": nrm((L, D, N_EXPERTS), D ** -0.5),
        "b_router_expert": nrm((L, N_EXPERTS), 0.01),
        "w_expert_gate": nrm((L, N_EXPERTS, D, D_EXPERT), D ** -0.5),
        "w_expert_up": nrm((L, N_EXPERTS, D, D_EXPERT), D ** -0.5),
        "w_expert_down": nrm((L, N_EXPERTS, D_EXPERT, D), DEEPNORM_BETA * D_EXPERT ** -0.5),
        "ln2_g": 1.0 + nrm((L, D), 0.02),
        "ln2_b": nrm((L, D), 0.02),
    }


def reference(x, c, ctx, c_ctx, w_ada, b_ada, w_in, gqa_q_norm, gqa_k_norm, mla_q_norm, mla_kv_norm,
              mla_w_uq, mla_w_ukv, na_rpb, w_branch_a, w_branch_b, w_branch_c, w_out, ln1_g, ln1_b,
              w_router_group, b_router_group, w_router_expert, b_router_expert,
              w_expert_gate, w_expert_up, w_expert_down, ln2_g, ln2_b):
    bsz, seq, dm = x.shape
    rope = (rope_tables(seq, HEAD_DIM), rope_tables(seq, MLA_ROPE_DIM))
    scale_a = 1.0 / math.sqrt(HEAD_DIM)
    scale_b = 1.0 / math.sqrt(MLA_QK_DIM)
    scale_c = 1.0 / math.sqrt(HEAD_DIM)
    for i in range(DEPTH):
        last = i == DEPTH - 1
        sh_m, sc_m, g_m, sh_f, sc_f, g_f = ada_modulation(c, w_ada[i], b_ada[i])
        csh_m, csc_m, cg_m, csh_f, csc_f, cg_f = ada_modulation(c_ctx, w_ada[i], b_ada[i])

        fx = project_mixers(modulate(x, sh_m[:, None], sc_m[:, None]), w_in[i], gqa_q_norm[i], gqa_k_norm[i],
                            mla_q_norm[i], mla_kv_norm[i], mla_w_uq[i], mla_w_ukv[i], rope)
        fc = project_mixers(modulate(ctx, csh_m, csc_m), w_in[i], gqa_q_norm[i], gqa_k_norm[i],
                            mla_q_norm[i], mla_kv_norm[i], mla_w_uq[i], mla_w_ukv[i], None)
        qa, ka, va, qb, kb, vb, qc, kc, vc, gx = fx
        qa_c, ka_c, va_c, qb_c, kb_c, vb_c, qc_c, kc_c, vc_c, gc = fc
        o_a = heads_to_tokens(blocked_attention(qa, jnp.concatenate([ka, ka_c], axis=2),
                                                jnp.concatenate([va, va_c], axis=2), scale_a))
        o_b = heads_to_tokens(blocked_attention(qb, jnp.concatenate([kb, kb_c], axis=2),
                                                jnp.concatenate([vb, vb_c], axis=2), scale_b))
        o_c = neighbourhood_attention(qc, kc, vc, kc_c, vc_c, na_rpb[i], scale_c)
        y = merge_branches(o_a, o_b, o_c, gx, w_branch_a[i], w_branch_b[i], w_branch_c[i], w_out[i])
        x = layer_norm(DEEPNORM_ALPHA * x + g_m[:, None] * y, ln1_g[i], ln1_b[i])
        if not last:
            oc_a = heads_to_tokens(dense_attention(qa_c, ka_c, va_c, scale_a))
            oc_b = heads_to_tokens(dense_attention(qb_c, kb_c, vb_c, scale_b))
            oc_c = heads_to_tokens(dense_attention(qc_c[:, :, None], kc_c, vc_c, scale_c))
            yc = merge_branches(oc_a, oc_b, oc_c, gc, w_branch_a[i], w_branch_b[i], w_branch_c[i], w_out[i])
            ctx = layer_norm(DEEPNORM_ALPHA * ctx + cg_m * yc, ln1_g[i], ln1_b[i])

        ux = modulate(x, sh_f[:, None], sc_f[:, None]).reshape(-1, dm)
        if last:
            mx = hierarchical_moe(ux, w_router_group[i], b_router_group[i], w_router_expert[i], b_router_expert[i],
                                  w_expert_gate[i], w_expert_up[i], w_expert_down[i])
            x = layer_norm(DEEPNORM_ALPHA * x + g_f[:, None] * mx.reshape(bsz, seq, dm), ln2_g[i], ln2_b[i])
        else:
            uc = modulate(ctx, csh_f, csc_f).reshape(-1, dm)
            n_x = ux.shape[0]
            m_all = hierarchical_moe(jnp.concatenate([ux, uc], axis=0), w_router_group[i], b_router_group[i],
                                     w_router_expert[i], b_router_expert[i],
                                     w_expert_gate[i], w_expert_up[i], w_expert_down[i])
            x = layer_norm(DEEPNORM_ALPHA * x + g_f[:, None] * m_all[:n_x].reshape(bsz, seq, dm), ln2_g[i], ln2_b[i])
            ctx = layer_norm(DEEPNORM_ALPHA * ctx + cg_f * m_all[n_x:].reshape(ctx.shape), ln2_g[i], ln2_b[i])
    return x
```

```python
import math
from contextlib import ExitStack

import numpy as np
import concourse.bass as bass
import concourse.mybir as mybir
from concourse.bass_utils import run_bass_kernel_spmd

F32 = mybir.dt.float32
BF16 = mybir.dt.bfloat16
AF = mybir.ActivationFunctionType
ALU = mybir.AluOpType
AX = mybir.AxisListType

PE, ACT, DVE, POOL, SP = "tensor", "scalar", "vector", "gpsimd", "sync"
ENGS = [PE, ACT, DVE, POOL, SP]

D = 2048
KC = 16
NOWN = 2048
NCTX = 256
T = NOWN + NCTX
NNAT = 4096
NKEY = NNAT + NCTX
NKT = NKEY // 128
TL = NNAT + NCTX
EPS = 1e-6
ALPHA = 4 ** 0.25
NEXP = 64
DEXP = 512

SEG = {}
_o = 0
for _n, _w in (("aq", 768), ("ak", 256), ("av", 256), ("bq", 512), ("bkv", 256), ("bkr", 64),
               ("cq", 640), ("ck", 640), ("cv", 640), ("gate", 6144)):
    SEG[_n] = (_o, _w)
    _o += _w
IN_W = _o


class Op:
    __slots__ = ("eng", "emit", "deps", "idx", "sig_sem", "sig_val", "is_dma", "needs_sig", "acc")

    def __init__(self, eng, emit, is_dma):
        self.eng = eng
        self.emit = emit
        self.deps = set()
        self.is_dma = is_dma
        self.needs_sig = False
        self.sig_sem = None
        self.sig_val = 0
        self.acc = False


class Phase:
    def __init__(self, nc, sems, name="ph"):
        self.nc = nc
        self.name = name
        self.ops = []
        self.last_writer = {}
        self.readers = {}
        self.sem_pool = sems
        self.dma_sem_of = {}
        self.dma_cnt = {}
        self.n_dma = {"dsw": 0, "dhw": 0}

    def add(self, eng, emit, reads=(), writes=(), dma=None, acc=False):
        op = Op(eng, emit, dma is not None)
        op.idx = len(self.ops)
        op.acc = acc
        deps = op.deps
        for b in reads:
            w = self.last_writer.get(b)
            if w is not None:
                deps.add(w)
        for b in writes:
            w = self.last_writer.get(b)
            if w is not None:
                wop = self.ops[w]
                if not (acc and wop.acc and wop.eng == eng):
                    deps.add(w)
            for r in self.readers.get(b, ()):
                deps.add(r)
        for b in reads:
            self.readers.setdefault(b, []).append(op.idx)
        for b in writes:
            self.last_writer[b] = op.idx
            self.readers[b] = []
        if dma is not None:
            kind = "dsw" if eng == POOL else "dhw"
            dma = (kind, dma)
            if dma not in self.dma_sem_of:
                self.dma_sem_of[dma] = self.n_dma[kind]
                self.n_dma[kind] += 1
                self.dma_cnt[dma] = self.sem_pool["dsw_cnt"][self.dma_sem_of[dma]] if kind == "dsw" else 0
            self.dma_cnt[dma] += 16
            if kind == "dsw":
                self.sem_pool["dsw_cnt"][self.dma_sem_of[dma]] = self.dma_cnt[dma]
            op.sig_sem = (kind, self.dma_sem_of[dma])
            op.sig_val = self.dma_cnt[dma]
            op.needs_sig = True
        self.ops.append(op)
        return op.idx

    def finalize(self):
        nc = self.nc
        ops = self.ops
        for op in ops:
            for d in op.deps:
                ops[d].needs_sig = True
        cnt = {e: 0 for e in ENGS}
        for op in ops:
            if not op.is_dma and op.needs_sig:
                cnt[op.eng] += 1
                op.sig_sem = ("eng", op.eng)
                op.sig_val = cnt[op.eng]
        for kind in ("dsw", "dhw"):
            assert self.n_dma[kind] <= len(self.sem_pool[kind]), f"{self.name}: need {self.n_dma[kind]} {kind} sems"

        def semh(key):
            return self.sem_pool[key[0]][key[1]]

        waited = {e: {} for e in ENGS}
        plan = {e: [] for e in ENGS}
        for op in ops:
            need = {}
            for d in op.deps:
                p = ops[d]
                k = p.sig_sem
                if need.get(k, 0) < p.sig_val:
                    need[k] = p.sig_val
            waits = []
            for k, v in need.items():
                if waited[op.eng].get(k, 0) < v:
                    waited[op.eng][k] = v
                    waits.append((k, v))
            plan[op.eng].append((op, waits))
        final_waits = {e: {} for e in ENGS}
        for op in ops:
            if op.is_dma:
                final_waits[op.eng][op.sig_sem] = max(final_waits[op.eng].get(op.sig_sem, 0), op.sig_val)

        with nc.Block() as block:
            for e in ENGS:
                if not plan[e] and not final_waits[e]:
                    continue

                def body(eng, e=e):
                    for op, waits in plan[e]:
                        for k, v in waits:
                            eng.wait_ge(semh(k), v)
                        ins = op.emit(eng)
                        if op.needs_sig:
                            ins.then_inc(semh(op.sig_sem), 16 if op.is_dma else 1)
                    for k, v in final_waits[e].items():
                        eng.wait_ge(semh(k), v)

                getattr(block, e)(body)
        with nc.Block() as block2:
            def clr(eng):
                for e in ENGS:
                    eng.sem_clear(self.sem_pool["eng"][e])
                for i in range(self.n_dma["dhw"]):
                    eng.sem_clear(self.sem_pool["dhw"][i])
            block2.gpsimd(clr)
        n = len(ops)
        self.ops = []
        return n


def make_sems(nc, stack, nhw=40, nsw=12):
    pool = {"eng": {}, "dhw": [], "dsw": [], "dsw_cnt": [0] * nsw}
    for e in ENGS:
        pool["eng"][e] = stack.enter_context(nc.semaphore("s_" + e))
    for i in range(nhw):
        pool["dhw"].append(stack.enter_context(nc.semaphore(f"s_dhw{i}")))
    for i in range(nsw):
        pool["dsw"].append(stack.enter_context(nc.semaphore(f"s_dsw{i}")))
    return pool


def e_dma(out, in_):
    return lambda e: e.dma_start(out=out, in_=in_)


def e_mm(items):
    def emit(e):
        ins = None
        for (o, l, r, s, t) in items:
            ins = e.matmul(o, l, r, start=s, stop=t)
        return ins
    return emit


def e_act(out, in_, func, scale=None, bias=None):
    kw = {}
    if scale is not None:
        kw["scale"] = scale
    if bias is not None:
        kw["bias"] = bias
    return lambda e: e.activation(out=out, in_=in_, func=func, **kw)


def e_tt(out, a, b, op):
    return lambda e: e.tensor_tensor(out=out, in0=a, in1=b, op=op)


def e_ts(out, a, s1, op0, s2=None, op1=None):
    if op1 is None:
        return lambda e: e.tensor_scalar(out=out, in0=a, scalar1=s1, scalar2=None, op0=op0)
    return lambda e: e.tensor_scalar(out=out, in0=a, scalar1=s1, scalar2=s2, op0=op0, op1=op1)


def e_copy(out, in_):
    return lambda e: e.tensor_copy(out=out, in_=in_)


_UNIQ = [0]


def uname(name):
    _UNIQ[0] += 1
    return f"{name}_{_UNIQ[0]}"


class Ring:
    def __init__(self, nc, stack, name, shape, dtype, n):
        name = uname(name)
        self.bufs = [stack.enter_context(nc.sbuf_tensor(f"{name}_{i}", shape, dtype)) for i in range(n)]
        self.name = name
        self.i = 0

    def next(self):
        b = self.bufs[self.i % len(self.bufs)]
        k = (self.name, self.i % len(self.bufs))
        self.i += 1
        return b, k


class Ctx:
    pass


TABW = TL


def declare_io(nc, nlayers=2, debug=False, with_moe=True):
    C = Ctx()
    C.nc = nc

    def inp(name, shape, dt=F32):
        return nc.dram_tensor(name, list(shape), dt, kind="ExternalInput").ap()

    def scr(name, shape, dt=BF16, out=False):
        kind = "ExternalOutput" if (out or debug) else "Internal"
        return nc.dram_tensor(name, list(shape), dt, kind=kind).ap()

    C.xt = inp("xt", [D, TL])
    C.cond = inp("cond", [128, 32])
    C.tab128 = inp("tab128", [2, 128, TABW])
    C.tab64 = inp("tab64", [2, 64, TABW])
    C.rt128 = inp("rt128", [128, 128])
    C.rt64 = inp("rt64", [64, 64])
    C.ident = inp("ident", [128, 128])
    C.L = []
    for l in range(nlayers):
        W = Ctx()
        sfx = str(l)
        W.w_ada = inp("w_ada" + sfx, [D, 6 * D])
        W.b_ada = inp("b_ada" + sfx, [128, 96])
        W.w_in = inp("w_in" + sfx, [D, IN_W])
        W.gq = inp("gq" + sfx, [128, 1])
        W.gk = inp("gk" + sfx, [128, 1])
        W.mlaq_g = inp("mlaq_g" + sfx, [128, 4])
        W.mlakv_g = inp("mlakv_g" + sfx, [128, 2])
        W.w_uq = inp("w_uq" + sfx, [512, 960])
        W.w_ukv = inp("w_ukv" + sfx, [256, 1280])
        W.nab = inp("nab" + sfx, [5, 8 if l == 0 else 4, 8, 128, 512])
        W.w_ba = inp("w_ba" + sfx, [768, D])
        W.w_bb = inp("w_bb" + sfx, [640, D])
        W.w_bc = inp("w_bc" + sfx, [640, D])
        W.w_out = inp("w_out" + sfx, [D, D])
        W.lnp = inp("lnp" + sfx, [128, 4, 16])
        W.w_r = inp("w_r" + sfx, [D, 72])
        W.b_r = inp("b_r" + sfx, [128, 72])
        if with_moe:
            W.w_eg = inp("w_eg" + sfx, [NEXP, D, DEXP])
            W.w_eu = inp("w_eu" + sfx, [NEXP, D, DEXP])
            W.w_ed = inp("w_ed" + sfx, [NEXP, DEXP, D])
        C.L.append(W)
    C.QA = scr("QA", [6, 128, TL])
    C.KA = scr("KA", [2, 128, NKEY])
    C.VA = scr("VA", [NKEY, 256])
    C.QBN = scr("QBN", [5, 128, TL])
    C.QBR = scr("QBR", [5, 64, TL])
    C.KBN = scr("KBN", [5, 128, NKEY])
    C.KBR = scr("KBR", [64, NKEY])
    C.VB = scr("VB", [NKEY, 640])
    C.QC = scr("QC", [5, 128, TL])
    C.KCs = scr("KCs", [5, 128, NKEY])
    C.VC = scr("VC", [NKEY, 640])
    C.G = scr("G", [48, 128, TL])
    C.OA = scr("OA", [6, 128, TL])
    C.OB = scr("OB", [5, 128, TL])
    C.OC = scr("OC", [5, 128, TL])
    C.X1 = scr("X1", [D, TL], F32)
    C.XO0 = scr("XO0", [D, TL], F32)
    C.MODD = scr("MODD", [128, 192], F32)
    C.xo = scr("xo", [D, NOWN], F32, out=True)
    return C


def alloc_globals(C, st):
    nc = C.nc
    sb = lambda name, shape, dt: st.enter_context(nc.sbuf_tensor(name, shape, dt))
    C.sems = make_sems(nc, st)
    C.ps = [st.enter_context(nc.psum_tensor(f"psb{i}", [128, 512], F32)) for i in range(8)]
    C.ones_bf = sb("ones_bf", [128, 128], BF16)
    C.ones_f = sb("ones_f", [128, 128], F32)
    C.ident_f = sb("ident_f", [128, 128], F32)
    C.ident_bf = sb("ident_bf", [128, 128], BF16)
    C.rtq = sb("rtq", [128, 128], BF16)
    C.rtk = sb("rtk", [128, 128], BF16)
    C.rt64b = sb("rt64b", [64, 64], BF16)
    C.mod = sb("mod", [128, 2, 96], F32)
    C.sc1 = sb("sc1", [128, 2, 2, 16], F32)
    C.gqs = sb("gqs", [128, 1], F32)
    C.gks = sb("gks", [128, 1], F32)
    C.mlaqg = sb("mlaqg", [128, 4], F32)
    C.mlakvg = sb("mlakvg", [128, 2], F32)
    C.lnps = sb("lnps", [128, 4, 16], F32)
    C.wuq = sb("wuq", [128, 4, 960], BF16)
    C.wukv = sb("wukv", [128, 2, 1280], BF16)
    C.wukv_v = sb("wukv_v", [128, 2, 640], BF16)


def phase_ada(C, W):
    nc = C.nc
    ph = Phase(nc, C.sems, "ada")
    with ExitStack() as st:
        sb = lambda name, shape, dt: st.enter_context(nc.sbuf_tensor(uname(name), shape, dt))
        cond = sb("a_cond", [128, 32], F32)
        silu = sb("a_silu", [128, 32], F32)
        bada = sb("a_bada", [128, 96], F32)
        rt_f = sb("a_rtf", [128, 128], F32)
        rt64_f = sb("a_rt64f", [64, 64], F32)
        wring = Ring(nc, st, "a_w", [128, KC, 384], F32, 3)
        ph.add(SP, e_dma(cond[:], C.cond), writes=["cond"], dma="cond")
        ph.add(SP, e_dma(bada[:], W.b_ada), writes=["bada"], dma="bada")
        ph.add(SP, e_dma(C.ident_f[:], C.ident), writes=["ident_f"], dma="ident")
        ph.add(SP, e_dma(rt_f[:], C.rt128), writes=["rt_f"], dma="rt_f")
        ph.add(SP, e_dma(rt64_f[:], C.rt64), writes=["rt64_f"], dma="rt64_f")
        ph.add(SP, e_dma(C.gqs[:], W.gq), writes=["gqs"], dma="gqs")
        ph.add(SP, e_dma(C.gks[:], W.gk), writes=["gks"], dma="gks")
        ph.add(SP, e_dma(C.mlaqg[:], W.mlaq_g), writes=["mlaqg"], dma="mlaqg")
        ph.add(SP, e_dma(C.mlakvg[:], W.mlakv_g), writes=["mlakvg"], dma="mlakvg")
        ph.add(SP, e_dma(C.lnps[:], W.lnp), writes=["lnps"], dma="lnps")
        ph.add(POOL, e_dma(C.wuq[:], W.w_uq.rearrange("(c p) n -> p c n", p=128)), writes=["wuq"], dma="wuq")
        ph.add(POOL, e_dma(C.wukv[:], W.w_ukv.rearrange("(c p) n -> p c n", p=128)), writes=["wukv"], dma="wukv")
        ph.add(POOL, lambda e: e.memset(C.ones_bf[:], 1.0), writes=["ones_bf"])
        ph.add(POOL, lambda e: e.memset(C.ones_f[:], 1.0), writes=["ones_f"])
        ph.add(DVE, e_copy(C.ident_bf[:], C.ident_f[:]), reads=["ident_f"], writes=["ident_bf"])
        ph.add(DVE, e_ts(C.rtq[:], rt_f[:], C.gqs[:, 0:1], ALU.mult), reads=["rt_f", "gqs"], writes=["rtq"])
        ph.add(DVE, e_ts(C.rtk[:], rt_f[:], C.gks[:, 0:1], ALU.mult), reads=["rt_f", "gks"], writes=["rtk"])
        ph.add(DVE, e_copy(C.rt64b[:], rt64_f[:]), reads=["rt64_f"], writes=["rt64b"])
        for c in range(4):
            ph.add(DVE, e_ts(C.wuq[:, c, :], C.wuq[:, c, :], C.mlaqg[:, c:c + 1], ALU.mult, math.sqrt(512.0), ALU.mult),
                   reads=["wuq", "mlaqg"], writes=["wuq"])
        for c in range(2):
            ph.add(DVE, e_ts(C.wukv[:, c, :], C.wukv[:, c, :], C.mlakvg[:, c:c + 1], ALU.mult, math.sqrt(256.0), ALU.mult),
                   reads=["wukv", "mlakvg"], writes=["wukv"])
        for c in range(2):
            src = C.wukv[:, c, :].rearrange("p (h x) -> p h x", x=256)[:, :, 128:256]
            dst = C.wukv_v[:, c, :].rearrange("p (h x) -> p h x", x=128)
            ph.add(DVE, e_copy(dst, src), reads=["wukv"], writes=["wukv_v"])
        ph.add(ACT, e_act(silu[:], cond[:], AF.Silu), reads=["cond"], writes=["silu"])
        acc = C.ps[0]
        wv = W.w_ada.rearrange("(kc p) n -> p kc n", p=128)
        for s in range(32):
            w, wk = wring.next()
            ph.add(SP, e_dma(w[:], wv[:, :, s * 384:(s + 1) * 384]), writes=[wk], dma=wk)
            for jj in range(3):
                j = s * 3 + jj
                ph.add(PE, e_mm([(acc[:, j * 2:(j + 1) * 2], w[:, k, jj * 128:(jj + 1) * 128], silu[:, k * 2:(k + 1) * 2],
                                  k == 0, k == KC - 1) for k in range(KC)]),
                       reads=[wk, "silu"], writes=["acc"], acc=True)
        accv = acc[:, 0:192].rearrange("p (j c) -> p j c", c=2)
        for col in range(2):
            ph.add(DVE, e_tt(C.mod[:, col, :], accv[:, :, col], bada[:], ALU.add), reads=["acc", "bada"], writes=["mod"])
        for col in range(2):
            for mf in range(2):
                seg = 1 if mf == 0 else 4
                ph.add(DVE, e_ts(C.sc1[:, col, mf, :], C.mod[:, col, seg * 16:(seg + 1) * 16], 1.0, ALU.add),
                       reads=["mod"], writes=["sc1"])
        ph.add(SP, e_dma(C.MODD, C.mod[:].rearrange("p c j -> p (c j)")), reads=["mod"], dma="modd")
        n = ph.finalize()
    return n


SCALE_A = 1.0 / math.sqrt(128.0)
SCALE_B = 1.0 / math.sqrt(192.0)
SCALE_C = 1.0 / math.sqrt(128.0)


def phase_proj(C, W, blocks):
    nc = C.nc
    ph = Phase(nc, C.sems, "proj")
    ps = C.ps
    with ExitStack() as st:
        sb = lambda name, shape, dt: st.enter_context(nc.sbuf_tensor(uname(name), shape, dt))
        xs_r = Ring(nc, st, "p_xs", [128, 4, 512], F32, 2)
        u_r = Ring(nc, st, "p_u", [128, KC, 512], BF16, 2)
        SW = 1024
        w_r = Ring(nc, st, "p_w", [128, KC, SW], BF16, 2)
        tb128 = sb("p_tb128", [128, 2, 512], F32)
        tb64 = sb("p_tb64", [64, 2, 512], F32)
        cosq = sb("p_cosq", [128, 512], F32)
        cosk = sb("p_cosk", [128, 512], F32)
        sink = sb("p_sink", [128, 512], F32)
        c64q = sb("p_c64q", [64, 512], F32)
        s64q = sb("p_s64q", [64, 512], F32)
        sq_r = Ring(nc, st, "p_sq", [128, 512], BF16, 2)
        ln_r = Ring(nc, st, "p_ln", [128, 512], F32, 2)
        rs_r = Ring(nc, st, "p_rs", [128, 512], F32, 2)
        qn_r = Ring(nc, st, "p_qn", [128, 512], BF16, 2)
        t1_r = Ring(nc, st, "p_t1", [128, 512], F32, 2)
        t2_r = Ring(nc, st, "p_t2", [128, 512], F32, 2)
        stg_r = Ring(nc, st, "p_stg", [128, 512], BF16, 4)
        vst_r = Ring(nc, st, "p_vst", [128, 640], BF16, 2)
        latq = sb("p_latq", [128, 4, 512], BF16)
        latkv = sb("p_latkv", [128, 2, 512], BF16)
        main_i = [0]
        aux_i = [0]

        def mainbank():
            b = main_i[0] % 4
            main_i[0] += 1
            return b

        def auxpair():
            a = 4 + 2 * (aux_i[0] % 2)
            aux_i[0] += 1
            return a, a + 1

        wv = W.w_in.rearrange("(kc p) n -> p kc n", p=128)

        def load_w(c0, ncols):
            w, wk = w_r.next()
            ph.add(POOL, e_dma(w[:, :, 0:ncols], wv[:, :, c0:c0 + ncols]), writes=[wk], dma=wk)
            return w, wk

        def store(dst, src, srckey):
            ph.add(SP, e_dma(dst, src), reads=[srckey], dma=srckey)

        for blk in blocks:
            n = blk["ntok"]
            mc = blk["modcol"]
            tc0 = blk["tabcol"]
            qcol, kd, kn = blk["qcol"], blk["kd"], blk["kn"]
            u, uk = u_r.next()
            srcv = blk["src"].rearrange("(kc p) t -> p kc t", p=128)
            for q4 in range(4):
                xs, xk = xs_r.next()
                ph.add(SP, e_dma(xs[:, :, 0:n], srcv[:, q4 * 4:(q4 + 1) * 4, :]), writes=[xk], dma=xk)
                for kk in range(4):
                    k = q4 * 4 + kk
                    ph.add(ACT, e_act(u[:, k, 0:n], xs[:, kk, 0:n], AF.Identity, scale=C.sc1[:, mc, 0, k:k + 1],
                                      bias=C.mod[:, mc, k:k + 1]), reads=[xk, "g"], writes=[(uk, k)])
            ukeys = [(uk, k) for k in range(KC)]
            ph.add(SP, e_dma(tb128[:, :, 0:n], C.tab128[:, :, tc0:tc0 + n].rearrange("a p t -> p a t")),
                   writes=["tb128"], dma="tb128")
            ph.add(SP, e_dma(tb64[:, :, 0:n], C.tab64[:, :, tc0:tc0 + n].rearrange("a p t -> p a t")),
                   writes=["tb64"], dma="tb64")
            if qcol is not None:
                ph.add(DVE, e_ts(cosq[:, 0:n], tb128[:, 0, 0:n], C.gqs[:, 0:1], ALU.mult), reads=["tb128"], writes=["cosq"])
                ph.add(DVE, e_ts(c64q[:, 0:n], tb64[:, 0, 0:n], SCALE_B, ALU.mult), reads=["tb64"], writes=["c64q"])
                ph.add(DVE, e_ts(s64q[:, 0:n], tb64[:, 1, 0:n], SCALE_B, ALU.mult), reads=["tb64"], writes=["s64q"])
            if kd is not None:
                ph.add(DVE, e_ts(cosk[:, 0:n], tb128[:, 0, 0:n], C.gks[:, 0:1], ALU.mult, math.sqrt(128.0), ALU.mult),
                       reads=["tb128"], writes=["cosk"])
                ph.add(DVE, e_ts(sink[:, 0:n], tb128[:, 1, 0:n], math.sqrt(128.0), ALU.mult), reads=["tb128"], writes=["sink"])

            def gemm_chunk(w, wk, cc, m=128):
                b = mainbank()
                ph.add(PE, e_mm([(ps[b][0:m, 0:n], w[:, k, cc:cc + m], u[:, k, 0:n], k == 0, k == KC - 1) for k in range(KC)]),
                       reads=[wk] + ukeys, writes=[("ps", b)])
                return b

            def rope128(b, rt, rtkey, cos_t, coskey, sin_t, sinkey, dst):
                a0, a1 = auxpair()
                sq, sqk = sq_r.next()
                ph.add(ACT, e_act(sq[:, 0:n], ps[b][:, 0:n], AF.Square), reads=[("ps", b)], writes=[sqk])
                ph.add(PE, e_mm([(ps[a0][:, 0:n], C.ones_bf[:], sq[:, 0:n], True, True)]), reads=[sqk, "g"], writes=[("ps", a0)])
                ln, lnk = ln_r.next()
                ph.add(ACT, e_act(ln[:, 0:n], ps[a0][:, 0:n], AF.Ln, bias=128.0 * EPS), reads=[("ps", a0)], writes=[lnk])
                rs, rsk = rs_r.next()
                ph.add(ACT, e_act(rs[:, 0:n], ln[:, 0:n], AF.Exp, scale=-0.5), reads=[lnk], writes=[rsk])
                qn, qnk = qn_r.next()
                ph.add(DVE, e_tt(qn[:, 0:n], ps[b][:, 0:n], rs[:, 0:n], ALU.mult), reads=[("ps", b), rsk], writes=[qnk])
                ph.add(PE, e_mm([(ps[a1][:, 0:n], rt[:], qn[:, 0:n], True, True)]), reads=[qnk, "g"], writes=[("ps", a1)])
                t1, t1k = t1_r.next()
                ph.add(POOL, e_tt(t1[:, 0:n], qn[:, 0:n], cos_t[:, 0:n], ALU.mult), reads=[qnk, coskey], writes=[t1k])
                t2, t2k = t2_r.next()
                ph.add(DVE, e_tt(t2[:, 0:n], ps[a1][:, 0:n], sin_t[:, 0:n], ALU.mult), reads=[("ps", a1), sinkey], writes=[t2k])
                sg, sgk = stg_r.next()
                ph.add(POOL, e_tt(sg[:, 0:n], t1[:, 0:n], t2[:, 0:n], ALU.add), reads=[t1k, t2k], writes=[sgk])
                store(dst, sg[:, 0:n], sgk)

            def rope64(b, cos_t, coskey, sin_t, sinkey, dst):
                a0, a1 = auxpair()
                qn, qnk = qn_r.next()
                ph.add(DVE, e_copy(qn[0:64, 0:n], ps[b][0:64, 0:n]), reads=[("ps", b)], writes=[qnk])
                ph.add(PE, e_mm([(ps[a1][0:64, 0:n], C.rt64b[:], qn[0:64, 0:n], True, True)]), reads=[qnk, "g"], writes=[("ps", a1)])
                t1, t1k = t1_r.next()
                ph.add(POOL, e_tt(t1[0:64, 0:n], qn[0:64, 0:n], cos_t[:, 0:n], ALU.mult), reads=[qnk, coskey], writes=[t1k])
                t2, t2k = t2_r.next()
                ph.add(DVE, e_tt(t2[0:64, 0:n], ps[a1][0:64, 0:n], sin_t[:, 0:n], ALU.mult), reads=[("ps", a1), sinkey], writes=[t2k])
                sg, sgk = stg_r.next()
                ph.add(POOL, e_tt(sg[0:64, 0:n], t1[0:64, 0:n], t2[0:64, 0:n], ALU.add), reads=[t1k, t2k], writes=[sgk])
                store(dst, sg[0:64, 0:n], sgk)

            def copy_out(b, dst, scale=1.0, func=AF.Copy, m=128):
                sg, sgk = stg_r.next()
                ph.add(ACT, e_act(sg[0:m, 0:n], ps[b][0:m, 0:n], func, scale=scale), reads=[("ps", b)], writes=[sgk])
                store(dst, sg[0:m, 0:n], sgk)

            def latent(seg, nch, lat, latkey):
                c0, wd = SEG[seg]
                w, wk = load_w(c0, wd)
                banks = [gemm_chunk(w, wk, c * 128) for c in range(nch)]
                a0, a1 = auxpair()
                sqs = []
                for c in range(nch):
                    sq, sqk = sq_r.next()
                    ph.add(ACT, e_act(sq[:, 0:n], ps[banks[c]][:, 0:n], AF.Square), reads=[("ps", banks[c])], writes=[sqk])
                    ph.add(PE, e_mm([(ps[a0][:, 0:n], C.ones_bf[:], sq[:, 0:n], c == 0, c == nch - 1)]),
                           reads=[sqk, "g"], writes=[("ps", a0)], acc=True)
                ln, lnk = ln_r.next()
                ph.add(ACT, e_act(ln[:, 0:n], ps[a0][:, 0:n], AF.Ln, bias=128.0 * nch * EPS), reads=[("ps", a0)], writes=[lnk])
                rs, rsk = rs_r.next()
                ph.add(ACT, e_act(rs[:, 0:n], ln[:, 0:n], AF.Exp, scale=-0.5), reads=[lnk], writes=[rsk])
                for c in range(nch):
                    ph.add(DVE, e_tt(lat[:, c, 0:n], ps[banks[c]][:, 0:n], rs[:, 0:n], ALU.mult),
                           reads=[("ps", banks[c]), rsk], writes=[(latkey, c)])

            def tokmajor(seg, dstv, key0):
                c0, wd = SEG[seg]
                w, wk = load_w(c0, wd)
                for s0 in range(0, wd, 512):
                    ncols = min(512, wd - s0)
                    for tt in range(n // 128):
                        b = mainbank()
                        ph.add(PE, e_mm([(ps[b][:, 0:ncols], u[:, k, tt * 128:(tt + 1) * 128], w[:, k, s0:s0 + ncols], k == 0, k == KC - 1)
                                         for k in range(KC)]), reads=[wk] + ukeys, writes=[("ps", b)])
                        v, vk = vst_r.next()
                        ph.add(DVE if tt % 2 == 0 else ACT,
                               e_copy(v[:, 0:ncols], ps[b][:, 0:ncols]) if tt % 2 == 0 else e_act(v[:, 0:ncols], ps[b][:, 0:ncols], AF.Copy),
                               reads=[("ps", b)], writes=[vk])
                        store(dstv[key0 + tt * 128:key0 + (tt + 1) * 128, s0:s0 + ncols], v[:, 0:ncols], vk)

            if qcol is not None:
                c0, wd = SEG["aq"]
                for s0 in range(0, wd, SW):
                    ncols = min(SW, wd - s0)
                    w, wk = load_w(c0 + s0, ncols)
                    for cc in range(0, ncols, 128):
                        h = (s0 + cc) // 128
                        b = gemm_chunk(w, wk, cc)
                        rope128(b, C.rtq, "g", cosq, "cosq", tb128[:, 1, :], "tb128", C.QA[h, :, qcol:qcol + n])
                latent("bq", 4, latq, "latq")
                for h in range(5):
                    b = mainbank()
                    ph.add(PE, e_mm([(ps[b][:, 0:n], C.wuq[:, c, h * 192:h * 192 + 128], latq[:, c, 0:n], c == 0, c == 3) for c in range(4)]),
                           reads=[("latq", c) for c in range(4)] + ["g"], writes=[("ps", b)])
                    copy_out(b, C.QBN[h, :, qcol:qcol + n], scale=SCALE_B)
                    b = mainbank()
                    ph.add(PE, e_mm([(ps[b][0:64, 0:n], C.wuq[:, c, h * 192 + 128:h * 192 + 192], latq[:, c, 0:n], c == 0, c == 3) for c in range(4)]),
                           reads=[("latq", c) for c in range(4)] + ["g"], writes=[("ps", b)])
                    rope64(b, c64q, "c64q", s64q, "s64q", C.QBR[h, :, qcol:qcol + n])
                c0, wd = SEG["cq"]
                for s0 in range(0, wd, SW):
                    ncols = min(SW, wd - s0)
                    w, wk = load_w(c0 + s0, ncols)
                    for cc in range(0, ncols, 128):
                        h = (s0 + cc) // 128
                        b = gemm_chunk(w, wk, cc)
                        copy_out(b, C.QC[h, :, qcol:qcol + n], scale=SCALE_C)
                c0, wd = SEG["gate"]
                for s0 in range(0, wd, SW):
                    w, wk = load_w(c0 + s0, SW)
                    for cc in range(0, SW, 128):
                        j = (s0 + cc) // 128
                        b = gemm_chunk(w, wk, cc)
                        copy_out(b, C.G[j, :, qcol:qcol + n], func=AF.Sigmoid)
            if kd is not None:
                c0, wd = SEG["ak"]
                w, wk = load_w(c0, wd)
                for h in range(2):
                    b = gemm_chunk(w, wk, h * 128)
                    rope128(b, C.rtk, "g", cosk, "cosk", sink, "sink", C.KA[h, :, kd:kd + n])
                tokmajor("av", C.VA, kd)
                latent("bkv", 2, latkv, "latkv")
                for h in range(5):
                    b = mainbank()
                    ph.add(PE, e_mm([(ps[b][:, 0:n], C.wukv[:, c, h * 256:h * 256 + 128], latkv[:, c, 0:n], c == 0, c == 1) for c in range(2)]),
                           reads=[("latkv", c) for c in range(2)] + ["g"], writes=[("ps", b)])
                    copy_out(b, C.KBN[h, :, kd:kd + n])
                for tt in range(n // 128):
                    b0 = mainbank()
                    ph.add(PE, e_mm([(ps[b0][:, 0:512], latkv[:, c, tt * 128:(tt + 1) * 128], C.wukv_v[:, c, 0:512], c == 0, c == 1) for c in range(2)]),
                           reads=[("latkv", c) for c in range(2)] + ["g"], writes=[("ps", b0)])
                    b1 = mainbank()
                    ph.add(PE, e_mm([(ps[b1][:, 0:128], latkv[:, c, tt * 128:(tt + 1) * 128], C.wukv_v[:, c, 512:640], c == 0, c == 1) for c in range(2)]),
                           reads=[("latkv", c) for c in range(2)] + ["g"], writes=[("ps", b1)])
                    v, vk = vst_r.next()
                    ph.add(DVE, e_copy(v[:, 0:512], ps[b0][:, 0:512]), reads=[("ps", b0)], writes=[vk])
                    ph.add(ACT, e_act(v[:, 512:640], ps[b1][:, 0:128], AF.Copy), reads=[("ps", b1)], writes=[vk])
                    store(C.VB[kd + tt * 128:kd + (tt + 1) * 128, :], v[:, :], vk)
                c0, wd = SEG["bkr"]
                w, wk = load_w(c0, wd)
                b = gemm_chunk(w, wk, 0, m=64)
                rope64(b, tb64[:, 0, :], "tb64", tb64[:, 1, :], "tb64", C.KBR[:, kd:kd + n])
            if kn is not None:
                c0, wd = SEG["ck"]
                for s0 in range(0, wd, SW):
                    ncols = min(SW, wd - s0)
                    w, wk = load_w(c0 + s0, ncols)
                    for cc in range(0, ncols, 128):
                        h = (s0 + cc) // 128
                        b = gemm_chunk(w, wk, cc)
                        copy_out(b, C.KCs[h, :, kn:kn + n])
                tokmajor("cv", C.VC, kn)
        nops = ph.finalize()
    return nops


def _rope_table(pos, dim):
    pos = np.asarray(pos, dtype=np.int64)
    row = (pos // 64).astype(np.float32)
    col = (pos % 64).astype(np.float32)
    quarter = dim // 4
    inv_freq = (np.float32(10000.0) ** (-np.arange(quarter, dtype=np.float32) / np.float32(quarter))).astype(np.float32)
    ang_r = row[:, None] * inv_freq
    ang_c = col[:, None] * inv_freq
    ang = np.concatenate([ang_r, ang_r, ang_c, ang_c], axis=-1).astype(np.float32)
    return np.cos(ang).T.astype(np.float32), np.sin(ang).T.astype(np.float32)


def _rot_T(dim):
    q = dim // 4
    RT = np.zeros((dim, dim), np.float32)
    for m in range(dim):
        b = (m % (2 * q)) // q
        if b == 0:
            RT[m + q, m] = -1.0
        else:
            RT[m - q, m] = 1.0
    return RT


def _local_pos(half):
    own = np.arange(half * NOWN, (half + 1) * NOWN)
    oth = np.arange((1 - half) * NOWN, (2 - half) * NOWN)
    return np.concatenate([own, oth])


def _tables(half):
    pos = _local_pos(half)
    t128 = np.zeros((2, 128, TABW), np.float32)
    t64 = np.zeros((2, 64, TABW), np.float32)
    for dim, t in ((128, t128), (64, t64)):
        c, s = _rope_table(pos, dim)
        t[0, :, 0:NNAT] = c
        t[1, :, 0:NNAT] = s
        t[0, :, NNAT:] = 1.0
    return t128, t64


def _na_bias_index(half):
    ri = np.zeros((8, 8, 128, 512), np.int64)
    ci = np.zeros((8, 8, 128, 512), np.int64)
    valid = np.zeros((8, 8, 128, 512), bool)
    for jl in range(8):
        s_, j = jl // 4, jl % 4
        qhalf = half if s_ == 0 else 1 - half
        q_rows = qhalf * 32 + 8 * j + np.arange(8)
        qr = np.repeat(q_rows, 64)
        qc = np.tile(np.arange(64), 8)
        r_start = np.clip(qr - 4, 0, 64 - 8)
        c_start = np.clip(qc - 8, 0, 64 - 16)
        for i, t in enumerate(na_tiles(jl)):
            thalf = half if t < 16 else 1 - half
            rows = thalf * 32 + 2 * (t % 16) + np.arange(2)
            kr = np.repeat(rows, 64)
            kc = np.tile(np.arange(64), 2)
            v = ((kr[:, None] >= r_start[None, :]) & (kr[:, None] < r_start[None, :] + 8) &
                 (kc[:, None] >= c_start[None, :]) & (kc[:, None] < c_start[None, :] + 16))
            ri[jl, i] = np.clip(kr[:, None] - qr[None, :] + 7, 0, 14)
            ci[jl, i] = np.clip(kc[:, None] - qc[None, :] + 15, 0, 30)
            valid[jl, i] = v
    return ri, ci, valid


_CONST_CACHE = {}


def _consts(half):
    if half not in _CONST_CACHE:
        t128, t64 = _tables(half)
        _CONST_CACHE[half] = dict(tab128=t128, tab64=t64, nabidx=_na_bias_index(half))
    return _CONST_CACHE[half]


def _fm(v, nch):
    return np.ascontiguousarray(np.asarray(v, np.float32).reshape(nch, 128).T)


def prep_layer_shared(inp, l, with_moe=True):
    sh = {}
    sh["w_ada"] = np.ascontiguousarray(inp["w_ada"][l])
    sh["b_ada"] = _fm(inp["b_ada"][l], 96)
    sh["w_in"] = np.ascontiguousarray(inp["w_in"][l])
    sh["gq"] = np.ascontiguousarray(inp["gqa_q_norm"][l].reshape(128, 1))
    sh["gk"] = np.ascontiguousarray(inp["gqa_k_norm"][l].reshape(128, 1))
    sh["mlaq_g"] = _fm(inp["mla_q_norm"][l], 4)
    sh["mlakv_g"] = _fm(inp["mla_kv_norm"][l], 2)
    sh["w_uq"] = np.ascontiguousarray(inp["mla_w_uq"][l])
    sh["w_ukv"] = np.ascontiguousarray(inp["mla_w_ukv"][l])
    sh["w_ba"] = np.ascontiguousarray(inp["w_branch_a"][l])
    sh["w_bb"] = np.ascontiguousarray(inp["w_branch_b"][l])
    sh["w_bc"] = np.ascontiguousarray(inp["w_branch_c"][l])
    sh["w_out"] = np.ascontiguousarray(inp["w_out"][l])
    sh["lnp"] = np.ascontiguousarray(np.stack([_fm(inp[n][l], 16) for n in ("ln1_g", "ln1_b", "ln2_g", "ln2_b")], axis=1))
    sh["w_r"] = np.ascontiguousarray(np.concatenate([inp["w_router_group"][l], inp["w_router_expert"][l]], axis=1))
    br = np.concatenate([inp["b_router_group"][l], inp["b_router_expert"][l]])[None, :]
    sh["b_r"] = np.ascontiguousarray(np.broadcast_to(br, (128, 72)).astype(np.float32))
    if with_moe:
        sh["w_eg"] = np.ascontiguousarray(inp["w_expert_gate"][l])
        sh["w_eu"] = np.ascontiguousarray(inp["w_expert_up"][l])
        sh["w_ed"] = np.ascontiguousarray(inp["w_expert_down"][l])
    return {k + str(l): v for k, v in sh.items()}


def prep_core(inp, core, shared, nlayers=2):
    b, half = core // 2, core % 2
    cst = _consts(half)
    m = dict(shared)
    xb = np.asarray(inp["x"][b], np.float32)
    pos = _local_pos(half)
    m["xt"] = np.ascontiguousarray(np.concatenate([xb[pos].T, np.asarray(inp["ctx"][b], np.float32).T], axis=1))
    cond = np.stack([inp["c"][b], inp["c_ctx"]], axis=1)
    m["cond"] = np.ascontiguousarray(cond.reshape(KC, 128, 2).transpose(1, 0, 2).reshape(128, 32).astype(np.float32))
    m["tab128"] = cst["tab128"]
    m["tab64"] = cst["tab64"]
    m["rt128"] = _rot_T(128)
    m["rt64"] = _rot_T(64)
    m["ident"] = np.eye(128, dtype=np.float32)
    ri, ci, valid = cst["nabidx"]
    for l in range(nlayers):
        nq = 8 if l == 0 else 4
        rpb = np.asarray(inp["na_rpb"][l], np.float32)
        m["nab" + str(l)] = np.ascontiguousarray(
            np.where(valid[None, :nq], rpb[:, ri[:nq], ci[:nq]], np.float32(-30000.0)).astype(np.float32))
    return m


def proj_blocks(src, layer):
    blocks = []
    for i in range(8):
        q = (i * 512) if (layer == 0 or i < 4) else None
        blocks.append(dict(src=src[:, i * 512:(i + 1) * 512], ntok=512, modcol=0, tabcol=i * 512, qcol=q, kd=i * 512, kn=i * 512))
    blocks.append(dict(src=src[:, NNAT:TL], ntok=NCTX, modcol=1, tabcol=NNAT, qcol=(NNAT if layer == 0 else None), kd=NNAT, kn=NNAT))
    return blocks


def na_tiles(jl):
    s_, j = jl // 4, jl % 4
    base = 16 * s_
    obase = 16 * (1 - s_)
    tiles = []
    for i in range(8):
        lt = 4 * j - 2 + i
        if 0 <= lt < 16:
            tiles.append(base + lt)
        elif lt < 0:
            tiles.append(obase + 16 + lt)
        else:
            tiles.append(obase + lt - 16)
    return tiles


def q_blocks(layer):
    qb = [(i * 512, 512, i) for i in range(8 if layer == 0 else 4)]
    if layer == 0:
        qb.append((NNAT, NCTX, None))
    return qb


def phase_attn(C, W, kind, qblocks):
    nc = C.nc
    ph = Phase(nc, C.sems, "att" + kind)
    ps = C.ps
    nkeys = NKEY
    nkt = nkeys // 128
    with ExitStack() as st:
        sb = lambda name, shape, dt: st.enter_context(nc.sbuf_tensor(uname(name), shape, dt))
        kt_r = Ring(nc, st, "t_k", [128, nkeys], BF16, 2)
        v_r = Ring(nc, st, "t_v", [128, nkt, 128], BF16, 2)
        q_r = Ring(nc, st, "t_q", [128, 512], BF16, 2)
        p_r = Ring(nc, st, "t_p", [128, 512], BF16, 3)
        rd_r = Ring(nc, st, "t_rd", [128, 512], F32, 2)
        o_r = Ring(nc, st, "t_o", [128, 512], BF16, 2)
        if kind == "b":
            kr = sb("t_kr", [64, nkeys], BF16)
            qr_r = Ring(nc, st, "t_qr", [64, 512], BF16, 2)
            ph.add(SP, e_dma(kr[:], C.KBR), writes=["kr"], dma="kr")
        if kind == "c":
            b_r = Ring(nc, st, "t_b", [128, 512], BF16, 3)
        s_i = [0]
        blk_i = [0]
        nheads = {"a": 6, "b": 5, "c": 5}[kind]
        Ksrc = {"a": C.KA, "b": C.KBN, "c": C.KCs}[kind]
        Vsrc = {"a": C.VA, "b": C.VB, "c": C.VC}[kind]
        Qsrc = {"a": C.QA, "b": C.QBN, "c": C.QC}[kind]
        Odst = {"a": C.OA, "b": C.OB, "c": C.OC}[kind]
        Vv = Vsrc.rearrange("(t p) c -> p t c", p=128)
        kT = vv = None
        for h in range(nheads):
            kvh = h // 3 if kind == "a" else h
            if kind != "a" or h % 3 == 0:
                kT, kTk = kt_r.next()
                ph.add(SP, e_dma(kT[:], Ksrc[kvh]), writes=[kTk], dma=kTk)
                vv, vk = v_r.next()
                ph.add(SP, e_dma(vv[:], Vv[:, :, kvh * 128:(kvh + 1) * 128]), writes=[vk], dma=vk)
            for (qcol, n, j) in qblocks:
                q, qk = q_r.next()
                ph.add(SP, e_dma(q[:, 0:n], Qsrc[h, :, qcol:qcol + n]), writes=[qk], dma=qk)
                rkeys = [qk, kTk]
                if kind == "b":
                    qr, qrk = qr_r.next()
                    ph.add(SP, e_dma(qr[:, 0:n], C.QBR[h, :, qcol:qcol + n]), writes=[qrk], dma=qrk)
                    rkeys += [qrk, "kr"]
                if j is None:
                    tiles = [(nkt - 2, None), (nkt - 1, None)]
                elif kind == "c":
                    tiles = [(loc, i) for i, loc in enumerate(na_tiles(j))]
                    tiles += [(nkt - 2, None), (nkt - 1, None)]
                else:
                    tiles = [(t, None) for t in range(nkt)]
                bo = 3 + (blk_i[0] % 2)
                bd = 5 + (blk_i[0] % 2)
                blk_i[0] += 1
                nt = len(tiles)
                pend = {}

                def emit_s(idx):
                    t, bi = tiles[idx]
                    sbk = s_i[0] % 3
                    s_i[0] += 1
                    items = [(ps[sbk][:, 0:n], kT[:, t * 128:(t + 1) * 128], q[:, 0:n], True, (kind == "a") or (kind == "c" and bi is None))]
                    rk = list(rkeys)
                    if kind == "b":
                        items.append((ps[sbk][:, 0:n], kr[:, t * 128:(t + 1) * 128], qr[:, 0:n], False, True))
                    if kind == "c" and bi is not None:
                        bt, btk = b_r.next()
                        ph.add(POOL, e_dma(bt[:, :], W.nab[h, j, bi]), writes=[btk], dma=btk)
                        items.append((ps[sbk][:, 0:n], C.ident_bf[:], bt[:, 0:n], False, True))
                        rk.append(btk)
                    ph.add(PE, e_mm(items), reads=rk, writes=[("ps", sbk)])
                    pend[idx] = sbk

                emit_s(0)
                for idx in range(nt):
                    if idx + 1 < nt:
                        emit_s(idx + 1)
                    sbk = pend.pop(idx)
                    p, pk = p_r.next()
                    ph.add(ACT, e_act(p[:, 0:n], ps[sbk][:, 0:n], AF.Exp), reads=[("ps", sbk)], writes=[pk])
                    t, _ = tiles[idx]
                    ph.add(PE, e_mm([(ps[bo][:, 0:n], vv[:, t, :], p[:, 0:n], idx == 0, idx == nt - 1),
                                     (ps[bd][:, 0:n], C.ones_bf[:], p[:, 0:n], idx == 0, idx == nt - 1)]),
                           reads=[pk, vk, "g"], writes=[("ps", bo), ("ps", bd)], acc=True)
                rd, rdk = rd_r.next()
                ph.add(DVE, lambda e, rd=rd, bd=bd, n=n: e.reciprocal(out=rd[:, 0:n], in_=ps[bd][:, 0:n]), reads=[("ps", bd)], writes=[rdk])
                o, ok = o_r.next()
                ph.add(DVE, e_tt(o[:, 0:n], ps[bo][:, 0:n], rd[:, 0:n], ALU.mult), reads=[("ps", bo), rdk], writes=[ok])
                ph.add(SP, e_dma(Odst[h, :, qcol:qcol + n], o[:, 0:n]), reads=[ok], dma=ok)
        nops = ph.finalize()
    return nops


def ln_block(ph, C, rings, v, vkey, n, col, gi, dst, dstkey_prefix, off=0):
    ps = C.ps
    sq_r, st_r, o_r = rings["sq"], rings["stat"], rings["out"]
    bs, bq = 6, 7
    for m in range(KC):
        sq, sqk = sq_r.next()
        ph.add(ACT, e_act(sq[:, 0:n], v[:, m, off:off + n], AF.Square), reads=[(vkey, m)], writes=[sqk])
        ph.add(PE, e_mm([(ps[bs][:, 0:n], C.ones_f[:], v[:, m, off:off + n], m == 0, m == KC - 1)]),
               reads=[(vkey, m), "g"], writes=[("ps", bs)], acc=True)
        ph.add(PE, e_mm([(ps[bq][:, 0:n], C.ones_f[:], sq[:, 0:n], m == 0, m == KC - 1)]),
               reads=[sqk, "g"], writes=[("ps", bq)], acc=True)
    mean, meank = st_r.next()
    ph.add(DVE, e_ts(mean[:, 0:n], ps[bs][:, 0:n], 1.0 / D, ALU.mult), reads=[("ps", bs)], writes=[meank])
    msq, msqk = st_r.next()
    ph.add(DVE, e_tt(msq[:, 0:n], mean[:, 0:n], mean[:, 0:n], ALU.mult), reads=[meank], writes=[msqk])
    var, vark = st_r.next()
    ph.add(DVE, lambda e, var=var, msq=msq: e.scalar_tensor_tensor(out=var[:, 0:n], in0=ps[bq][:, 0:n], scalar=1.0 / D, in1=msq[:, 0:n],
                                                                     op0=ALU.mult, op1=ALU.subtract),
           reads=[("ps", bq), msqk], writes=[vark])
    lnv, lnk = st_r.next()
    ph.add(ACT, e_act(lnv[:, 0:n], var[:, 0:n], AF.Ln, bias=EPS), reads=[vark], writes=[lnk])
    rstd, rstdk = st_r.next()
    ph.add(ACT, e_act(rstd[:, 0:n], lnv[:, 0:n], AF.Exp, scale=-0.5), reads=[lnk], writes=[rstdk])
    for m in range(KC):
        t, tk = sq_r.next()
        ph.add(DVE, e_tt(t[:, 0:n], v[:, m, off:off + n], mean[:, 0:n], ALU.subtract), reads=[(vkey, m), meank], writes=[tk])
        t2, t2k = sq_r.next()
        ph.add(POOL, e_tt(t2[:, 0:n], t[:, 0:n], rstd[:, 0:n], ALU.mult), reads=[tk, rstdk], writes=[t2k])
        o, ok = o_r.next()
        ph.add(ACT, e_act(o[:, 0:n], t2[:, 0:n], AF.Identity, scale=C.lnps[:, gi, m:m + 1], bias=C.lnps[:, gi + 1, m:m + 1]),
               reads=[t2k, "g"], writes=[ok])
        ph.add(SP, e_dma(dst[m * 128:(m + 1) * 128, :], o[:, 0:n]), reads=[ok], dma=ok)


def phase_merge(C, W, xsrc, layer):
    nc = C.nc
    ph = Phase(nc, C.sems, "merge")
    ps = C.ps
    NB = 256
    with ExitStack() as st:
        sb = lambda name, shape, dt: st.enter_context(nc.sbuf_tensor(uname(name), shape, dt))
        wba = sb("m_wba", [128, 6, D], BF16)
        wbb = sb("m_wbb", [128, 5, D], BF16)
        wbc = sb("m_wbc", [128, 5, D], BF16)
        ph.add(POOL, e_dma(wba[:], W.w_ba.rearrange("(k p) n -> p k n", p=128)), writes=["wba"], dma="wba")
        ph.add(POOL, e_dma(wbb[:], W.w_bb.rearrange("(k p) n -> p k n", p=128)), writes=["wbb"], dma="wbb")
        ph.add(POOL, e_dma(wbc[:], W.w_bc.rearrange("(k p) n -> p k n", p=128)), writes=["wbc"], dma="wbc")
        wo_r = Ring(nc, st, "m_wo", [128, KC, 512], BF16, 2)
        o_in = Ring(nc, st, "m_oin", [128, 16, NB], BF16, 2)
        g_r = Ring(nc, st, "m_g", [128, 3, NB], BF16, 3)
        ta_r = Ring(nc, st, "m_ta", [128, NB], F32, 6)
        mix = sb("m_mix", [128, KC, NB], BF16)
        v = sb("m_v", [128, KC, NB], F32)
        x_r = Ring(nc, st, "m_x", [128, NB], F32, 3)
        xa_r = Ring(nc, st, "m_xa", [128, NB], F32, 3)
        rings = dict(sq=Ring(nc, st, "m_sq", [128, NB], F32, 4), stat=Ring(nc, st, "m_st", [128, NB], F32, 5),
                     out=Ring(nc, st, "m_out", [128, NB], F32, 3))
        wov = W.w_out.rearrange("(kc p) n -> p kc n", p=128)
        Gv = C.G.rearrange("(b m) p t -> m p b t", b=3)
        blocks = [(i * NB, NB, 0) for i in range((NNAT if layer == 0 else NOWN) // NB)]
        if layer == 0:
            blocks.append((NNAT, NCTX, 1))
        pi = [0]
        for (qcol, n, col) in blocks:
            oin, oink = o_in.next()
            ph.add(SP, e_dma(oin[:, 0:6, 0:n], C.OA[:, :, qcol:qcol + n].rearrange("h p t -> p h t")), writes=[(oink, 0)], dma=(oink, 0))
            ph.add(SP, e_dma(oin[:, 6:11, 0:n], C.OB[:, :, qcol:qcol + n].rearrange("h p t -> p h t")), writes=[(oink, 1)], dma=(oink, 1))
            ph.add(SP, e_dma(oin[:, 11:16, 0:n], C.OC[:, :, qcol:qcol + n].rearrange("h p t -> p h t")), writes=[(oink, 2)], dma=(oink, 2))
            for m in range(KC):
                g, gk = g_r.next()
                ph.add(SP, e_dma(g[:, :, 0:n], Gv[m, :, :, qcol:qcol + n]), writes=[gk], dma=gk)
                b3 = [(pi[0] * 3 + i) % 6 for i in range(3)]
                pi[0] += 1
                ms = slice(m * 128, (m + 1) * 128)
                ph.add(PE, e_mm([(ps[b3[0]][:, 0:n], wba[:, k, ms], oin[:, k, 0:n], k == 0, k == 5) for k in range(6)]),
                       reads=["wba", (oink, 0)], writes=[("ps", b3[0])])
                ph.add(PE, e_mm([(ps[b3[1]][:, 0:n], wbb[:, k, ms], oin[:, 6 + k, 0:n], k == 0, k == 4) for k in range(5)]),
                       reads=["wbb", (oink, 1)], writes=[("ps", b3[1])])
                ph.add(PE, e_mm([(ps[b3[2]][:, 0:n], wbc[:, k, ms], oin[:, 11 + k, 0:n], k == 0, k == 4) for k in range(5)]),
                       reads=["wbc", (oink, 2)], writes=[("ps", b3[2])])
                ta, tak = ta_r.next()
                ph.add(DVE, e_tt(ta[:, 0:n], ps[b3[0]][:, 0:n], g[:, 0, 0:n], ALU.mult), reads=[("ps", b3[0]), gk], writes=[tak])
                tb, tbk = ta_r.next()
                ph.add(DVE, e_tt(tb[:, 0:n], ps[b3[1]][:, 0:n], g[:, 1, 0:n], ALU.mult), reads=[("ps", b3[1]), gk], writes=[tbk])
                tcc, tck = ta_r.next()
                ph.add(DVE, e_tt(tcc[:, 0:n], ps[b3[2]][:, 0:n], g[:, 2, 0:n], ALU.mult), reads=[("ps", b3[2]), gk], writes=[tck])
                ph.add(POOL, e_tt(ta[:, 0:n], ta[:, 0:n], tb[:, 0:n], ALU.add), reads=[tak, tbk], writes=[tak])
                ph.add(POOL, e_tt(mix[:, m, 0:n], ta[:, 0:n], tcc[:, 0:n], ALU.add), reads=[tak, tck], writes=[("mix", m)])
            mixkeys = [("mix", m) for m in range(KC)]
            for m in range(KC):
                if m % 4 == 0:
                    wo, wok = wo_r.next()
                    ph.add(POOL, e_dma(wo[:], wov[:, :, m * 128:m * 128 + 512]), writes=[wok], dma=wok)
                by = 6 + (m % 2)
                ph.add(PE, e_mm([(ps[by][:, 0:n], wo[:, k, (m % 4) * 128:(m % 4 + 1) * 128], mix[:, k, 0:n], k == 0, k == KC - 1) for k in range(KC)]),
                       reads=[wok] + mixkeys, writes=[("ps", by)])
                x, xk = x_r.next()
                ph.add(SP, e_dma(x[:, 0:n], xsrc[m * 128:(m + 1) * 128, qcol:qcol + n]), writes=[xk], dma=xk)
                xa, xak = xa_r.next()
                ph.add(ACT, e_act(xa[:, 0:n], x[:, 0:n], AF.Identity, scale=ALPHA), reads=[xk], writes=[xak])
                ph.add(DVE, lambda e, m=m, by=by, xa=xa, n=n, col=col: e.scalar_tensor_tensor(
                    out=v[:, m, 0:n], in0=ps[by][:, 0:n], scalar=C.mod[:, col, 32 + m:33 + m], in1=xa[:, 0:n], op0=ALU.mult, op1=ALU.add),
                    reads=[("ps", by), xak, "g"], writes=[("v", m)])
            ln_block(ph, C, rings, v, "v", n, col, 0, C.X1[:, qcol:qcol + n], "x1")
        nops = ph.finalize()
    return nops


def phase_moe(C, W, layer, dst, n_exp=NEXP):
    nc = C.nc
    ph = Phase(nc, C.sems, "moe")
    ps = C.ps
    NB = 512
    BIG = 1.0e30
    with ExitStack() as st:
        sb = lambda name, shape, dt: st.enter_context(nc.sbuf_tensor(uname(name), shape, dt))
        wr = sb("e_wr", [128, KC, 72], F32)
        br = sb("e_br", [128, 72], F32)
        ph.add(SP, e_dma(wr[:], W.w_r.rearrange("(kc p) n -> p kc n", p=128)), writes=["wr"], dma="wr")
        ph.add(SP, e_dma(br[:], W.b_r), writes=["br"], dma="br")
        nops = ph.finalize()
        xs_r = Ring(nc, st, "e_xs", [128, 4, NB], F32, 1)
        u32_r = Ring(nc, st, "e_u32", [128, 4, NB], F32, 2)
        u16 = sb("e_u16", [128, KC, NB], BF16)
        w_r = Ring(nc, st, "e_w", [128, 8192], BF16, 4)
        mx = sb("e_mx", [128, KC, NB], F32)
        hh_r = Ring(nc, st, "e_hh", [128, NB], F32, 2)
        sg_r = Ring(nc, st, "e_sg", [128, NB], F32, 2)
        H_r = Ring(nc, st, "e_H", [128, 4, NB], BF16, 2)
        wgtT = sb("e_wgtT", [64, NB], F32)
        wm_r = Ring(nc, st, "e_wm", [64, NB], F32, 2)
        lg = sb("e_lg", [128, 72], F32)
        r8 = sb("e_r8", [128, 8], F32)
        goh = sb("e_goh", [128, 8], F32)
        pen = sb("e_pen", [128, 8], F32)
        ex8 = sb("e_ex8", [128, 8], F32)
        lm = sb("e_lm", [128, 64], F32)
        lm2 = sb("e_lm2", [128, 64], F32)
        oh1 = sb("e_oh1", [128, 64], F32)
        oh2 = sb("e_oh2", [128, 64], F32)
        sc = sb("e_sc", [128, 16], F32)
        wgt = sb("e_wgt", [128, 64], F32)
        rings = dict(sq=Ring(nc, st, "e_sq", [128, 256], F32, 4), stat=Ring(nc, st, "e_st", [128, 256], F32, 5),
                     out=Ring(nc, st, "e_out", [128, 256], F32, 3))
        x_r = Ring(nc, st, "e_x", [128, NB], F32, 2)
        xa_r = Ring(nc, st, "e_xa", [128, NB], F32, 2)
        blocks = [(i * NB, NB, 0) for i in range((NNAT if layer == 0 else NOWN) // NB)]
        if layer == 0:
            blocks.append((NNAT, NCTX, 1))
        gi = [0]
        for (qcol, n, col) in blocks:
            ph = Phase(nc, C.sems, "moe_b")
            ntt = n // 128
            srcv = C.X1[:, qcol:qcol + n].rearrange("(kc p) t -> p kc t", p=128)
            for q4 in range(4):
                xs, xk = xs_r.next()
                ph.add(SP, e_dma(xs[:, :, 0:n], srcv[:, q4 * 4:(q4 + 1) * 4, :]), writes=[xk], dma=xk)
                u32, u32k = u32_r.next()
                for kk in range(4):
                    k = q4 * 4 + kk
                    ph.add(ACT, e_act(u32[:, kk, 0:n], xs[:, kk, 0:n], AF.Identity, scale=C.sc1[:, col, 1, k:k + 1],
                                      bias=C.mod[:, col, 48 + k:49 + k]), reads=[xk, "g"], writes=[(u32k, kk)])
                    ph.add(POOL, e_copy(u16[:, k, 0:n], u32[:, kk, 0:n]), reads=[(u32k, kk)], writes=[("u16", k)])
                    for tt in range(ntt):
                        ph.add(PE, e_mm([(ps[tt][:, 0:72], u32[:, kk, tt * 128:(tt + 1) * 128], wr[:, k, :], k == 0, k == KC - 1)]),
                               reads=[(u32k, kk), "wr"], writes=[("ps", tt)], acc=True)
            for tt in range(ntt):
                A = lambda o, a, b, op: ph.add(DVE, e_tt(o, a, b, op), reads=["rt"], writes=["rt"])
                S = lambda o, a, s1, op0, s2=None, op1=None: ph.add(DVE, e_ts(o, a, s1, op0, s2, op1), reads=["rt"], writes=["rt"])
                ph.add(DVE, e_tt(lg[:], ps[tt][:, 0:72], br[:], ALU.add), reads=[("ps", tt), "br", "rt"], writes=["rt"])
                ph.add(DVE, lambda e: e.tensor_reduce(out=sc[:, 0:1], in_=lg[:, 0:8], axis=AX.X, op=ALU.max), reads=["rt"], writes=["rt"])
                S(goh[:], lg[:, 0:8], sc[:, 0:1], ALU.is_equal)
                S(sc[:, 1:2], sc[:, 0:1], -1.0, ALU.mult)
                ph.add(ACT, e_act(ex8[:], lg[:, 0:8], AF.Exp, bias=sc[:, 1:2]), reads=["rt"], writes=["rt"])
                ph.add(DVE, lambda e: e.tensor_reduce(out=sc[:, 2:3], in_=ex8[:], axis=AX.X, op=ALU.add), reads=["rt"], writes=["rt"])
                ph.add(DVE, lambda e: e.reciprocal(out=sc[:, 3:4], in_=sc[:, 2:3]), reads=["rt"], writes=["rt"])
                S(pen[:], goh[:], -1.0, ALU.add, BIG, ALU.mult)
                for g in range(8):
                    S(lm[:, g * 8:(g + 1) * 8], lg[:, 8 + g * 8:16 + g * 8], pen[:, g:g + 1], ALU.add)
                ph.add(DVE, lambda e: e.tensor_reduce(out=sc[:, 4:5], in_=lm[:], axis=AX.X, op=ALU.max), reads=["rt"], writes=["rt"])
                S(oh1[:], lm[:], sc[:, 4:5], ALU.is_equal)
                S(lm2[:], oh1[:], -BIG, ALU.mult)
                A(lm2[:], lm2[:], lm[:], ALU.add)
                ph.add(DVE, lambda e: e.tensor_reduce(out=sc[:, 5:6], in_=lm2[:], axis=AX.X, op=ALU.max), reads=["rt"], writes=["rt"])
                S(oh2[:], lm2[:], sc[:, 5:6], ALU.is_equal)
                A(sc[:, 6:7], sc[:, 5:6], sc[:, 4:5], ALU.subtract)
                ph.add(ACT, e_act(sc[:, 7:8], sc[:, 6:7], AF.Exp), reads=["rt"], writes=["rt"])
                S(sc[:, 8:9], sc[:, 7:8], 1.0, ALU.add)
                ph.add(DVE, lambda e: e.reciprocal(out=sc[:, 9:10], in_=sc[:, 8:9]), reads=["rt"], writes=["rt"])
                A(sc[:, 10:11], sc[:, 7:8], sc[:, 9:10], ALU.mult)
                A(sc[:, 11:12], sc[:, 9:10], sc[:, 3:4], ALU.mult)
                A(sc[:, 12:13], sc[:, 10:11], sc[:, 3:4], ALU.mult)
                S(wgt[:], oh1[:], sc[:, 11:12], ALU.mult)
                S(oh2[:], oh2[:], sc[:, 12:13], ALU.mult)
                A(wgt[:], wgt[:], oh2[:], ALU.add)
                ph.add(PE, lambda e: e.transpose(out=ps[4][0:64, 0:128], in_=wgt[:], identity=C.ident_f[:]),
                       reads=["rt", "g"], writes=[("ps", 4)])
                ph.add(DVE, e_copy(wgtT[:, tt * 128:(tt + 1) * 128], ps[4][0:64, 0:128]), reads=[("ps", 4), "rt"], writes=["wgtT", "rt"])
            u16keys = [("u16", k) for k in range(KC)]
            for e_i in range(n_exp):
                wg, wgk = w_r.next()
                ph.add(POOL, e_dma(wg[:].rearrange("p (k n) -> p k n", k=KC), W.w_eg[e_i].rearrange("(kc p) n -> p kc n", p=128)), writes=[wgk], dma=wgk)
                wu, wuk = w_r.next()
                ph.add(POOL, e_dma(wu[:].rearrange("p (k n) -> p k n", k=KC), W.w_eu[e_i].rearrange("(kc p) n -> p kc n", p=128)), writes=[wuk], dma=wuk)
                wd, wdk = w_r.next()
                ph.add(POOL, e_dma(wd[:].rearrange("p (k n) -> p k n", k=4), W.w_ed[e_i].rearrange("(kc p) n -> p kc n", p=128)), writes=[wdk], dma=wdk)
                wgv = wg[:].rearrange("p (k n) -> p k n", k=KC)
                wuv = wu[:].rearrange("p (k n) -> p k n", k=KC)
                wdv = wd[:].rearrange("p (k n) -> p k n", k=4)
                def emit_wm(ei):
                    wm, wmk = wm_r.next()
                    ph.add(DVE, e_ts(wm[:, 0:n], wgtT[:, 0:n], C.ident_f[0:64, ei:ei + 1], ALU.mult), reads=["wgtT", "g"], writes=[wmk])
                    return wm, wmk

                def emit_bc(ei, wm, wmk):
                    bb_ = 4 + (ei % 2)
                    ph.add(PE, e_mm([(ps[bb_][:, 0:n], C.ones_f[0:64, :], wm[:, 0:n], True, True)]), reads=[wmk, "g"], writes=[("ps", bb_)])

                if e_i == 0:
                    wm_c = emit_wm(0)
                    emit_bc(0, *wm_c)
                bb = 4 + (e_i % 2)
                wm_n = None
                H, Hk = H_r.next()
                for c in range(4):
                    if c == 2 and e_i + 1 < n_exp:
                        wm_n = emit_wm(e_i + 1)
                    bg = (gi[0] % 2) * 2
                    gi[0] += 1
                    cs = slice(c * 128, (c + 1) * 128)
                    ph.add(PE, e_mm([(ps[bg][:, 0:n], wgv[:, k, cs], u16[:, k, 0:n], k == 0, k == KC - 1) for k in range(KC)]),
                           reads=[wgk] + u16keys, writes=[("ps", bg)])
                    ph.add(PE, e_mm([(ps[bg + 1][:, 0:n], wuv[:, k, cs], u16[:, k, 0:n], k == 0, k == KC - 1) for k in range(KC)]),
                           reads=[wuk] + u16keys, writes=[("ps", bg + 1)])
                    sg, sgk = sg_r.next()
                    ph.add(ACT, e_act(sg[:, 0:n], ps[bg][:, 0:n], AF.Silu), reads=[("ps", bg)], writes=[sgk])
                    hh, hhk = hh_r.next()
                    ph.add(DVE, e_tt(hh[:, 0:n], ps[bg + 1][:, 0:n], sg[:, 0:n], ALU.mult), reads=[("ps", bg + 1), sgk], writes=[hhk])
                    ph.add(DVE, e_tt(H[:, c, 0:n], ps[bb][:, 0:n], hh[:, 0:n], ALU.mult), reads=[("ps", bb), hhk], writes=[(Hk, c)])
                if wm_n is not None:
                    emit_bc(e_i + 1, *wm_n)
                Hkeys = [(Hk, c) for c in range(4)]
                for m in range(KC):
                    by = 6 + (m % 2)
                    ph.add(PE, e_mm([(ps[by][:, 0:n], wdv[:, c, m * 128:(m + 1) * 128], H[:, c, 0:n], c == 0, c == 3) for c in range(4)]),
                           reads=[wdk] + Hkeys, writes=[("ps", by)])
                    if e_i == 0:
                        ph.add(DVE, e_copy(mx[:, m, 0:n], ps[by][:, 0:n]), reads=[("ps", by)], writes=[("mx", m)])
                    else:
                        ph.add(DVE, e_tt(mx[:, m, 0:n], ps[by][:, 0:n], mx[:, m, 0:n], ALU.add), reads=[("ps", by), ("mx", m)], writes=[("mx", m)])
            for m in range(KC):
                x, xk = x_r.next()
                ph.add(SP, e_dma(x[:, 0:n], C.X1[m * 128:(m + 1) * 128, qcol:qcol + n]), writes=[xk], dma=xk)
                xa, xak = xa_r.next()
                ph.add(ACT, e_act(xa[:, 0:n], x[:, 0:n], AF.Identity, scale=ALPHA), reads=[xk], writes=[xak])
                ph.add(DVE, lambda e, m=m, xa=xa, n=n, col=col: e.scalar_tensor_tensor(
                    out=mx[:, m, 0:n], in0=mx[:, m, 0:n], scalar=C.mod[:, col, 80 + m:81 + m], in1=xa[:, 0:n], op0=ALU.mult, op1=ALU.add),
                    reads=[("mx", m), xak, "g"], writes=[("mx", m)])
            for off in range(0, n, 256):
                ln_block(ph, C, rings, mx, "mx", 256, col, 2, dst[:, qcol + off:qcol + off + 256], "xo", off=off)
            nops += ph.finalize()
    return nops


def build_program(nlayers=2, debug=False, with_moe=True, stop_after=None, n_exp=NEXP):
    nc = bass.Bass("TRN2", target_bir_lowering=False)
    C = declare_io(nc, nlayers=nlayers, debug=debug, with_moe=with_moe)
    n = 0
    with ExitStack() as st:
        alloc_globals(C, st)
        for l in range(nlayers):
            W = C.L[l]
            src = C.xt if l == 0 else C.XO0
            lay = 0 if l < nlayers - 1 or nlayers == 1 else 1
            if nlayers == 1:
                lay = 0
            n += phase_ada(C, W)
            if stop_after == "ada":
                break
            n += phase_proj(C, W, proj_blocks(src, lay))
            if stop_after == "proj":
                break
            for k in "abc":
                n += phase_attn(C, W, k, q_blocks(lay))
            if stop_after == "att":
                break
            n += phase_merge(C, W, src, lay)
            if stop_after == "merge":
                break
            if with_moe:
                n += phase_moe(C, W, lay, C.XO0 if lay == 0 else C.xo, n_exp=n_exp)
    return nc, n


_PROG = {}


def kernel(**inputs):
    inp = {k: np.asarray(v) for k, v in inputs.items()}
    if "nc" not in _PROG:
        _PROG["nc"] = build_program(2)[0]
    nc = _PROG["nc"]
    shared = {}
    for l in range(2):
        shared.update(prep_layer_shared(inp, l))
    in_maps = [prep_core(inp, c, shared) for c in range(8)]
    res = run_bass_kernel_spmd(nc, in_maps, core_ids=list(range(8)))
    del in_maps
    x = np.asarray(inp["x"])
    out = np.empty(x.shape, np.float32)
    for c in range(8):
        xo = np.asarray(res.results[c]["xo"])
        b, half = c // 2, c % 2
        out[b, half * NOWN:(half + 1) * NOWN] = xo.T
    return out
```

```python
import math
from contextlib import ExitStack

import numpy as np
import concourse.bass as bass
import concourse.mybir as mybir
from concourse.bass_utils import run_bass_kernel_spmd

F32 = mybir.dt.float32
BF16 = mybir.dt.bfloat16
AF = mybir.ActivationFunctionType
ALU = mybir.AluOpType
AX = mybir.AxisListType

PE, ACT, DVE, POOL, SP = "tensor", "scalar", "vector", "gpsimd", "sync"
ENGS = [PE, ACT, DVE, POOL, SP]

D = 2048
KC = 16
NOWN = 2048
NCTX = 256
T = NOWN + NCTX
NNAT = 4096
NKEY = NNAT + NCTX
NKT = NKEY // 128
TL = NNAT + NCTX
EPS = 1e-6
ALPHA = 4 ** 0.25
NEXP = 64
DEXP = 512

SEG = {}
_o = 0
for _n, _w in (("aq", 768), ("ak", 256), ("av", 256), ("bq", 512), ("bkv", 256), ("bkr", 64),
               ("cq", 640), ("ck", 640), ("cv", 640), ("gate", 6144)):
    SEG[_n] = (_o, _w)
    _o += _w
IN_W = _o


class Op:
    __slots__ = ("eng", "emit", "deps", "idx", "sig_sem", "sig_val", "is_dma", "needs_sig", "acc")

    def __init__(self, eng, emit, is_dma):
        self.eng = eng
        self.emit = emit
        self.deps = set()
        self.is_dma = is_dma
        self.needs_sig = False
        self.sig_sem = None
        self.sig_val = 0
        self.acc = False


class Phase:
    def __init__(self, nc, sems, name="ph"):
        self.nc = nc
        self.name = name
        self.ops = []
        self.last_writer = {}
        self.readers = {}
        self.sem_pool = sems
        self.dma_sem_of = {}
        self.dma_cnt = {}
        self.n_dma = {"dsw": 0, "dhw": 0}

    def add(self, eng, emit, reads=(), writes=(), dma=None, acc=False):
        op = Op(eng, emit, dma is not None)
        op.idx = len(self.ops)
        op.acc = acc
        deps = op.deps
        for b in reads:
            w = self.last_writer.get(b)
            if w is not None:
                deps.add(w)
        for b in writes:
            w = self.last_writer.get(b)
            if w is not None:
                wop = self.ops[w]
                if not (acc and wop.acc and wop.eng == eng):
                    deps.add(w)
            for r in self.readers.get(b, ()):
                deps.add(r)
        for b in reads:
            self.readers.setdefault(b, []).append(op.idx)
        for b in writes:
            self.last_writer[b] = op.idx
            self.readers[b] = []
        if dma is not None:
            kind = "dsw" if eng == POOL else "dhw"
            dma = (kind, dma)
            if dma not in self.dma_sem_of:
                self.dma_sem_of[dma] = self.n_dma[kind]
                self.n_dma[kind] += 1
                self.dma_cnt[dma] = self.sem_pool["dsw_cnt"][self.dma_sem_of[dma]] if kind == "dsw" else 0
            self.dma_cnt[dma] += 16
            if kind == "dsw":
                self.sem_pool["dsw_cnt"][self.dma_sem_of[dma]] = self.dma_cnt[dma]
            op.sig_sem = (kind, self.dma_sem_of[dma])
            op.sig_val = self.dma_cnt[dma]
            op.needs_sig = True
        self.ops.append(op)
        return op.idx

    def finalize(self):
        nc = self.nc
        ops = self.ops
        for op in ops:
            for d in op.deps:
                ops[d].needs_sig = True
        cnt = {e: 0 for e in ENGS}
        for op in ops:
            if not op.is_dma and op.needs_sig:
                cnt[op.eng] += 1
                op.sig_sem = ("eng", op.eng)
                op.sig_val = cnt[op.eng]
        for kind in ("dsw", "dhw"):
            assert self.n_dma[kind] <= len(self.sem_pool[kind]), f"{self.name}: need {self.n_dma[kind]} {kind} sems"

        def semh(key):
            return self.sem_pool[key[0]][key[1]]

        waited = {e: {} for e in ENGS}
        plan = {e: [] for e in ENGS}
        for op in ops:
            need = {}
            for d in op.deps:
                p = ops[d]
                k = p.sig_sem
                if need.get(k, 0) < p.sig_val:
                    need[k] = p.sig_val
            waits = []
            for k, v in need.items():
                if waited[op.eng].get(k, 0) < v:
                    waited[op.eng][k] = v
                    waits.append((k, v))
            plan[op.eng].append((op, waits))
        final_waits = {e: {} for e in ENGS}
        for op in ops:
            if op.is_dma:
                final_waits[op.eng][op.sig_sem] = max(final_waits[op.eng].get(op.sig_sem, 0), op.sig_val)

        with nc.Block() as block:
            for e in ENGS:
                if not plan[e] and not final_waits[e]:
                    continue

                def body(eng, e=e):
                    for op, waits in plan[e]:
                        for k, v in waits:
                            eng.wait_ge(semh(k), v)
                        ins = op.emit(eng)
                        if op.needs_sig:
                            ins.then_inc(semh(op.sig_sem), 16 if op.is_dma else 1)
                    for k, v in final_waits[e].items():
                        eng.wait_ge(semh(k), v)

                getattr(block, e)(body)
        with nc.Block() as block2:
            def clr(eng):
                for e in ENGS:
                    eng.sem_clear(self.sem_pool["eng"][e])
                for i in range(self.n_dma["dhw"]):
                    eng.sem_clear(self.sem_pool["dhw"][i])
            block2.gpsimd(clr)
        n = len(ops)
        self.ops = []
        return n


def make_sems(nc, stack, nhw=40, nsw=12):
    pool = {"eng": {}, "dhw": [], "dsw": [], "dsw_cnt": [0] * nsw}
    for e in ENGS:
        pool["eng"][e] = stack.enter_context(nc.semaphore("s_" + e))
    for i in range(nhw):
        pool["dhw"].append(stack.enter_context(nc.semaphore(f"s_dhw{i}")))
    for i in range(nsw):
        pool["dsw"].append(stack.enter_context(nc.semaphore(f"s_dsw{i}")))
    return pool


def e_dma(out, in_):
    return lambda e: e.dma_start(out=out, in_=in_)


def e_mm(items):
    def emit(e):
        ins = None
        for (o, l, r, s, t) in items:
            ins = e.matmul(o, l, r, start=s, stop=t)
        return ins
    return emit


def e_act(out, in_, func, scale=None, bias=None):
    kw = {}
    if scale is not None:
        kw["scale"] = scale
    if bias is not None:
        kw["bias"] = bias
    return lambda e: e.activation(out=out, in_=in_, func=func, **kw)


def e_tt(out, a, b, op):
    return lambda e: e.tensor_tensor(out=out, in0=a, in1=b, op=op)


def e_ts(out, a, s1, op0, s2=None, op1=None):
    if op1 is None:
        return lambda e: e.tensor_scalar(out=out, in0=a, scalar1=s1, scalar2=None, op0=op0)
    return lambda e: e.tensor_scalar(out=out, in0=a, scalar1=s1, scalar2=s2, op0=op0, op1=op1)


def e_copy(out, in_):
    return lambda e: e.tensor_copy(out=out, in_=in_)


_UNIQ = [0]


def uname(name):
    _UNIQ[0] += 1
    return f"{name}_{_UNIQ[0]}"


class Ring:
    def __init__(self, nc, stack, name, shape, dtype, n):
        name = uname(name)
        self.bufs = [stack.enter_context(nc.sbuf_tensor(f"{name}_{i}", shape, dtype)) for i in range(n)]
        self.name = name
        self.i = 0

    def next(self):
        b = self.bufs[self.i % len(self.bufs)]
        k = (self.name, self.i % len(self.bufs))
        self.i += 1
        return b, k


class Ctx:
    pass


TABW = TL


def declare_io(nc, nlayers=2, debug=False, with_moe=True):
    C = Ctx()
    C.nc = nc

    def inp(name, shape, dt=F32):
        return nc.dram_tensor(name, list(shape), dt, kind="ExternalInput").ap()

    def scr(name, shape, dt=BF16, out=False):
        kind = "ExternalOutput" if (out or debug) else "Internal"
        return nc.dram_tensor(name, list(shape), dt, kind=kind).ap()

    C.xt = inp("xt", [D, TL])
    C.cond = inp("cond", [128, 32])
    C.tab128 = inp("tab128", [2, 128, TABW])
    C.tab64 = inp("tab64", [2, 64, TABW])
    C.rt128 = inp("rt128", [128, 128])
    C.rt64 = inp("rt64", [64, 64])
    C.ident = inp("ident", [128, 128])
    C.L = []
    for l in range(nlayers):
        W = Ctx()
        sfx = str(l)
        W.w_ada = inp("w_ada" + sfx, [D, 6 * D])
        W.b_ada = inp("b_ada" + sfx, [128, 96])
        W.w_in = inp("w_in" + sfx, [D, IN_W])
        W.gq = inp("gq" + sfx, [128, 1])
        W.gk = inp("gk" + sfx, [128, 1])
        W.mlaq_g = inp("mlaq_g" + sfx, [128, 4])
        W.mlakv_g = inp("mlakv_g" + sfx, [128, 2])
        W.w_uq = inp("w_uq" + sfx, [512, 960])
        W.w_ukv = inp("w_ukv" + sfx, [256, 1280])
        W.nab = inp("nab" + sfx, [5, 8 if l == 0 else 4, 8, 128, 512])
        W.w_ba = inp("w_ba" + sfx, [768, D])
        W.w_bb = inp("w_bb" + sfx, [640, D])
        W.w_bc = inp("w_bc" + sfx, [640, D])
        W.w_out = inp("w_out" + sfx, [D, D])
        W.lnp = inp("lnp" + sfx, [128, 4, 16])
        W.w_r = inp("w_r" + sfx, [D, 72])
        W.b_r = inp("b_r" + sfx, [128, 72])
        if with_moe:
            W.w_eg = inp("w_eg" + sfx, [NEXP, D, DEXP])
            W.w_eu = inp("w_eu" + sfx, [NEXP, D, DEXP])
            W.w_ed = inp("w_ed" + sfx, [NEXP, DEXP, D])
        C.L.append(W)
    C.QA = scr("QA", [6, 128, TL])
    C.KA = scr("KA", [2, 128, NKEY])
    C.VA = scr("VA", [NKEY, 256])
    C.QBN = scr("QBN", [5, 128, TL])
    C.QBR = scr("QBR", [5, 64, TL])
    C.KBN = scr("KBN", [5, 128, NKEY])
    C.KBR = scr("KBR", [64, NKEY])
    C.VB = scr("VB", [NKEY, 640])
    C.QC = scr("QC", [5, 128, TL])
    C.KCs = scr("KCs", [5, 128, NKEY])
    C.VC = scr("VC", [NKEY, 640])
    C.G = scr("G", [48, 128, TL])
    C.OA = scr("OA", [6, 128, TL])
    C.OB = scr("OB", [5, 128, TL])
    C.OC = scr("OC", [5, 128, TL])
    C.X1 = scr("X1", [D, TL], F32)
    C.XO0 = scr("XO0", [D, TL], F32)
    C.MODD = scr("MODD", [128, 192], F32)
    C.xo = scr("xo", [D, NOWN], F32, out=True)
    return C


def alloc_globals(C, st):
    nc = C.nc
    sb = lambda name, shape, dt: st.enter_context(nc.sbuf_tensor(name, shape, dt))
    C.sems = make_sems(nc, st)
    C.ps = [st.enter_context(nc.psum_tensor(f"psb{i}", [128, 512], F32)) for i in range(8)]
    C.ones_bf = sb("ones_bf", [128, 128], BF16)
    C.ones_f = sb("ones_f", [128, 128], F32)
    C.ident_f = sb("ident_f", [128, 128], F32)
    C.ident_bf = sb("ident_bf", [128, 128], BF16)
    C.rtq = sb("rtq", [128, 128], BF16)
    C.rtk = sb("rtk", [128, 128], BF16)
    C.rt64b = sb("rt64b", [64, 64], BF16)
    C.mod = sb("mod", [128, 2, 96], F32)
    C.sc1 = sb("sc1", [128, 2, 2, 16], F32)
    C.gqs = sb("gqs", [128, 1], F32)
    C.gks = sb("gks", [128, 1], F32)
    C.mlaqg = sb("mlaqg", [128, 4], F32)
    C.mlakvg = sb("mlakvg", [128, 2], F32)
    C.lnps = sb("lnps", [128, 4, 16], F32)
    C.wuq = sb("wuq", [128, 4, 960], BF16)
    C.wukv = sb("wukv", [128, 2, 1280], BF16)
    C.wukv_v = sb("wukv_v", [128, 2, 640], BF16)


def phase_ada(C, W):
    nc = C.nc
    ph = Phase(nc, C.sems, "ada")
    with ExitStack() as st:
        sb = lambda name, shape, dt: st.enter_context(nc.sbuf_tensor(uname(name), shape, dt))
        cond = sb("a_cond", [128, 32], F32)
        silu = sb("a_silu", [128, 32], F32)
        bada = sb("a_bada", [128, 96], F32)
        rt_f = sb("a_rtf", [128, 128], F32)
        rt64_f = sb("a_rt64f", [64, 64], F32)
        wring = Ring(nc, st, "a_w", [128, KC, 384], F32, 3)
        ph.add(SP, e_dma(cond[:], C.cond), writes=["cond"], dma="cond")
        ph.add(SP, e_dma(bada[:], W.b_ada), writes=["bada"], dma="bada")
        ph.add(SP, e_dma(C.ident_f[:], C.ident), writes=["ident_f"], dma="ident")
        ph.add(SP, e_dma(rt_f[:], C.rt128), writes=["rt_f"], dma="rt_f")
        ph.add(SP, e_dma(rt64_f[:], C.rt64), writes=["rt64_f"], dma="rt64_f")
        ph.add(SP, e_dma(C.gqs[:], W.gq), writes=["gqs"], dma="gqs")
        ph.add(SP, e_dma(C.gks[:], W.gk), writes=["gks"], dma="gks")
        ph.add(SP, e_dma(C.mlaqg[:], W.mlaq_g), writes=["mlaqg"], dma="mlaqg")
        ph.add(SP, e_dma(C.mlakvg[:], W.mlakv_g), writes=["mlakvg"], dma="mlakvg")
        ph.add(SP, e_dma(C.lnps[:], W.lnp), writes=["lnps"], dma="lnps")
        ph.add(POOL, e_dma(C.wuq[:], W.w_uq.rearrange("(c p) n -> p c n", p=128)), writes=["wuq"], dma="wuq")
        ph.add(POOL, e_dma(C.wukv[:], W.w_ukv.rearrange("(c p) n -> p c n", p=128)), writes=["wukv"], dma="wukv")
        ph.add(POOL, lambda e: e.memset(C.ones_bf[:], 1.0), writes=["ones_bf"])
        ph.add(POOL, lambda e: e.memset(C.ones_f[:], 1.0), writes=["ones_f"])
        ph.add(DVE, e_copy(C.ident_bf[:], C.ident_f[:]), reads=["ident_f"], writes=["ident_bf"])
        ph.add(DVE, e_ts(C.rtq[:], rt_f[:], C.gqs[:, 0:1], ALU.mult), reads=["rt_f", "gqs"], writes=["rtq"])
        ph.add(DVE, e_ts(C.rtk[:], rt_f[:], C.gks[:, 0:1], ALU.mult), reads=["rt_f", "gks"], writes=["rtk"])
        ph.add(DVE, e_copy(C.rt64b[:], rt64_f[:]), reads=["rt64_f"], writes=["rt64b"])
        for c in range(4):
            ph.add(DVE, e_ts(C.wuq[:, c, :], C.wuq[:, c, :], C.mlaqg[:, c:c + 1], ALU.mult, math.sqrt(512.0), ALU.mult),
                   reads=["wuq", "mlaqg"], writes=["wuq"])
        for c in range(2):
            ph.add(DVE, e_ts(C.wukv[:, c, :], C.wukv[:, c, :], C.mlakvg[:, c:c + 1], ALU.mult, math.sqrt(256.0), ALU.mult),
                   reads=["wukv", "mlakvg"], writes=["wukv"])
        for c in range(2):
            src = C.wukv[:, c, :].rearrange("p (h x) -> p h x", x=256)[:, :, 128:256]
            dst = C.wukv_v[:, c, :].rearrange("p (h x) -> p h x", x=128)
            ph.add(DVE, e_copy(dst, src), reads=["wukv"], writes=["wukv_v"])
        ph.add(ACT, e_act(silu[:], cond[:], AF.Silu), reads=["cond"], writes=["silu"])
        acc = C.ps[0]
        wv = W.w_ada.rearrange("(kc p) n -> p kc n", p=128)
        for s in range(32):
            w, wk = wring.next()
            ph.add(SP, e_dma(w[:], wv[:, :, s * 384:(s + 1) * 384]), writes=[wk], dma=wk)
            for jj in range(3):
                j = s * 3 + jj
                ph.add(PE, e_mm([(acc[:, j * 2:(j + 1) * 2], w[:, k, jj * 128:(jj + 1) * 128], silu[:, k * 2:(k + 1) * 2],
                                  k == 0, k == KC - 1) for k in range(KC)]),
                       reads=[wk, "silu"], writes=["acc"], acc=True)
        accv = acc[:, 0:192].rearrange("p (j c) -> p j c", c=2)
        for col in range(2):
            ph.add(DVE, e_tt(C.mod[:, col, :], accv[:, :, col], bada[:], ALU.add), reads=["acc", "bada"], writes=["mod"])
        for col in range(2):
            for mf in range(2):
                seg = 1 if mf == 0 else 4
                ph.add(DVE, e_ts(C.sc1[:, col, mf, :], C.mod[:, col, seg * 16:(seg + 1) * 16], 1.0, ALU.add),
                       reads=["mod"], writes=["sc1"])
        ph.add(SP, e_dma(C.MODD, C.mod[:].rearrange("p c j -> p (c j)")), reads=["mod"], dma="modd")
        n = ph.finalize()
    return n


SCALE_A = 1.0 / math.sqrt(128.0)
SCALE_B = 1.0 / math.sqrt(192.0)
SCALE_C = 1.0 / math.sqrt(128.0)


def phase_proj(C, W, blocks):
    nc = C.nc
    ph = Phase(nc, C.sems, "proj")
    ps = C.ps
    with ExitStack() as st:
        sb = lambda name, shape, dt: st.enter_context(nc.sbuf_tensor(uname(name), shape, dt))
        xs_r = Ring(nc, st, "p_xs", [128, 4, 512], F32, 2)
        u_r = Ring(nc, st, "p_u", [128, KC, 512], BF16, 2)
        SW = 1024
        w_r = Ring(nc, st, "p_w", [128, KC, SW], BF16, 2)
        tb128 = sb("p_tb128", [128, 2, 512], F32)
        tb64 = sb("p_tb64", [64, 2, 512], F32)
        cosq = sb("p_cosq", [128, 512], F32)
        cosk = sb("p_cosk", [128, 512], F32)
        sink = sb("p_sink", [128, 512], F32)
        c64q = sb("p_c64q", [64, 512], F32)
        s64q = sb("p_s64q", [64, 512], F32)
        sq_r = Ring(nc, st, "p_sq", [128, 512], BF16, 2)
        ln_r = Ring(nc, st, "p_ln", [128, 512], F32, 2)
        rs_r = Ring(nc, st, "p_rs", [128, 512], F32, 2)
        qn_r = Ring(nc, st, "p_qn", [128, 512], BF16, 2)
        t1_r = Ring(nc, st, "p_t1", [128, 512], F32, 2)
        t2_r = Ring(nc, st, "p_t2", [128, 512], F32, 2)
        stg_r = Ring(nc, st, "p_stg", [128, 512], BF16, 4)
        vst_r = Ring(nc, st, "p_vst", [128, 640], BF16, 2)
        latq = sb("p_latq", [128, 4, 512], BF16)
        latkv = sb("p_latkv", [128, 2, 512], BF16)
        main_i = [0]
        aux_i = [0]

        def mainbank():
            b = main_i[0] % 4
            main_i[0] += 1
            return b

        def auxpair():
            a = 4 + 2 * (aux_i[0] % 2)
            aux_i[0] += 1
            return a, a + 1

        wv = W.w_in.rearrange("(kc p) n -> p kc n", p=128)

        def load_w(c0, ncols):
            w, wk = w_r.next()
            ph.add(POOL, e_dma(w[:, :, 0:ncols], wv[:, :, c0:c0 + ncols]), writes=[wk], dma=wk)
            return w, wk

        def store(dst, src, srckey):
            ph.add(SP, e_dma(dst, src), reads=[srckey], dma=srckey)

        for blk in blocks:
            n = blk["ntok"]
            mc = blk["modcol"]
            tc0 = blk["tabcol"]
            qcol, kd, kn = blk["qcol"], blk["kd"], blk["kn"]
            u, uk = u_r.next()
            srcv = blk["src"].rearrange("(kc p) t -> p kc t", p=128)
            for q4 in range(4):
                xs, xk = xs_r.next()
                ph.add(SP, e_dma(xs[:, :, 0:n], srcv[:, q4 * 4:(q4 + 1) * 4, :]), writes=[xk], dma=xk)
                for kk in range(4):
                    k = q4 * 4 + kk
                    ph.add(ACT, e_act(u[:, k, 0:n], xs[:, kk, 0:n], AF.Identity, scale=C.sc1[:, mc, 0, k:k + 1],
                                      bias=C.mod[:, mc, k:k + 1]), reads=[xk, "g"], writes=[(uk, k)])
            ukeys = [(uk, k) for k in range(KC)]
            ph.add(SP, e_dma(tb128[:, :, 0:n], C.tab128[:, :, tc0:tc0 + n].rearrange("a p t -> p a t")),
                   writes=["tb128"], dma="tb128")
            ph.add(SP, e_dma(tb64[:, :, 0:n], C.tab64[:, :, tc0:tc0 + n].rearrange("a p t -> p a t")),
                   writes=["tb64"], dma="tb64")
            if qcol is not None:
                ph.add(DVE, e_ts(cosq[:, 0:n], tb128[:, 0, 0:n], C.gqs[:, 0:1], ALU.mult), reads=["tb128"], writes=["cosq"])
                ph.add(DVE, e_ts(c64q[:, 0:n], tb64[:, 0, 0:n], SCALE_B, ALU.mult), reads=["tb64"], writes=["c64q"])
                ph.add(DVE, e_ts(s64q[:, 0:n], tb64[:, 1, 0:n], SCALE_B, ALU.mult), reads=["tb64"], writes=["s64q"])
            if kd is not None:
                ph.add(DVE, e_ts(cosk[:, 0:n], tb128[:, 0, 0:n], C.gks[:, 0:1], ALU.mult, math.sqrt(128.0), ALU.mult),
                       reads=["tb128"], writes=["cosk"])
                ph.add(DVE, e_ts(sink[:, 0:n], tb128[:, 1, 0:n], math.sqrt(128.0), ALU.mult), reads=["tb128"], writes=["sink"])

            def gemm_chunk(w, wk, cc, m=128):
                b = mainbank()
                ph.add(PE, e_mm([(ps[b][0:m, 0:n], w[:, k, cc:cc + m], u[:, k, 0:n], k == 0, k == KC - 1) for k in range(KC)]),
                       reads=[wk] + ukeys, writes=[("ps", b)])
                return b

            def rope128(b, rt, rtkey, cos_t, coskey, sin_t, sinkey, dst):
                a0, a1 = auxpair()
                sq, sqk = sq_r.next()
                ph.add(ACT, e_act(sq[:, 0:n], ps[b][:, 0:n], AF.Square), reads=[("ps", b)], writes=[sqk])
                ph.add(PE, e_mm([(ps[a0][:, 0:n], C.ones_bf[:], sq[:, 0:n], True, True)]), reads=[sqk, "g"], writes=[("ps", a0)])
                ln, lnk = ln_r.next()
                ph.add(ACT, e_act(ln[:, 0:n], ps[a0][:, 0:n], AF.Ln, bias=128.0 * EPS), reads=[("ps", a0)], writes=[lnk])
                rs, rsk = rs_r.next()
                ph.add(ACT, e_act(rs[:, 0:n], ln[:, 0:n], AF.Exp, scale=-0.5), reads=[lnk], writes=[rsk])
                qn, qnk = qn_r.next()
                ph.add(DVE, e_tt(qn[:, 0:n], ps[b][:, 0:n], rs[:, 0:n], ALU.mult), reads=[("ps", b), rsk], writes=[qnk])
                ph.add(PE, e_mm([(ps[a1][:, 0:n], rt[:], qn[:, 0:n], True, True)]), reads=[qnk, "g"], writes=[("ps", a1)])
                t1, t1k = t1_r.next()
                ph.add(DVE, e_tt(t1[:, 0:n], qn[:, 0:n], cos_t[:, 0:n], ALU.mult), reads=[qnk, coskey], writes=[t1k])
                t2, t2k = t2_r.next()
                ph.add(DVE, e_tt(t2[:, 0:n], ps[a1][:, 0:n], sin_t[:, 0:n], ALU.mult), reads=[("ps", a1), sinkey], writes=[t2k])
                sg, sgk = stg_r.next()
                ph.add(DVE, e_tt(sg[:, 0:n], t1[:, 0:n], t2[:, 0:n], ALU.add), reads=[t1k, t2k], writes=[sgk])
                store(dst, sg[:, 0:n], sgk)

            def rope64(b, cos_t, coskey, sin_t, sinkey, dst):
                a0, a1 = auxpair()
                qn, qnk = qn_r.next()
                ph.add(DVE, e_copy(qn[0:64, 0:n], ps[b][0:64, 0:n]), reads=[("ps", b)], writes=[qnk])
                ph.add(PE, e_mm([(ps[a1][0:64, 0:n], C.rt64b[:], qn[0:64, 0:n], True, True)]), reads=[qnk, "g"], writes=[("ps", a1)])
                t1, t1k = t1_r.next()
                ph.add(DVE, e_tt(t1[0:64, 0:n], qn[0:64, 0:n], cos_t[:, 0:n], ALU.mult), reads=[qnk, coskey], writes=[t1k])
                t2, t2k = t2_r.next()
                ph.add(DVE, e_tt(t2[0:64, 0:n], ps[a1][0:64, 0:n], sin_t[:, 0:n], ALU.mult), reads=[("ps", a1), sinkey], writes=[t2k])
                sg, sgk = stg_r.next()
                ph.add(DVE, e_tt(sg[0:64, 0:n], t1[0:64, 0:n], t2[0:64, 0:n], ALU.add), reads=[t1k, t2k], writes=[sgk])
                store(dst, sg[0:64, 0:n], sgk)

            def copy_out(b, dst, scale=1.0, func=AF.Copy, m=128):
                sg, sgk = stg_r.next()
                ph.add(ACT, e_act(sg[0:m, 0:n], ps[b][0:m, 0:n], func, scale=scale), reads=[("ps", b)], writes=[sgk])
                store(dst, sg[0:m, 0:n], sgk)

            def latent(seg, nch, lat, latkey):
                c0, wd = SEG[seg]
                w, wk = load_w(c0, wd)
                banks = [gemm_chunk(w, wk, c * 128) for c in range(nch)]
                a0, a1 = auxpair()
                sqs = []
                for c in range(nch):
                    sq, sqk = sq_r.next()
                    ph.add(ACT, e_act(sq[:, 0:n], ps[banks[c]][:, 0:n], AF.Square), reads=[("ps", banks[c])], writes=[sqk])
                    ph.add(PE, e_mm([(ps[a0][:, 0:n], C.ones_bf[:], sq[:, 0:n], c == 0, c == nch - 1)]),
                           reads=[sqk, "g"], writes=[("ps", a0)], acc=True)
                ln, lnk = ln_r.next()
                ph.add(ACT, e_act(ln[:, 0:n], ps[a0][:, 0:n], AF.Ln, bias=128.0 * nch * EPS), reads=[("ps", a0)], writes=[lnk])
                rs, rsk = rs_r.next()
                ph.add(ACT, e_act(rs[:, 0:n], ln[:, 0:n], AF.Exp, scale=-0.5), reads=[lnk], writes=[rsk])
                for c in range(nch):
                    ph.add(DVE, e_tt(lat[:, c, 0:n], ps[banks[c]][:, 0:n], rs[:, 0:n], ALU.mult),
                           reads=[("ps", banks[c]), rsk], writes=[(latkey, c)])

            def tokmajor(seg, dstv, key0):
                c0, wd = SEG[seg]
                w, wk = load_w(c0, wd)
                for s0 in range(0, wd, 512):
                    ncols = min(512, wd - s0)
                    for tt in range(n // 128):
                        b = mainbank()
                        ph.add(PE, e_mm([(ps[b][:, 0:ncols], u[:, k, tt * 128:(tt + 1) * 128], w[:, k, s0:s0 + ncols], k == 0, k == KC - 1)
                                         for k in range(KC)]), reads=[wk] + ukeys, writes=[("ps", b)])
                        v, vk = vst_r.next()
                        ph.add(DVE if tt % 2 == 0 else ACT,
                               e_copy(v[:, 0:ncols], ps[b][:, 0:ncols]) if tt % 2 == 0 else e_act(v[:, 0:ncols], ps[b][:, 0:ncols], AF.Copy),
                               reads=[("ps", b)], writes=[vk])
                        store(dstv[key0 + tt * 128:key0 + (tt + 1) * 128, s0:s0 + ncols], v[:, 0:ncols], vk)

            if qcol is not None:
                c0, wd = SEG["aq"]
                for s0 in range(0, wd, SW):
                    ncols = min(SW, wd - s0)
                    w, wk = load_w(c0 + s0, ncols)
                    for cc in range(0, ncols, 128):
                        h = (s0 + cc) // 128
                        b = gemm_chunk(w, wk, cc)
                        rope128(b, C.rtq, "g", cosq, "cosq", tb128[:, 1, :], "tb128", C.QA[h, :, qcol:qcol + n])
                latent("bq", 4, latq, "latq")
                for h in range(5):
                    b = mainbank()
                    ph.add(PE, e_mm([(ps[b][:, 0:n], C.wuq[:, c, h * 192:h * 192 + 128], latq[:, c, 0:n], c == 0, c == 3) for c in range(4)]),
                           reads=[("latq", c) for c in range(4)] + ["g"], writes=[("ps", b)])
                    copy_out(b, C.QBN[h, :, qcol:qcol + n], scale=SCALE_B)
                    b = mainbank()
                    ph.add(PE, e_mm([(ps[b][0:64, 0:n], C.wuq[:, c, h * 192 + 128:h * 192 + 192], latq[:, c, 0:n], c == 0, c == 3) for c in range(4)]),
                           reads=[("latq", c) for c in range(4)] + ["g"], writes=[("ps", b)])
                    rope64(b, c64q, "c64q", s64q, "s64q", C.QBR[h, :, qcol:qcol + n])
                c0, wd = SEG["cq"]
                for s0 in range(0, wd, SW):
                    ncols = min(SW, wd - s0)
                    w, wk = load_w(c0 + s0, ncols)
                    for cc in range(0, ncols, 128):
                        h = (s0 + cc) // 128
                        b = gemm_chunk(w, wk, cc)
                        copy_out(b, C.QC[h, :, qcol:qcol + n], scale=SCALE_C)
                c0, wd = SEG["gate"]
                for s0 in range(0, wd, SW):
                    w, wk = load_w(c0 + s0, SW)
                    for cc in range(0, SW, 128):
                        j = (s0 + cc) // 128
                        b = gemm_chunk(w, wk, cc)
                        copy_out(b, C.G[j, :, qcol:qcol + n], func=AF.Sigmoid)
            if kd is not None:
                c0, wd = SEG["ak"]
                w, wk = load_w(c0, wd)
                for h in range(2):
                    b = gemm_chunk(w, wk, h * 128)
                    rope128(b, C.rtk, "g", cosk, "cosk", sink, "sink", C.KA[h, :, kd:kd + n])
                tokmajor("av", C.VA, kd)
                latent("bkv", 2, latkv, "latkv")
                for h in range(5):
                    b = mainbank()
                    ph.add(PE, e_mm([(ps[b][:, 0:n], C.wukv[:, c, h * 256:h * 256 + 128], latkv[:, c, 0:n], c == 0, c == 1) for c in range(2)]),
                           reads=[("latkv", c) for c in range(2)] + ["g"], writes=[("ps", b)])
                    copy_out(b, C.KBN[h, :, kd:kd + n])
                for tt in range(n // 128):
                    b0 = mainbank()
                    ph.add(PE, e_mm([(ps[b0][:, 0:512], latkv[:, c, tt * 128:(tt + 1) * 128], C.wukv_v[:, c, 0:512], c == 0, c == 1) for c in range(2)]),
                           reads=[("latkv", c) for c in range(2)] + ["g"], writes=[("ps", b0)])
                    b1 = mainbank()
                    ph.add(PE, e_mm([(ps[b1][:, 0:128], latkv[:, c, tt * 128:(tt + 1) * 128], C.wukv_v[:, c, 512:640], c == 0, c == 1) for c in range(2)]),
                           reads=[("latkv", c) for c in range(2)] + ["g"], writes=[("ps", b1)])
                    v, vk = vst_r.next()
                    ph.add(DVE, e_copy(v[:, 0:512], ps[b0][:, 0:512]), reads=[("ps", b0)], writes=[vk])
                    ph.add(ACT, e_act(v[:, 512:640], ps[b1][:, 0:128], AF.Copy), reads=[("ps", b1)], writes=[vk])
                    store(C.VB[kd + tt * 128:kd + (tt + 1) * 128, :], v[:, :], vk)
                c0, wd = SEG["bkr"]
                w, wk = load_w(c0, wd)
                b = gemm_chunk(w, wk, 0, m=64)
                rope64(b, tb64[:, 0, :], "tb64", tb64[:, 1, :], "tb64", C.KBR[:, kd:kd + n])
            if kn is not None:
                c0, wd = SEG["ck"]
                for s0 in range(0, wd, SW):
                    ncols = min(SW, wd - s0)
                    w, wk = load_w(c0 + s0, ncols)
                    for cc in range(0, ncols, 128):
                        h = (s0 + cc) // 128
                        b = gemm_chunk(w, wk, cc)
                        copy_out(b, C.KCs[h, :, kn:kn + n])
                tokmajor("cv", C.VC, kn)
        nops = ph.finalize()
    return nops


def _rope_table(pos, dim):
    pos = np.asarray(pos, dtype=np.int64)
    row = (pos // 64).astype(np.float32)
    col = (pos % 64).astype(np.float32)
    quarter = dim // 4
    inv_freq = (np.float32(10000.0) ** (-np.arange(quarter, dtype=np.float32) / np.float32(quarter))).astype(np.float32)
    ang_r = row[:, None] * inv_freq
    ang_c = col[:, None] * inv_freq
    ang = np.concatenate([ang_r, ang_r, ang_c, ang_c], axis=-1).astype(np.float32)
    return np.cos(ang).T.astype(np.float32), np.sin(ang).T.astype(np.float32)


def _rot_T(dim):
    q = dim // 4
    RT = np.zeros((dim, dim), np.float32)
    for m in range(dim):
        b = (m % (2 * q)) // q
        if b == 0:
            RT[m + q, m] = -1.0
        else:
            RT[m - q, m] = 1.0
    return RT


def _local_pos(half):
    own = np.arange(half * NOWN, (half + 1) * NOWN)
    oth = np.arange((1 - half) * NOWN, (2 - half) * NOWN)
    return np.concatenate([own, oth])


def _tables(half):
    pos = _local_pos(half)
    t128 = np.zeros((2, 128, TABW), np.float32)
    t64 = np.zeros((2, 64, TABW), np.float32)
    for dim, t in ((128, t128), (64, t64)):
        c, s = _rope_table(pos, dim)
        t[0, :, 0:NNAT] = c
        t[1, :, 0:NNAT] = s
        t[0, :, NNAT:] = 1.0
    return t128, t64


def _na_bias_index(half):
    ri = np.zeros((8, 8, 128, 512), np.int64)
    ci = np.zeros((8, 8, 128, 512), np.int64)
    valid = np.zeros((8, 8, 128, 512), bool)
    for jl in range(8):
        s_, j = jl // 4, jl % 4
        qhalf = half if s_ == 0 else 1 - half
        q_rows = qhalf * 32 + 8 * j + np.arange(8)
        qr = np.repeat(q_rows, 64)
        qc = np.tile(np.arange(64), 8)
        r_start = np.clip(qr - 4, 0, 64 - 8)
        c_start = np.clip(qc - 8, 0, 64 - 16)
        for i, t in enumerate(na_tiles(jl)):
            thalf = half if t < 16 else 1 - half
            rows = thalf * 32 + 2 * (t % 16) + np.arange(2)
            kr = np.repeat(rows, 64)
            kc = np.tile(np.arange(64), 2)
            v = ((kr[:, None] >= r_start[None, :]) & (kr[:, None] < r_start[None, :] + 8) &
                 (kc[:, None] >= c_start[None, :]) & (kc[:, None] < c_start[None, :] + 16))
            ri[jl, i] = np.clip(kr[:, None] - qr[None, :] + 7, 0, 14)
            ci[jl, i] = np.clip(kc[:, None] - qc[None, :] + 15, 0, 30)
            valid[jl, i] = v
    return ri, ci, valid


_CONST_CACHE = {}


def _consts(half):
    if half not in _CONST_CACHE:
        t128, t64 = _tables(half)
        _CONST_CACHE[half] = dict(tab128=t128, tab64=t64, nabidx=_na_bias_index(half))
    return _CONST_CACHE[half]


def _fm(v, nch):
    return np.ascontiguousarray(np.asarray(v, np.float32).reshape(nch, 128).T)


def prep_layer_shared(inp, l, with_moe=True):
    sh = {}
    sh["w_ada"] = np.ascontiguousarray(inp["w_ada"][l])
    sh["b_ada"] = _fm(inp["b_ada"][l], 96)
    sh["w_in"] = np.ascontiguousarray(inp["w_in"][l])
    sh["gq"] = np.ascontiguousarray(inp["gqa_q_norm"][l].reshape(128, 1))
    sh["gk"] = np.ascontiguousarray(inp["gqa_k_norm"][l].reshape(128, 1))
    sh["mlaq_g"] = _fm(inp["mla_q_norm"][l], 4)
    sh["mlakv_g"] = _fm(inp["mla_kv_norm"][l], 2)
    sh["w_uq"] = np.ascontiguousarray(inp["mla_w_uq"][l])
    sh["w_ukv"] = np.ascontiguousarray(inp["mla_w_ukv"][l])
    sh["w_ba"] = np.ascontiguousarray(inp["w_branch_a"][l])
    sh["w_bb"] = np.ascontiguousarray(inp["w_branch_b"][l])
    sh["w_bc"] = np.ascontiguousarray(inp["w_branch_c"][l])
    sh["w_out"] = np.ascontiguousarray(inp["w_out"][l])
    sh["lnp"] = np.ascontiguousarray(np.stack([_fm(inp[n][l], 16) for n in ("ln1_g", "ln1_b", "ln2_g", "ln2_b")], axis=1))
    sh["w_r"] = np.ascontiguousarray(np.concatenate([inp["w_router_group"][l], inp["w_router_expert"][l]], axis=1))
    br = np.concatenate([inp["b_router_group"][l], inp["b_router_expert"][l]])[None, :]
    sh["b_r"] = np.ascontiguousarray(np.broadcast_to(br, (128, 72)).astype(np.float32))
    if with_moe:
        sh["w_eg"] = np.ascontiguousarray(inp["w_expert_gate"][l])
        sh["w_eu"] = np.ascontiguousarray(inp["w_expert_up"][l])
        sh["w_ed"] = np.ascontiguousarray(inp["w_expert_down"][l])
    return {k + str(l): v for k, v in sh.items()}


def prep_core(inp, core, shared, nlayers=2):
    b, half = core // 2, core % 2
    cst = _consts(half)
    m = dict(shared)
    xb = np.asarray(inp["x"][b], np.float32)
    pos = _local_pos(half)
    m["xt"] = np.ascontiguousarray(np.concatenate([xb[pos].T, np.asarray(inp["ctx"][b], np.float32).T], axis=1))
    cond = np.stack([inp["c"][b], inp["c_ctx"]], axis=1)
    m["cond"] = np.ascontiguousarray(cond.reshape(KC, 128, 2).transpose(1, 0, 2).reshape(128, 32).astype(np.float32))
    m["tab128"] = cst["tab128"]
    m["tab64"] = cst["tab64"]
    m["rt128"] = _rot_T(128)
    m["rt64"] = _rot_T(64)
    m["ident"] = np.eye(128, dtype=np.float32)
    ri, ci, valid = cst["nabidx"]
    for l in range(nlayers):
        nq = 8 if l == 0 else 4
        rpb = np.asarray(inp["na_rpb"][l], np.float32)
        m["nab" + str(l)] = np.ascontiguousarray(
            np.where(valid[None, :nq], rpb[:, ri[:nq], ci[:nq]], np.float32(-30000.0)).astype(np.float32))
    return m


def proj_blocks(src, layer):
    blocks = []
    for i in range(8):
        q = (i * 512) if (layer == 0 or i < 4) else None
        blocks.append(dict(src=src[:, i * 512:(i + 1) * 512], ntok=512, modcol=0, tabcol=i * 512, qcol=q, kd=i * 512, kn=i * 512))
    blocks.append(dict(src=src[:, NNAT:TL], ntok=NCTX, modcol=1, tabcol=NNAT, qcol=(NNAT if layer == 0 else None), kd=NNAT, kn=NNAT))
    return blocks


def na_tiles(jl):
    s_, j = jl // 4, jl % 4
    base = 16 * s_
    obase = 16 * (1 - s_)
    tiles = []
    for i in range(8):
        lt = 4 * j - 2 + i
        if 0 <= lt < 16:
            tiles.append(base + lt)
        elif lt < 0:
            tiles.append(obase + 16 + lt)
        else:
            tiles.append(obase + lt - 16)
    return tiles


def q_blocks(layer):
    qb = [(i * 512, 512, i) for i in range(8 if layer == 0 else 4)]
    if layer == 0:
        qb.append((NNAT, NCTX, None))
    return qb


def phase_attn(C, W, kind, qblocks):
    nc = C.nc
    ph = Phase(nc, C.sems, "att" + kind)
    ps = C.ps
    nkeys = NKEY
    nkt = nkeys // 128
    with ExitStack() as st:
        sb = lambda name, shape, dt: st.enter_context(nc.sbuf_tensor(uname(name), shape, dt))
        kt_r = Ring(nc, st, "t_k", [128, nkeys], BF16, 2)
        v_r = Ring(nc, st, "t_v", [128, nkt, 128], BF16, 2)
        q_r = Ring(nc, st, "t_q", [128, 512], BF16, 2)
        p_r = Ring(nc, st, "t_p", [128, 512], BF16, 3)
        rd_r = Ring(nc, st, "t_rd", [128, 512], F32, 2)
        o_r = Ring(nc, st, "t_o", [128, 512], BF16, 2)
        if kind == "b":
            kr = sb("t_kr", [64, nkeys], BF16)
            qr_r = Ring(nc, st, "t_qr", [64, 512], BF16, 2)
            ph.add(SP, e_dma(kr[:], C.KBR), writes=["kr"], dma="kr")
        if kind == "c":
            b_r = Ring(nc, st, "t_b", [128, 512], BF16, 3)
        s_i = [0]
        blk_i = [0]
        nheads = {"a": 6, "b": 5, "c": 5}[kind]
        Ksrc = {"a": C.KA, "b": C.KBN, "c": C.KCs}[kind]
        Vsrc = {"a": C.VA, "b": C.VB, "c": C.VC}[kind]
        Qsrc = {"a": C.QA, "b": C.QBN, "c": C.QC}[kind]
        Odst = {"a": C.OA, "b": C.OB, "c": C.OC}[kind]
        Vv = Vsrc.rearrange("(t p) c -> p t c", p=128)
        kT = vv = None
        for h in range(nheads):
            kvh = h // 3 if kind == "a" else h
            if kind != "a" or h % 3 == 0:
                kT, kTk = kt_r.next()
                ph.add(SP, e_dma(kT[:], Ksrc[kvh]), writes=[kTk], dma=kTk)
                vv, vk = v_r.next()
                ph.add(SP, e_dma(vv[:], Vv[:, :, kvh * 128:(kvh + 1) * 128]), writes=[vk], dma=vk)
            for (qcol, n, j) in qblocks:
                q, qk = q_r.next()
                ph.add(SP, e_dma(q[:, 0:n], Qsrc[h, :, qcol:qcol + n]), writes=[qk], dma=qk)
                rkeys = [qk, kTk]
                if kind == "b":
                    qr, qrk = qr_r.next()
                    ph.add(SP, e_dma(qr[:, 0:n], C.QBR[h, :, qcol:qcol + n]), writes=[qrk], dma=qrk)
                    rkeys += [qrk, "kr"]
                if j is None:
                    tiles = [(nkt - 2, None), (nkt - 1, None)]
                elif kind == "c":
                    tiles = [(loc, i) for i, loc in enumerate(na_tiles(j))]
                    tiles += [(nkt - 2, None), (nkt - 1, None)]
                else:
                    tiles = [(t, None) for t in range(nkt)]
                bo = 3 + (blk_i[0] % 2)
                bd = 5 + (blk_i[0] % 2)
                blk_i[0] += 1
                nt = len(tiles)
                pend = {}

                def emit_s(idx):
                    t, bi = tiles[idx]
                    sbk = s_i[0] % 3
                    s_i[0] += 1
                    items = [(ps[sbk][:, 0:n], kT[:, t * 128:(t + 1) * 128], q[:, 0:n], True, (kind == "a") or (kind == "c" and bi is None))]
                    rk = list(rkeys)
                    if kind == "b":
                        items.append((ps[sbk][:, 0:n], kr[:, t * 128:(t + 1) * 128], qr[:, 0:n], False, True))
                    if kind == "c" and bi is not None:
                        bt, btk = b_r.next()
                        ph.add(POOL, e_dma(bt[:, :], W.nab[h, j, bi]), writes=[btk], dma=btk)
                        items.append((ps[sbk][:, 0:n], C.ident_bf[:], bt[:, 0:n], False, True))
                        rk.append(btk)
                    ph.add(PE, e_mm(items), reads=rk, writes=[("ps", sbk)])
                    pend[idx] = sbk

                emit_s(0)
                for idx in range(nt):
                    if idx + 1 < nt:
                        emit_s(idx + 1)
                    sbk = pend.pop(idx)
                    p, pk = p_r.next()
                    ph.add(ACT, e_act(p[:, 0:n], ps[sbk][:, 0:n], AF.Exp), reads=[("ps", sbk)], writes=[pk])
                    t, _ = tiles[idx]
                    ph.add(PE, e_mm([(ps[bo][:, 0:n], vv[:, t, :], p[:, 0:n], idx == 0, idx == nt - 1),
                                     (ps[bd][:, 0:n], C.ones_bf[:], p[:, 0:n], idx == 0, idx == nt - 1)]),
                           reads=[pk, vk, "g"], writes=[("ps", bo), ("ps", bd)], acc=True)
                rd, rdk = rd_r.next()
                ph.add(DVE, lambda e, rd=rd, bd=bd, n=n: e.reciprocal(out=rd[:, 0:n], in_=ps[bd][:, 0:n]), reads=[("ps", bd)], writes=[rdk])
                o, ok = o_r.next()
                ph.add(DVE, e_tt(o[:, 0:n], ps[bo][:, 0:n], rd[:, 0:n], ALU.mult), reads=[("ps", bo), rdk], writes=[ok])
                ph.add(SP, e_dma(Odst[h, :, qcol:qcol + n], o[:, 0:n]), reads=[ok], dma=ok)
        nops = ph.finalize()
    return nops


def ln_block(ph, C, rings, v, vkey, n, col, gi, dst, dstkey_prefix, off=0):
    ps = C.ps
    sq_r, st_r, o_r = rings["sq"], rings["stat"], rings["out"]
    bs, bq = 6, 7
    for m in range(KC):
        sq, sqk = sq_r.next()
        ph.add(ACT, e_act(sq[:, 0:n], v[:, m, off:off + n], AF.Square), reads=[(vkey, m)], writes=[sqk])
        ph.add(PE, e_mm([(ps[bs][:, 0:n], C.ones_f[:], v[:, m, off:off + n], m == 0, m == KC - 1)]),
               reads=[(vkey, m), "g"], writes=[("ps", bs)], acc=True)
        ph.add(PE, e_mm([(ps[bq][:, 0:n], C.ones_f[:], sq[:, 0:n], m == 0, m == KC - 1)]),
               reads=[sqk, "g"], writes=[("ps", bq)], acc=True)
    mean, meank = st_r.next()
    ph.add(DVE, e_ts(mean[:, 0:n], ps[bs][:, 0:n], 1.0 / D, ALU.mult), reads=[("ps", bs)], writes=[meank])
    msq, msqk = st_r.next()
    ph.add(DVE, e_tt(msq[:, 0:n], mean[:, 0:n], mean[:, 0:n], ALU.mult), reads=[meank], writes=[msqk])
    var, vark = st_r.next()
    ph.add(DVE, lambda e, var=var, msq=msq: e.scalar_tensor_tensor(out=var[:, 0:n], in0=ps[bq][:, 0:n], scalar=1.0 / D, in1=msq[:, 0:n],
                                                                     op0=ALU.mult, op1=ALU.subtract),
           reads=[("ps", bq), msqk], writes=[vark])
    lnv, lnk = st_r.next()
    ph.add(ACT, e_act(lnv[:, 0:n], var[:, 0:n], AF.Ln, bias=EPS), reads=[vark], writes=[lnk])
    rstd, rstdk = st_r.next()
    ph.add(ACT, e_act(rstd[:, 0:n], lnv[:, 0:n], AF.Exp, scale=-0.5), reads=[lnk], writes=[rstdk])
    for m in range(KC):
        t, tk = sq_r.next()
        ph.add(DVE, e_tt(t[:, 0:n], v[:, m, off:off + n], mean[:, 0:n], ALU.subtract), reads=[(vkey, m), meank], writes=[tk])
        t2, t2k = sq_r.next()
        ph.add(DVE, e_tt(t2[:, 0:n], t[:, 0:n], rstd[:, 0:n], ALU.mult), reads=[tk, rstdk], writes=[t2k])
        o, ok = o_r.next()
        ph.add(ACT, e_act(o[:, 0:n], t2[:, 0:n], AF.Identity, scale=C.lnps[:, gi, m:m + 1], bias=C.lnps[:, gi + 1, m:m + 1]),
               reads=[t2k, "g"], writes=[ok])
        ph.add(SP, e_dma(dst[m * 128:(m + 1) * 128, :], o[:, 0:n]), reads=[ok], dma=ok)


def phase_merge(C, W, xsrc, layer):
    nc = C.nc
    ph = Phase(nc, C.sems, "merge")
    ps = C.ps
    NB = 256
    with ExitStack() as st:
        sb = lambda name, shape, dt: st.enter_context(nc.sbuf_tensor(uname(name), shape, dt))
        wba = sb("m_wba", [128, 6, D], BF16)
        wbb = sb("m_wbb", [128, 5, D], BF16)
        wbc = sb("m_wbc", [128, 5, D], BF16)
        ph.add(POOL, e_dma(wba[:], W.w_ba.rearrange("(k p) n -> p k n", p=128)), writes=["wba"], dma="wba")
        ph.add(POOL, e_dma(wbb[:], W.w_bb.rearrange("(k p) n -> p k n", p=128)), writes=["wbb"], dma="wbb")
        ph.add(POOL, e_dma(wbc[:], W.w_bc.rearrange("(k p) n -> p k n", p=128)), writes=["wbc"], dma="wbc")
        wo_r = Ring(nc, st, "m_wo", [128, KC, 512], BF16, 2)
        o_in = Ring(nc, st, "m_oin", [128, 16, NB], BF16, 2)
        g_r = Ring(nc, st, "m_g", [128, 3, NB], BF16, 3)
        ta_r = Ring(nc, st, "m_ta", [128, NB], F32, 6)
        mix = sb("m_mix", [128, KC, NB], BF16)
        v = sb("m_v", [128, KC, NB], F32)
        x_r = Ring(nc, st, "m_x", [128, NB], F32, 3)
        xa_r = Ring(nc, st, "m_xa", [128, NB], F32, 3)
        rings = dict(sq=Ring(nc, st, "m_sq", [128, NB], F32, 4), stat=Ring(nc, st, "m_st", [128, NB], F32, 5),
                     out=Ring(nc, st, "m_out", [128, NB], F32, 3))
        wov = W.w_out.rearrange("(kc p) n -> p kc n", p=128)
        Gv = C.G.rearrange("(b m) p t -> m p b t", b=3)
        blocks = [(i * NB, NB, 0) for i in range((NNAT if layer == 0 else NOWN) // NB)]
        if layer == 0:
            blocks.append((NNAT, NCTX, 1))
        pi = [0]
        for (qcol, n, col) in blocks:
            oin, oink = o_in.next()
            ph.add(SP, e_dma(oin[:, 0:6, 0:n], C.OA[:, :, qcol:qcol + n].rearrange("h p t -> p h t")), writes=[(oink, 0)], dma=(oink, 0))
            ph.add(SP, e_dma(oin[:, 6:11, 0:n], C.OB[:, :, qcol:qcol + n].rearrange("h p t -> p h t")), writes=[(oink, 1)], dma=(oink, 1))
            ph.add(SP, e_dma(oin[:, 11:16, 0:n], C.OC[:, :, qcol:qcol + n].rearrange("h p t -> p h t")), writes=[(oink, 2)], dma=(oink, 2))
            for m in range(KC):
                g, gk = g_r.next()
                ph.add(SP, e_dma(g[:, :, 0:n], Gv[m, :, :, qcol:qcol + n]), writes=[gk], dma=gk)
                b3 = [(pi[0] * 3 + i) % 6 for i in range(3)]
                pi[0] += 1
                ms = slice(m * 128, (m + 1) * 128)
                ph.add(PE, e_mm([(ps[b3[0]][:, 0:n], wba[:, k, ms], oin[:, k, 0:n], k == 0, k == 5) for k in range(6)]),
                       reads=["wba", (oink, 0)], writes=[("ps", b3[0])])
                ph.add(PE, e_mm([(ps[b3[1]][:, 0:n], wbb[:, k, ms], oin[:, 6 + k, 0:n], k == 0, k == 4) for k in range(5)]),
                       reads=["wbb", (oink, 1)], writes=[("ps", b3[1])])
                ph.add(PE, e_mm([(ps[b3[2]][:, 0:n], wbc[:, k, ms], oin[:, 11 + k, 0:n], k == 0, k == 4) for k in range(5)]),
                       reads=["wbc", (oink, 2)], writes=[("ps", b3[2])])
                ta, tak = ta_r.next()
                ph.add(DVE, e_tt(ta[:, 0:n], ps[b3[0]][:, 0:n], g[:, 0, 0:n], ALU.mult), reads=[("ps", b3[0]), gk], writes=[tak])
                tb, tbk = ta_r.next()
                ph.add(DVE, e_tt(tb[:, 0:n], ps[b3[1]][:, 0:n], g[:, 1, 0:n], ALU.mult), reads=[("ps", b3[1]), gk], writes=[tbk])
                tcc, tck = ta_r.next()
                ph.add(DVE, e_tt(tcc[:, 0:n], ps[b3[2]][:, 0:n], g[:, 2, 0:n], ALU.mult), reads=[("ps", b3[2]), gk], writes=[tck])
                ph.add(DVE, e_tt(ta[:, 0:n], ta[:, 0:n], tb[:, 0:n], ALU.add), reads=[tak, tbk], writes=[tak])
                ph.add(DVE, e_tt(mix[:, m, 0:n], ta[:, 0:n], tcc[:, 0:n], ALU.add), reads=[tak, tck], writes=[("mix", m)])
            mixkeys = [("mix", m) for m in range(KC)]
            for m in range(KC):
                if m % 4 == 0:
                    wo, wok = wo_r.next()
                    ph.add(POOL, e_dma(wo[:], wov[:, :, m * 128:m * 128 + 512]), writes=[wok], dma=wok)
                by = 6 + (m % 2)
                ph.add(PE, e_mm([(ps[by][:, 0:n], wo[:, k, (m % 4) * 128:(m % 4 + 1) * 128], mix[:, k, 0:n], k == 0, k == KC - 1) for k in range(KC)]),
                       reads=[wok] + mixkeys, writes=[("ps", by)])
                x, xk = x_r.next()
                ph.add(SP, e_dma(x[:, 0:n], xsrc[m * 128:(m + 1) * 128, qcol:qcol + n]), writes=[xk], dma=xk)
                xa, xak = xa_r.next()
                ph.add(ACT, e_act(xa[:, 0:n], x[:, 0:n], AF.Identity, scale=ALPHA), reads=[xk], writes=[xak])
                ph.add(DVE, lambda e, m=m, by=by, xa=xa, n=n, col=col: e.scalar_tensor_tensor(
                    out=v[:, m, 0:n], in0=ps[by][:, 0:n], scalar=C.mod[:, col, 32 + m:33 + m], in1=xa[:, 0:n], op0=ALU.mult, op1=ALU.add),
                    reads=[("ps", by), xak, "g"], writes=[("v", m)])
            ln_block(ph, C, rings, v, "v", n, col, 0, C.X1[:, qcol:qcol + n], "x1")
        nops = ph.finalize()
    return nops


def phase_moe(C, W, layer, dst, n_exp=NEXP):
    nc = C.nc
    ph = Phase(nc, C.sems, "moe")
    ps = C.ps
    NB = 512
    BIG = 1.0e30
    with ExitStack() as st:
        sb = lambda name, shape, dt: st.enter_context(nc.sbuf_tensor(uname(name), shape, dt))
        wr = sb("e_wr", [128, KC, 72], F32)
        br = sb("e_br", [128, 72], F32)
        ph.add(SP, e_dma(wr[:], W.w_r.rearrange("(kc p) n -> p kc n", p=128)), writes=["wr"], dma="wr")
        ph.add(SP, e_dma(br[:], W.b_r), writes=["br"], dma="br")
        nops = ph.finalize()
        xs_r = Ring(nc, st, "e_xs", [128, 4, NB], F32, 1)
        u32_r = Ring(nc, st, "e_u32", [128, 4, NB], F32, 2)
        u16 = sb("e_u16", [128, KC, NB], BF16)
        w_r = Ring(nc, st, "e_w", [128, 8192], BF16, 4)
        mx = sb("e_mx", [128, KC, NB], F32)
        hh_r = Ring(nc, st, "e_hh", [128, NB], F32, 2)
        sg_r = Ring(nc, st, "e_sg", [128, NB], F32, 2)
        H_r = Ring(nc, st, "e_H", [128, 4, NB], BF16, 2)
        wgtT = sb("e_wgtT", [64, NB], F32)
        wm_r = Ring(nc, st, "e_wm", [64, NB], F32, 2)
        lg = sb("e_lg", [128, 72], F32)
        r8 = sb("e_r8", [128, 8], F32)
        goh = sb("e_goh", [128, 8], F32)
        pen = sb("e_pen", [128, 8], F32)
        ex8 = sb("e_ex8", [128, 8], F32)
        lm = sb("e_lm", [128, 64], F32)
        lm2 = sb("e_lm2", [128, 64], F32)
        oh1 = sb("e_oh1", [128, 64], F32)
        oh2 = sb("e_oh2", [128, 64], F32)
        sc = sb("e_sc", [128, 16], F32)
        wgt = sb("e_wgt", [128, 64], F32)
        rings = dict(sq=Ring(nc, st, "e_sq", [128, 256], F32, 4), stat=Ring(nc, st, "e_st", [128, 256], F32, 5),
                     out=Ring(nc, st, "e_out", [128, 256], F32, 3))
        x_r = Ring(nc, st, "e_x", [128, NB], F32, 2)
        xa_r = Ring(nc, st, "e_xa", [128, NB], F32, 2)
        blocks = [(i * NB, NB, 0) for i in range((NNAT if layer == 0 else NOWN) // NB)]
        if layer == 0:
            blocks.append((NNAT, NCTX, 1))
        gi = [0]
        for (qcol, n, col) in blocks:
            ph = Phase(nc, C.sems, "moe_b")
            ntt = n // 128
            srcv = C.X1[:, qcol:qcol + n].rearrange("(kc p) t -> p kc t", p=128)
            for q4 in range(4):
                xs, xk = xs_r.next()
                ph.add(SP, e_dma(xs[:, :, 0:n], srcv[:, q4 * 4:(q4 + 1) * 4, :]), writes=[xk], dma=xk)
                u32, u32k = u32_r.next()
                for kk in range(4):
                    k = q4 * 4 + kk
                    ph.add(ACT, e_act(u32[:, kk, 0:n], xs[:, kk, 0:n], AF.Identity, scale=C.sc1[:, col, 1, k:k + 1],
                                      bias=C.mod[:, col, 48 + k:49 + k]), reads=[xk, "g"], writes=[(u32k, kk)])
                    ph.add(POOL, e_copy(u16[:, k, 0:n], u32[:, kk, 0:n]), reads=[(u32k, kk)], writes=[("u16", k)])
                    for tt in range(ntt):
                        ph.add(PE, e_mm([(ps[tt][:, 0:72], u32[:, kk, tt * 128:(tt + 1) * 128], wr[:, k, :], k == 0, k == KC - 1)]),
                               reads=[(u32k, kk), "wr"], writes=[("ps", tt)], acc=True)
            for tt in range(ntt):
                A = lambda o, a, b, op: ph.add(DVE, e_tt(o, a, b, op), reads=["rt"], writes=["rt"])
                S = lambda o, a, s1, op0, s2=None, op1=None: ph.add(DVE, e_ts(o, a, s1, op0, s2, op1), reads=["rt"], writes=["rt"])
                ph.add(DVE, e_tt(lg[:], ps[tt][:, 0:72], br[:], ALU.add), reads=[("ps", tt), "br", "rt"], writes=["rt"])
                ph.add(DVE, lambda e: e.tensor_reduce(out=sc[:, 0:1], in_=lg[:, 0:8], axis=AX.X, op=ALU.max), reads=["rt"], writes=["rt"])
                S(goh[:], lg[:, 0:8], sc[:, 0:1], ALU.is_equal)
                S(sc[:, 1:2], sc[:, 0:1], -1.0, ALU.mult)
                ph.add(ACT, e_act(ex8[:], lg[:, 0:8], AF.Exp, bias=sc[:, 1:2]), reads=["rt"], writes=["rt"])
                ph.add(DVE, lambda e: e.tensor_reduce(out=sc[:, 2:3], in_=ex8[:], axis=AX.X, op=ALU.add), reads=["rt"], writes=["rt"])
                ph.add(DVE, lambda e: e.reciprocal(out=sc[:, 3:4], in_=sc[:, 2:3]), reads=["rt"], writes=["rt"])
                S(pen[:], goh[:], -1.0, ALU.add, BIG, ALU.mult)
                for g in range(8):
                    S(lm[:, g * 8:(g + 1) * 8], lg[:, 8 + g * 8:16 + g * 8], pen[:, g:g + 1], ALU.add)
                ph.add(DVE, lambda e: e.tensor_reduce(out=sc[:, 4:5], in_=lm[:], axis=AX.X, op=ALU.max), reads=["rt"], writes=["rt"])
                S(oh1[:], lm[:], sc[:, 4:5], ALU.is_equal)
                S(lm2[:], oh1[:], -BIG, ALU.mult)
                A(lm2[:], lm2[:], lm[:], ALU.add)
                ph.add(DVE, lambda e: e.tensor_reduce(out=sc[:, 5:6], in_=lm2[:], axis=AX.X, op=ALU.max), reads=["rt"], writes=["rt"])
                S(oh2[:], lm2[:], sc[:, 5:6], ALU.is_equal)
                A(sc[:, 6:7], sc[:, 5:6], sc[:, 4:5], ALU.subtract)
                ph.add(ACT, e_act(sc[:, 7:8], sc[:, 6:7], AF.Exp), reads=["rt"], writes=["rt"])
                S(sc[:, 8:9], sc[:, 7:8], 1.0, ALU.add)
                ph.add(DVE, lambda e: e.reciprocal(out=sc[:, 9:10], in_=sc[:, 8:9]), reads=["rt"], writes=["rt"])
                A(sc[:, 10:11], sc[:, 7:8], sc[:, 9:10], ALU.mult)
                A(sc[:, 11:12], sc[:, 9:10], sc[:, 3:4], ALU.mult)
                A(sc[:, 12:13], sc[:, 10:11], sc[:, 3:4], ALU.mult)
                S(wgt[:], oh1[:], sc[:, 11:12], ALU.mult)
                S(oh2[:], oh2[:], sc[:, 12:13], ALU.mult)
                A(wgt[:], wgt[:], oh2[:], ALU.add)
                ph.add(PE, lambda e: e.transpose(out=ps[4][0:64, 0:128], in_=wgt[:], identity=C.ident_f[:]),
                       reads=["rt", "g"], writes=[("ps", 4)])
                ph.add(DVE, e_copy(wgtT[:, tt * 128:(tt + 1) * 128], ps[4][0:64, 0:128]), reads=[("ps", 4), "rt"], writes=["wgtT", "rt"])
            u16keys = [("u16", k) for k in range(KC)]
            for e_i in range(n_exp):
                wg, wgk = w_r.next()
                ph.add(POOL, e_dma(wg[:].rearrange("p (k n) -> p k n", k=KC), W.w_eg[e_i].rearrange("(kc p) n -> p kc n", p=128)), writes=[wgk], dma=wgk)
                wu, wuk = w_r.next()
                ph.add(POOL, e_dma(wu[:].rearrange("p (k n) -> p k n", k=KC), W.w_eu[e_i].rearrange("(kc p) n -> p kc n", p=128)), writes=[wuk], dma=wuk)
                wd, wdk = w_r.next()
                ph.add(POOL, e_dma(wd[:].rearrange("p (k n) -> p k n", k=4), W.w_ed[e_i].rearrange("(kc p) n -> p kc n", p=128)), writes=[wdk], dma=wdk)
                wgv = wg[:].rearrange("p (k n) -> p k n", k=KC)
                wuv = wu[:].rearrange("p (k n) -> p k n", k=KC)
                wdv = wd[:].rearrange("p (k n) -> p k n", k=4)
                def emit_wm(ei):
                    wm, wmk = wm_r.next()
                    ph.add(DVE, e_ts(wm[:, 0:n], wgtT[:, 0:n], C.ident_f[0:64, ei:ei + 1], ALU.mult), reads=["wgtT", "g"], writes=[wmk])
                    return wm, wmk

                def emit_bc(ei, wm, wmk):
                    bb_ = 4 + (ei % 2)
                    ph.add(PE, e_mm([(ps[bb_][:, 0:n], C.ones_f[0:64, :], wm[:, 0:n], True, True)]), reads=[wmk, "g"], writes=[("ps", bb_)])

                if e_i == 0:
                    wm_c = emit_wm(0)
                    emit_bc(0, *wm_c)
                bb = 4 + (e_i % 2)
                wm_n = None
                H, Hk = H_r.next()
                for c in range(4):
                    if c == 2 and e_i + 1 < n_exp:
                        wm_n = emit_wm(e_i + 1)
                    bg = (gi[0] % 2) * 2
                    gi[0] += 1
                    cs = slice(c * 128, (c + 1) * 128)
                    ph.add(PE, e_mm([(ps[bg][:, 0:n], wgv[:, k, cs], u16[:, k, 0:n], k == 0, k == KC - 1) for k in range(KC)]),
                           reads=[wgk] + u16keys, writes=[("ps", bg)])
                    ph.add(PE, e_mm([(ps[bg + 1][:, 0:n], wuv[:, k, cs], u16[:, k, 0:n], k == 0, k == KC - 1) for k in range(KC)]),
                           reads=[wuk] + u16keys, writes=[("ps", bg + 1)])
                    sg, sgk = sg_r.next()
                    ph.add(ACT, e_act(sg[:, 0:n], ps[bg][:, 0:n], AF.Silu), reads=[("ps", bg)], writes=[sgk])
                    hh, hhk = hh_r.next()
                    ph.add(DVE, e_tt(hh[:, 0:n], ps[bg + 1][:, 0:n], sg[:, 0:n], ALU.mult), reads=[("ps", bg + 1), sgk], writes=[hhk])
                    ph.add(DVE, e_tt(H[:, c, 0:n], ps[bb][:, 0:n], hh[:, 0:n], ALU.mult), reads=[("ps", bb), hhk], writes=[(Hk, c)])
                if wm_n is not None:
                    emit_bc(e_i + 1, *wm_n)
                Hkeys = [(Hk, c) for c in range(4)]
                for m in range(KC):
                    by = 6 + (m % 2)
                    ph.add(PE, e_mm([(ps[by][:, 0:n], wdv[:, c, m * 128:(m + 1) * 128], H[:, c, 0:n], c == 0, c == 3) for c in range(4)]),
                           reads=[wdk] + Hkeys, writes=[("ps", by)])
                    if e_i == 0:
                        ph.add(DVE, e_copy(mx[:, m, 0:n], ps[by][:, 0:n]), reads=[("ps", by)], writes=[("mx", m)])
                    else:
                        ph.add(DVE, e_tt(mx[:, m, 0:n], ps[by][:, 0:n], mx[:, m, 0:n], ALU.add), reads=[("ps", by), ("mx", m)], writes=[("mx", m)])
            for m in range(KC):
                x, xk = x_r.next()
                ph.add(SP, e_dma(x[:, 0:n], C.X1[m * 128:(m + 1) * 128, qcol:qcol + n]), writes=[xk], dma=xk)
                xa, xak = xa_r.next()
                ph.add(ACT, e_act(xa[:, 0:n], x[:, 0:n], AF.Identity, scale=ALPHA), reads=[xk], writes=[xak])
                ph.add(DVE, lambda e, m=m, xa=xa, n=n, col=col: e.scalar_tensor_tensor(
                    out=mx[:, m, 0:n], in0=mx[:, m, 0:n], scalar=C.mod[:, col, 80 + m:81 + m], in1=xa[:, 0:n], op0=ALU.mult, op1=ALU.add),
                    reads=[("mx", m), xak, "g"], writes=[("mx", m)])
            for off in range(0, n, 256):
                ln_block(ph, C, rings, mx, "mx", 256, col, 2, dst[:, qcol + off:qcol + off + 256], "xo", off=off)
            nops += ph.finalize()
    return nops


def build_program(nlayers=2, debug=False, with_moe=True, stop_after=None, n_exp=NEXP):
    nc = bass.Bass("TRN2", target_bir_lowering=False)
    C = declare_io(nc, nlayers=nlayers, debug=debug, with_moe=with_moe)
    n = 0
    with ExitStack() as st:
        alloc_globals(C, st)
        for l in range(nlayers):
            W = C.L[l]
            src = C.xt if l == 0 else C.XO0
            lay = 0 if l < nlayers - 1 or nlayers == 1 else 1
            if nlayers == 1:
                lay = 0
            n += phase_ada(C, W)
            if stop_after == "ada":
                break
            n += phase_proj(C, W, proj_blocks(src, lay))
            if stop_after == "proj":
                break
            for k in "abc":
                n += phase_attn(C, W, k, q_blocks(lay))
            if stop_after == "att":
                break
            n += phase_merge(C, W, src, lay)
            if stop_after == "merge":
                break
            if with_moe:
                n += phase_moe(C, W, lay, C.XO0 if lay == 0 else C.xo, n_exp=n_exp)
    return nc, n


_PROG = {}


def kernel(**inputs):
    inp = {k: np.asarray(v) for k, v in inputs.items()}
    if "nc" not in _PROG:
        _PROG["nc"] = build_program(2)[0]
    nc = _PROG["nc"]
    shared = {}
    for l in range(2):
        shared.update(prep_layer_shared(inp, l))
    in_maps = [prep_core(inp, c, shared) for c in range(8)]
    res = run_bass_kernel_spmd(nc, in_maps, core_ids=list(range(8)))
    del in_maps
    x = np.asarray(inp["x"])
    out = np.empty(x.shape, np.float32)
    for c in range(8):
        xo = np.asarray(res.results[c]["xo"])
        b, half = c // 2, c % 2
        out[b, half * NOWN:(half + 1) * NOWN] = xo.T
    return out
```

```python
import math
from contextlib import ExitStack

import numpy as np
import concourse.bass as bass
import concourse.mybir as mybir
from concourse.bass_utils import run_bass_kernel_spmd

F32 = mybir.dt.float32
BF16 = mybir.dt.bfloat16
AF = mybir.ActivationFunctionType
ALU = mybir.AluOpType
AX = mybir.AxisListType

PE, ACT, DVE, POOL, SP = "tensor", "scalar", "vector", "gpsimd", "sync"
ENGS = [PE, ACT, DVE, POOL, SP]

D = 2048
KC = 16
NOWN = 2048
NCTX = 256
T = NOWN + NCTX
NNAT = 4096
NKEY = NNAT + NCTX
NKT = NKEY // 128
TL = NNAT + NCTX
EPS = 1e-6
ALPHA = 4 ** 0.25
NEXP = 64
DEXP = 512

SEG = {}
_o = 0
for _n, _w in (("aq", 768), ("ak", 256), ("av", 256), ("bq", 512), ("bkv", 256), ("bkr", 64),
               ("cq", 640), ("ck", 640), ("cv", 640), ("gate", 6144)):
    SEG[_n] = (_o, _w)
    _o += _w
IN_W = _o


class Op:
    __slots__ = ("eng", "emit", "deps", "idx", "sig_sem", "sig_val", "is_dma", "needs_sig", "acc")

    def __init__(self, eng, emit, is_dma):
        self.eng = eng
        self.emit = emit
        self.deps = set()
        self.is_dma = is_dma
        self.needs_sig = False
        self.sig_sem = None
        self.sig_val = 0
        self.acc = False


class Phase:
    def __init__(self, nc, sems, name="ph"):
        self.nc = nc
        self.name = name
        self.ops = []
        self.last_writer = {}
        self.readers = {}
        self.sem_pool = sems
        self.dma_sem_of = {}
        self.dma_cnt = {}
        self.n_dma = {"dsw": 0, "dhw": 0}

    def add(self, eng, emit, reads=(), writes=(), dma=None, acc=False):
        op = Op(eng, emit, dma is not None)
        op.idx = len(self.ops)
        op.acc = acc
        deps = op.deps
        for b in reads:
            w = self.last_writer.get(b)
            if w is not None:
                deps.add(w)
        for b in writes:
            w = self.last_writer.get(b)
            if w is not None:
                wop = self.ops[w]
                if not (acc and wop.acc and wop.eng == eng):
                    deps.add(w)
            for r in self.readers.get(b, ()):
                deps.add(r)
        for b in reads:
            self.readers.setdefault(b, []).append(op.idx)
        for b in writes:
            self.last_writer[b] = op.idx
            self.readers[b] = []
        if dma is not None:
            kind = "dsw" if eng == POOL else "dhw"
            dma = (kind, dma)
            if dma not in self.dma_sem_of:
                self.dma_sem_of[dma] = self.n_dma[kind]
                self.n_dma[kind] += 1
                self.dma_cnt[dma] = self.sem_pool["dsw_cnt"][self.dma_sem_of[dma]] if kind == "dsw" else 0
            self.dma_cnt[dma] += 16
            if kind == "dsw":
                self.sem_pool["dsw_cnt"][self.dma_sem_of[dma]] = self.dma_cnt[dma]
            op.sig_sem = (kind, self.dma_sem_of[dma])
            op.sig_val = self.dma_cnt[dma]
            op.needs_sig = True
        self.ops.append(op)
        return op.idx

    def finalize(self):
        nc = self.nc
        ops = self.ops
        for op in ops:
            for d in op.deps:
                ops[d].needs_sig = True
        cnt = {e: 0 for e in ENGS}
        for op in ops:
            if not op.is_dma and op.needs_sig:
                cnt[op.eng] += 1
                op.sig_sem = ("eng", op.eng)
                op.sig_val = cnt[op.eng]
        for kind in ("dsw", "dhw"):
            assert self.n_dma[kind] <= len(self.sem_pool[kind]), f"{self.name}: need {self.n_dma[kind]} {kind} sems"

        def semh(key):
            return self.sem_pool[key[0]][key[1]]

        waited = {e: {} for e in ENGS}
        plan = {e: [] for e in ENGS}
        for op in ops:
            need = {}
            for d in op.deps:
                p = ops[d]
                k = p.sig_sem
                if need.get(k, 0) < p.sig_val:
                    need[k] = p.sig_val
            waits = []
            for k, v in need.items():
                if waited[op.eng].get(k, 0) < v:
                    waited[op.eng][k] = v
                    waits.append((k, v))
            plan[op.eng].append((op, waits))
        final_waits = {e: {} for e in ENGS}
        for op in ops:
            if op.is_dma:
                final_waits[op.eng][op.sig_sem] = max(final_waits[op.eng].get(op.sig_sem, 0), op.sig_val)

        with nc.Block() as block:
            for e in ENGS:
                if not plan[e] and not final_waits[e]:
                    continue

                def body(eng, e=e):
                    for op, waits in plan[e]:
                        for k, v in waits:
                            eng.wait_ge(semh(k), v)
                        ins = op.emit(eng)
                        if op.needs_sig:
                            ins.then_inc(semh(op.sig_sem), 16 if op.is_dma else 1)
                    for k, v in final_waits[e].items():
                        eng.wait_ge(semh(k), v)

                getattr(block, e)(body)
        with nc.Block() as block2:
            def clr(eng):
                for e in ENGS:
                    eng.sem_clear(self.sem_pool["eng"][e])
                for i in range(self.n_dma["dhw"]):
                    eng.sem_clear(self.sem_pool["dhw"][i])
            block2.gpsimd(clr)
        n = len(ops)
        self.ops = []
        return n


def make_sems(nc, stack, nhw=40, nsw=12):
    pool = {"eng": {}, "dhw": [], "dsw": [], "dsw_cnt": [0] * nsw}
    for e in ENGS:
        pool["eng"][e] = stack.enter_context(nc.semaphore("s_" + e))
    for i in range(nhw):
        pool["dhw"].append(stack.enter_context(nc.semaphore(f"s_dhw{i}")))
    for i in range(nsw):
        pool["dsw"].append(stack.enter_context(nc.semaphore(f"s_dsw{i}")))
    return pool


def e_dma(out, in_):
    return lambda e: e.dma_start(out=out, in_=in_)


def e_mm(items):
    def emit(e):
        ins = None
        for (o, l, r, s, t) in items:
            ins = e.matmul(o, l, r, start=s, stop=t)
        return ins
    return emit


def e_act(out, in_, func, scale=None, bias=None):
    kw = {}
    if scale is not None:
        kw["scale"] = scale
    if bias is not None:
        kw["bias"] = bias
    return lambda e: e.activation(out=out, in_=in_, func=func, **kw)


def e_tt(out, a, b, op):
    return lambda e: e.tensor_tensor(out=out, in0=a, in1=b, op=op)


def e_ts(out, a, s1, op0, s2=None, op1=None):
    if op1 is None:
        return lambda e: e.tensor_scalar(out=out, in0=a, scalar1=s1, scalar2=None, op0=op0)
    return lambda e: e.tensor_scalar(out=out, in0=a, scalar1=s1, scalar2=s2, op0=op0, op1=op1)


def e_copy(out, in_):
    return lambda e: e.tensor_copy(out=out, in_=in_)


_UNIQ = [0]


def uname(name):
    _UNIQ[0] += 1
    return f"{name}_{_UNIQ[0]}"


class Ring:
    def __init__(self, nc, stack, name, shape, dtype, n):
        name = uname(name)
        self.bufs = [stack.enter_context(nc.sbuf_tensor(f"{name}_{i}", shape, dtype)) for i in range(n)]
        self.name = name
        self.i = 0

    def next(self):
        b = self.bufs[self.i % len(self.bufs)]
        k = (self.name, self.i % len(self.bufs))
        self.i += 1
        return b, k


class Ctx:
    pass


TABW = TL


def declare_io(nc, nlayers=2, debug=False, with_moe=True):
    C = Ctx()
    C.nc = nc

    def inp(name, shape, dt=F32):
        return nc.dram_tensor(name, list(shape), dt, kind="ExternalInput").ap()

    def scr(name, shape, dt=BF16, out=False):
        kind = "ExternalOutput" if (out or debug) else "Internal"
        return nc.dram_tensor(name, list(shape), dt, kind=kind).ap()

    C.xt = inp("xt", [D, TL])
    C.cond = inp("cond", [128, 32])
    C.tab128 = inp("tab128", [2, 128, TABW])
    C.tab64 = inp("tab64", [2, 64, TABW])
    C.rt128 = inp("rt128", [128, 128])
    C.rt64 = inp("rt64", [64, 64])
    C.ident = inp("ident", [128, 128])
    C.L = []
    for l in range(nlayers):
        W = Ctx()
        sfx = str(l)
        W.w_ada = inp("w_ada" + sfx, [D, 6 * D])
        W.b_ada = inp("b_ada" + sfx, [128, 96])
        W.w_in = inp("w_in" + sfx, [D, IN_W])
        W.gq = inp("gq" + sfx, [128, 1])
        W.gk = inp("gk" + sfx, [128, 1])
        W.mlaq_g = inp("mlaq_g" + sfx, [128, 4])
        W.mlakv_g = inp("mlakv_g" + sfx, [128, 2])
        W.w_uq = inp("w_uq" + sfx, [512, 960])
        W.w_ukv = inp("w_ukv" + sfx, [256, 1280])
        W.nab = inp("nab" + sfx, [5, 8 if l == 0 else 4, 8, 128, 512])
        W.w_ba = inp("w_ba" + sfx, [768, D])
        W.w_bb = inp("w_bb" + sfx, [640, D])
        W.w_bc = inp("w_bc" + sfx, [640, D])
        W.w_out = inp("w_out" + sfx, [D, D])
        W.lnp = inp("lnp" + sfx, [128, 4, 16])
        W.w_r = inp("w_r" + sfx, [D, 72])
        W.b_r = inp("b_r" + sfx, [128, 72])
        if with_moe:
            W.w_eg = inp("w_eg" + sfx, [NEXP, D, DEXP])
            W.w_eu = inp("w_eu" + sfx, [NEXP, D, DEXP])
            W.w_ed = inp("w_ed" + sfx, [NEXP, DEXP, D])
        C.L.append(W)
    C.QA = scr("QA", [6, 128, TL])
    C.KA = scr("KA", [2, 128, NKEY])
    C.VA = scr("VA", [NKEY, 256])
    C.QBN = scr("QBN", [5, 128, TL])
    C.QBR = scr("QBR", [5, 64, TL])
    C.KBN = scr("KBN", [5, 128, NKEY])
    C.KBR = scr("KBR", [64, NKEY])
    C.VB = scr("VB", [NKEY, 640])
    C.QC = scr("QC", [5, 128, TL])
    C.KCs = scr("KCs", [5, 128, NKEY])
    C.VC = scr("VC", [NKEY, 640])
    C.G = scr("G", [48, 128, TL])
    C.OA = scr("OA", [6, 128, TL])
    C.OB = scr("OB", [5, 128, TL])
    C.OC = scr("OC", [5, 128, TL])
    C.X1 = scr("X1", [D, TL], F32)
    C.XO0 = scr("XO0", [D, TL], F32)
    C.MODD = scr("MODD", [128, 192], F32)
    C.xo = scr("xo", [D, NOWN], F32, out=True)
    return C


def alloc_globals(C, st):
    nc = C.nc
    sb = lambda name, shape, dt: st.enter_context(nc.sbuf_tensor(name, shape, dt))
    C.sems = make_sems(nc, st)
    C.ps = [st.enter_context(nc.psum_tensor(f"psb{i}", [128, 512], F32)) for i in range(8)]
    C.ones_bf = sb("ones_bf", [128, 128], BF16)
    C.ones_f = sb("ones_f", [128, 128], F32)
    C.ident_f = sb("ident_f", [128, 128], F32)
    C.ident_bf = sb("ident_bf", [128, 128], BF16)
    C.rtq = sb("rtq", [128, 128], BF16)
    C.rtk = sb("rtk", [128, 128], BF16)
    C.rt64b = sb("rt64b", [64, 64], BF16)
    C.mod = sb("mod", [128, 2, 96], F32)
    C.sc1 = sb("sc1", [128, 2, 2, 16], F32)
    C.gqs = sb("gqs", [128, 1], F32)
    C.gks = sb("gks", [128, 1], F32)
    C.mlaqg = sb("mlaqg", [128, 4], F32)
    C.mlakvg = sb("mlakvg", [128, 2], F32)
    C.lnps = sb("lnps", [128, 4, 16], F32)
    C.wuq = sb("wuq", [128, 4, 960], BF16)
    C.wukv = sb("wukv", [128, 2, 1280], BF16)
    C.wukv_v = sb("wukv_v", [128, 2, 640], BF16)


def phase_ada(C, W):
    nc = C.nc
    ph = Phase(nc, C.sems, "ada")
    with ExitStack() as st:
        sb = lambda name, shape, dt: st.enter_context(nc.sbuf_tensor(uname(name), shape, dt))
        cond = sb("a_cond", [128, 32], F32)
        silu = sb("a_silu", [128, 32], F32)
        bada = sb("a_bada", [128, 96], F32)
        rt_f = sb("a_rtf", [128, 128], F32)
        rt64_f = sb("a_rt64f", [64, 64], F32)
        wring = Ring(nc, st, "a_w", [128, KC, 1024], F32, 2)
        ph.add(SP, e_dma(cond[:], C.cond), writes=["cond"], dma="cond")
        ph.add(SP, e_dma(bada[:], W.b_ada), writes=["bada"], dma="bada")
        ph.add(SP, e_dma(C.ident_f[:], C.ident), writes=["ident_f"], dma="ident")
        ph.add(SP, e_dma(rt_f[:], C.rt128), writes=["rt_f"], dma="rt_f")
        ph.add(SP, e_dma(rt64_f[:], C.rt64), writes=["rt64_f"], dma="rt64_f")
        ph.add(SP, e_dma(C.gqs[:], W.gq), writes=["gqs"], dma="gqs")
        ph.add(SP, e_dma(C.gks[:], W.gk), writes=["gks"], dma="gks")
        ph.add(SP, e_dma(C.mlaqg[:], W.mlaq_g), writes=["mlaqg"], dma="mlaqg")
        ph.add(SP, e_dma(C.mlakvg[:], W.mlakv_g), writes=["mlakvg"], dma="mlakvg")
        ph.add(SP, e_dma(C.lnps[:], W.lnp), writes=["lnps"], dma="lnps")
        ph.add(POOL, e_dma(C.wuq[:], W.w_uq.rearrange("(c p) n -> p c n", p=128)), writes=["wuq"], dma="wuq")
        ph.add(POOL, e_dma(C.wukv[:], W.w_ukv.rearrange("(c p) n -> p c n", p=128)), writes=["wukv"], dma="wukv")
        ph.add(POOL, lambda e: e.memset(C.ones_bf[:], 1.0), writes=["ones_bf"])
        ph.add(POOL, lambda e: e.memset(C.ones_f[:], 1.0), writes=["ones_f"])
        ph.add(DVE, e_copy(C.ident_bf[:], C.ident_f[:]), reads=["ident_f"], writes=["ident_bf"])
        ph.add(DVE, e_ts(C.rtq[:], rt_f[:], C.gqs[:, 0:1], ALU.mult), reads=["rt_f", "gqs"], writes=["rtq"])
        ph.add(DVE, e_ts(C.rtk[:], rt_f[:], C.gks[:, 0:1], ALU.mult), reads=["rt_f", "gks"], writes=["rtk"])
        ph.add(DVE, e_copy(C.rt64b[:], rt64_f[:]), reads=["rt64_f"], writes=["rt64b"])
        for c in range(4):
            ph.add(DVE, e_ts(C.wuq[:, c, :], C.wuq[:, c, :], C.mlaqg[:, c:c + 1], ALU.mult, math.sqrt(512.0), ALU.mult),
                   reads=["wuq", "mlaqg"], writes=["wuq"])
        for c in range(2):
            ph.add(DVE, e_ts(C.wukv[:, c, :], C.wukv[:, c, :], C.mlakvg[:, c:c + 1], ALU.mult, math.sqrt(256.0), ALU.mult),
                   reads=["wukv", "mlakvg"], writes=["wukv"])
        for c in range(2):
            src = C.wukv[:, c, :].rearrange("p (h x) -> p h x", x=256)[:, :, 128:256]
            dst = C.wukv_v[:, c, :].rearrange("p (h x) -> p h x", x=128)
            ph.add(DVE, e_copy(dst, src), reads=["wukv"], writes=["wukv_v"])
        ph.add(ACT, e_act(silu[:], cond[:], AF.Silu), reads=["cond"], writes=["silu"])
        acc = C.ps[0]
        wv = W.w_ada.rearrange("(kc p) n -> p kc n", p=128)
        for s in range(12):
            w, wk = wring.next()
            ph.add(SP, e_dma(w[:], wv[:, :, s * 1024:(s + 1) * 1024]), writes=[wk], dma=wk)
            for jj in range(8):
                j = s * 8 + jj
                ph.add(PE, e_mm([(acc[:, j * 2:(j + 1) * 2], w[:, k, jj * 128:(jj + 1) * 128], silu[:, k * 2:(k + 1) * 2],
                                  k == 0, k == KC - 1) for k in range(KC)]),
                       reads=[wk, "silu"], writes=["acc"], acc=True)
        accv = acc[:, 0:192].rearrange("p (j c) -> p j c", c=2)
        for col in range(2):
            ph.add(DVE, e_tt(C.mod[:, col, :], accv[:, :, col], bada[:], ALU.add), reads=["acc", "bada"], writes=["mod"])
        for col in range(2):
            for mf in range(2):
                seg = 1 if mf == 0 else 4
                ph.add(DVE, e_ts(C.sc1[:, col, mf, :], C.mod[:, col, seg * 16:(seg + 1) * 16], 1.0, ALU.add),
                       reads=["mod"], writes=["sc1"])
        ph.add(SP, e_dma(C.MODD, C.mod[:].rearrange("p c j -> p (c j)")), reads=["mod"], dma="modd")
        n = ph.finalize()
    return n


SCALE_A = 1.0 / math.sqrt(128.0)
SCALE_B = 1.0 / math.sqrt(192.0)
SCALE_C = 1.0 / math.sqrt(128.0)


def phase_proj(C, W, blocks):
    nc = C.nc
    ph = Phase(nc, C.sems, "proj")
    ps = C.ps
    with ExitStack() as st:
        sb = lambda name, shape, dt: st.enter_context(nc.sbuf_tensor(uname(name), shape, dt))
        xs_r = Ring(nc, st, "p_xs", [128, 4, 512], F32, 2)
        u_r = Ring(nc, st, "p_u", [128, KC, 512], BF16, 2)
        SW = 1024
        w_r = Ring(nc, st, "p_w", [128, KC, SW], BF16, 2)
        tb128 = sb("p_tb128", [128, 2, 512], F32)
        tb64 = sb("p_tb64", [64, 2, 512], F32)
        cosq = sb("p_cosq", [128, 512], F32)
        cosk = sb("p_cosk", [128, 512], F32)
        sink = sb("p_sink", [128, 512], F32)
        c64q = sb("p_c64q", [64, 512], F32)
        s64q = sb("p_s64q", [64, 512], F32)
        sq_r = Ring(nc, st, "p_sq", [128, 512], BF16, 2)
        ln_r = Ring(nc, st, "p_ln", [128, 512], F32, 2)
        rs_r = Ring(nc, st, "p_rs", [128, 512], F32, 2)
        qn_r = Ring(nc, st, "p_qn", [128, 512], BF16, 2)
        t1_r = Ring(nc, st, "p_t1", [128, 512], F32, 2)
        t2_r = Ring(nc, st, "p_t2", [128, 512], F32, 2)
        stg_r = Ring(nc, st, "p_stg", [128, 512], BF16, 4)
        vst_r = Ring(nc, st, "p_vst", [128, 640], BF16, 2)
        latq = sb("p_latq", [128, 4, 512], BF16)
        latkv = sb("p_latkv", [128, 2, 512], BF16)
        main_i = [0]
        aux_i = [0]

        def mainbank():
            b = main_i[0] % 4
            main_i[0] += 1
            return b

        def auxpair():
            a = 4 + 2 * (aux_i[0] % 2)
            aux_i[0] += 1
            return a, a + 1

        wv = W.w_in.rearrange("(kc p) n -> p kc n", p=128)

        def load_w(c0, ncols):
            w, wk = w_r.next()
            ph.add(POOL, e_dma(w[:, :, 0:ncols], wv[:, :, c0:c0 + ncols]), writes=[wk], dma=wk)
            return w, wk

        def store(dst, src, srckey):
            ph.add(SP, e_dma(dst, src), reads=[srckey], dma=srckey)

        for blk in blocks:
            n = blk["ntok"]
            mc = blk["modcol"]
            tc0 = blk["tabcol"]
            qcol, kd, kn = blk["qcol"], blk["kd"], blk["kn"]
            u, uk = u_r.next()
            srcv = blk["src"].rearrange("(kc p) t -> p kc t", p=128)
            for q4 in range(4):
                xs, xk = xs_r.next()
                ph.add(SP, e_dma(xs[:, :, 0:n], srcv[:, q4 * 4:(q4 + 1) * 4, :]), writes=[xk], dma=xk)
                for kk in range(4):
                    k = q4 * 4 + kk
                    ph.add(ACT, e_act(u[:, k, 0:n], xs[:, kk, 0:n], AF.Identity, scale=C.sc1[:, mc, 0, k:k + 1],
                                      bias=C.mod[:, mc, k:k + 1]), reads=[xk, "g"], writes=[(uk, k)])
            ukeys = [(uk, k) for k in range(KC)]
            ph.add(SP, e_dma(tb128[:, :, 0:n], C.tab128[:, :, tc0:tc0 + n].rearrange("a p t -> p a t")),
                   writes=["tb128"], dma="tb128")
            ph.add(SP, e_dma(tb64[:, :, 0:n], C.tab64[:, :, tc0:tc0 + n].rearrange("a p t -> p a t")),
                   writes=["tb64"], dma="tb64")
            if qcol is not None:
                ph.add(DVE, e_ts(cosq[:, 0:n], tb128[:, 0, 0:n], C.gqs[:, 0:1], ALU.mult), reads=["tb128"], writes=["cosq"])
                ph.add(DVE, e_ts(c64q[:, 0:n], tb64[:, 0, 0:n], SCALE_B, ALU.mult), reads=["tb64"], writes=["c64q"])
                ph.add(DVE, e_ts(s64q[:, 0:n], tb64[:, 1, 0:n], SCALE_B, ALU.mult), reads=["tb64"], writes=["s64q"])
            if kd is not None:
                ph.add(DVE, e_ts(cosk[:, 0:n], tb128[:, 0, 0:n], C.gks[:, 0:1], ALU.mult, math.sqrt(128.0), ALU.mult),
                       reads=["tb128"], writes=["cosk"])
                ph.add(DVE, e_ts(sink[:, 0:n], tb128[:, 1, 0:n], math.sqrt(128.0), ALU.mult), reads=["tb128"], writes=["sink"])

            def gemm_chunk(w, wk, cc, m=128):
                b = mainbank()
                ph.add(PE, e_mm([(ps[b][0:m, 0:n], w[:, k, cc:cc + m], u[:, k, 0:n], k == 0, k == KC - 1) for k in range(KC)]),
                       reads=[wk] + ukeys, writes=[("ps", b)])
                return b

            def rope128(b, rt, rtkey, cos_t, coskey, sin_t, sinkey, dst):
                a0, a1 = auxpair()
                sq, sqk = sq_r.next()
                ph.add(ACT, e_act(sq[:, 0:n], ps[b][:, 0:n], AF.Square), reads=[("ps", b)], writes=[sqk])
                ph.add(PE, e_mm([(ps[a0][:, 0:n], C.ones_bf[:], sq[:, 0:n], True, True)]), reads=[sqk, "g"], writes=[("ps", a0)])
                ln, lnk = ln_r.next()
                ph.add(ACT, e_act(ln[:, 0:n], ps[a0][:, 0:n], AF.Ln, bias=128.0 * EPS), reads=[("ps", a0)], writes=[lnk])
                rs, rsk = rs_r.next()
                ph.add(ACT, e_act(rs[:, 0:n], ln[:, 0:n], AF.Exp, scale=-0.5), reads=[lnk], writes=[rsk])
                qn, qnk = qn_r.next()
                ph.add(DVE, e_tt(qn[:, 0:n], ps[b][:, 0:n], rs[:, 0:n], ALU.mult), reads=[("ps", b), rsk], writes=[qnk])
                ph.add(PE, e_mm([(ps[a1][:, 0:n], rt[:], qn[:, 0:n], True, True)]), reads=[qnk, "g"], writes=[("ps", a1)])
                t1, t1k = t1_r.next()
                ph.add(DVE, e_tt(t1[:, 0:n], qn[:, 0:n], cos_t[:, 0:n], ALU.mult), reads=[qnk, coskey], writes=[t1k])
                t2, t2k = t2_r.next()
                ph.add(DVE, e_tt(t2[:, 0:n], ps[a1][:, 0:n], sin_t[:, 0:n], ALU.mult), reads=[("ps", a1), sinkey], writes=[t2k])
                sg, sgk = stg_r.next()
                ph.add(DVE, e_tt(sg[:, 0:n], t1[:, 0:n], t2[:, 0:n], ALU.add), reads=[t1k, t2k], writes=[sgk])
                store(dst, sg[:, 0:n], sgk)

            def rope64(b, cos_t, coskey, sin_t, sinkey, dst):
                a0, a1 = auxpair()
                qn, qnk = qn_r.next()
                ph.add(DVE, e_copy(qn[0:64, 0:n], ps[b][0:64, 0:n]), reads=[("ps", b)], writes=[qnk])
                ph.add(PE, e_mm([(ps[a1][0:64, 0:n], C.rt64b[:], qn[0:64, 0:n], True, True)]), reads=[qnk, "g"], writes=[("ps", a1)])
                t1, t1k = t1_r.next()
                ph.add(DVE, e_tt(t1[0:64, 0:n], qn[0:64, 0:n], cos_t[:, 0:n], ALU.mult), reads=[qnk, coskey], writes=[t1k])
                t2, t2k = t2_r.next()
                ph.add(DVE, e_tt(t2[0:64, 0:n], ps[a1][0:64, 0:n], sin_t[:, 0:n], ALU.mult), reads=[("ps", a1), sinkey], writes=[t2k])
                sg, sgk = stg_r.next()
                ph.add(DVE, e_tt(sg[0:64, 0:n], t1[0:64, 0:n], t2[0:64, 0:n], ALU.add), reads=[t1k, t2k], writes=[sgk])
                store(dst, sg[0:64, 0:n], sgk)

            def copy_out(b, dst, scale=1.0, func=AF.Copy, m=128):
                sg, sgk = stg_r.next()
                ph.add(ACT, e_act(sg[0:m, 0:n], ps[b][0:m, 0:n], func, scale=scale), reads=[("ps", b)], writes=[sgk])
                store(dst, sg[0:m, 0:n], sgk)

            def latent(seg, nch, lat, latkey):
                c0, wd = SEG[seg]
                w, wk = load_w(c0, wd)
                banks = [gemm_chunk(w, wk, c * 128) for c in range(nch)]
                a0, a1 = auxpair()
                sqs = []
                for c in range(nch):
                    sq, sqk = sq_r.next()
                    ph.add(ACT, e_act(sq[:, 0:n], ps[banks[c]][:, 0:n], AF.Square), reads=[("ps", banks[c])], writes=[sqk])
                    ph.add(PE, e_mm([(ps[a0][:, 0:n], C.ones_bf[:], sq[:, 0:n], c == 0, c == nch - 1)]),
                           reads=[sqk, "g"], writes=[("ps", a0)], acc=True)
                ln, lnk = ln_r.next()
                ph.add(ACT, e_act(ln[:, 0:n], ps[a0][:, 0:n], AF.Ln, bias=128.0 * nch * EPS), reads=[("ps", a0)], writes=[lnk])
                rs, rsk = rs_r.next()
                ph.add(ACT, e_act(rs[:, 0:n], ln[:, 0:n], AF.Exp, scale=-0.5), reads=[lnk], writes=[rsk])
                for c in range(nch):
                    ph.add(DVE, e_tt(lat[:, c, 0:n], ps[banks[c]][:, 0:n], rs[:, 0:n], ALU.mult),
                           reads=[("ps", banks[c]), rsk], writes=[(latkey, c)])

            def tokmajor(seg, dstv, key0):
                c0, wd = SEG[seg]
                w, wk = load_w(c0, wd)
                for s0 in range(0, wd, 512):
                    ncols = min(512, wd - s0)
                    for tt in range(n // 128):
                        b = mainbank()
                        ph.add(PE, e_mm([(ps[b][:, 0:ncols], u[:, k, tt * 128:(tt + 1) * 128], w[:, k, s0:s0 + ncols], k == 0, k == KC - 1)
                                         for k in range(KC)]), reads=[wk] + ukeys, writes=[("ps", b)])
                        v, vk = vst_r.next()
                        ph.add(DVE if tt % 2 == 0 else ACT,
                               e_copy(v[:, 0:ncols], ps[b][:, 0:ncols]) if tt % 2 == 0 else e_act(v[:, 0:ncols], ps[b][:, 0:ncols], AF.Copy),
                               reads=[("ps", b)], writes=[vk])
                        store(dstv[key0 + tt * 128:key0 + (tt + 1) * 128, s0:s0 + ncols], v[:, 0:ncols], vk)

            if qcol is not None:
                c0, wd = SEG["aq"]
                for s0 in range(0, wd, SW):
                    ncols = min(SW, wd - s0)
                    w, wk = load_w(c0 + s0, ncols)
                    for cc in range(0, ncols, 128):
                        h = (s0 + cc) // 128
                        b = gemm_chunk(w, wk, cc)
                        rope128(b, C.rtq, "g", cosq, "cosq", tb128[:, 1, :], "tb128", C.QA[h, :, qcol:qcol + n])
                latent("bq", 4, latq, "latq")
                for h in range(5):
                    b = mainbank()
                    ph.add(PE, e_mm([(ps[b][:, 0:n], C.wuq[:, c, h * 192:h * 192 + 128], latq[:, c, 0:n], c == 0, c == 3) for c in range(4)]),
                           reads=[("latq", c) for c in range(4)] + ["g"], writes=[("ps", b)])
                    copy_out(b, C.QBN[h, :, qcol:qcol + n], scale=SCALE_B)
                    b = mainbank()
                    ph.add(PE, e_mm([(ps[b][0:64, 0:n], C.wuq[:, c, h * 192 + 128:h * 192 + 192], latq[:, c, 0:n], c == 0, c == 3) for c in range(4)]),
                           reads=[("latq", c) for c in range(4)] + ["g"], writes=[("ps", b)])
                    rope64(b, c64q, "c64q", s64q, "s64q", C.QBR[h, :, qcol:qcol + n])
                c0, wd = SEG["cq"]
                for s0 in range(0, wd, SW):
                    ncols = min(SW, wd - s0)
                    w, wk = load_w(c0 + s0, ncols)
                    for cc in range(0, ncols, 128):
                        h = (s0 + cc) // 128
                        b = gemm_chunk(w, wk, cc)
                        copy_out(b, C.QC[h, :, qcol:qcol + n], scale=SCALE_C)
                c0, wd = SEG["gate"]
                for s0 in range(0, wd, SW):
                    w, wk = load_w(c0 + s0, SW)
                    for cc in range(0, SW, 128):
                        j = (s0 + cc) // 128
                        b = gemm_chunk(w, wk, cc)
                        copy_out(b, C.G[j, :, qcol:qcol + n], func=AF.Sigmoid)
            if kd is not None:
                c0, wd = SEG["ak"]
                w, wk = load_w(c0, wd)
                for h in range(2):
                    b = gemm_chunk(w, wk, h * 128)
                    rope128(b, C.rtk, "g", cosk, "cosk", sink, "sink", C.KA[h, :, kd:kd + n])
                tokmajor("av", C.VA, kd)
                latent("bkv", 2, latkv, "latkv")
                for h in range(5):
                    b = mainbank()
                    ph.add(PE, e_mm([(ps[b][:, 0:n], C.wukv[:, c, h * 256:h * 256 + 128], latkv[:, c, 0:n], c == 0, c == 1) for c in range(2)]),
                           reads=[("latkv", c) for c in range(2)] + ["g"], writes=[("ps", b)])
                    copy_out(b, C.KBN[h, :, kd:kd + n])
                for tt in range(n // 128):
                    b0 = mainbank()
                    ph.add(PE, e_mm([(ps[b0][:, 0:512], latkv[:, c, tt * 128:(tt + 1) * 128], C.wukv_v[:, c, 0:512], c == 0, c == 1) for c in range(2)]),
                           reads=[("latkv", c) for c in range(2)] + ["g"], writes=[("ps", b0)])
                    b1 = mainbank()
                    ph.add(PE, e_mm([(ps[b1][:, 0:128], latkv[:, c, tt * 128:(tt + 1) * 128], C.wukv_v[:, c, 512:640], c == 0, c == 1) for c in range(2)]),
                           reads=[("latkv", c) for c in range(2)] + ["g"], writes=[("ps", b1)])
                    v, vk = vst_r.next()
                    ph.add(DVE, e_copy(v[:, 0:512], ps[b0][:, 0:512]), reads=[("ps", b0)], writes=[vk])
                    ph.add(ACT, e_act(v[:, 512:640], ps[b1][:, 0:128], AF.Copy), reads=[("ps", b1)], writes=[vk])
                    store(C.VB[kd + tt * 128:kd + (tt + 1) * 128, :], v[:, :], vk)
                c0, wd = SEG["bkr"]
                w, wk = load_w(c0, wd)
                b = gemm_chunk(w, wk, 0, m=64)
                rope64(b, tb64[:, 0, :], "tb64", tb64[:, 1, :], "tb64", C.KBR[:, kd:kd + n])
            if kn is not None:
                c0, wd = SEG["ck"]
                for s0 in range(0, wd, SW):
                    ncols = min(SW, wd - s0)
                    w, wk = load_w(c0 + s0, ncols)
                    for cc in range(0, ncols, 128):
                        h = (s0 + cc) // 128
                        b = gemm_chunk(w, wk, cc)
                        copy_out(b, C.KCs[h, :, kn:kn + n])
                tokmajor("cv", C.VC, kn)
        nops = ph.finalize()
    return nops


def _rope_table(pos, dim):
    pos = np.asarray(pos, dtype=np.int64)
    row = (pos // 64).astype(np.float32)
    col = (pos % 64).astype(np.float32)
    quarter = dim // 4
    inv_freq = (np.float32(10000.0) ** (-np.arange(quarter, dtype=np.float32) / np.float32(quarter))).astype(np.float32)
    ang_r = row[:, None] * inv_freq
    ang_c = col[:, None] * inv_freq
    ang = np.concatenate([ang_r, ang_r, ang_c, ang_c], axis=-1).astype(np.float32)
    return np.cos(ang).T.astype(np.float32), np.sin(ang).T.astype(np.float32)


def _rot_T(dim):
    q = dim // 4
    RT = np.zeros((dim, dim), np.float32)
    for m in range(dim):
        b = (m % (2 * q)) // q
        if b == 0:
            RT[m + q, m] = -1.0
        else:
            RT[m - q, m] = 1.0
    return RT


def _local_pos(half):
    own = np.arange(half * NOWN, (half + 1) * NOWN)
    oth = np.arange((1 - half) * NOWN, (2 - half) * NOWN)
    return np.concatenate([own, oth])


def _tables(half):
    pos = _local_pos(half)
    t128 = np.zeros((2, 128, TABW), np.float32)
    t64 = np.zeros((2, 64, TABW), np.float32)
    for dim, t in ((128, t128), (64, t64)):
        c, s = _rope_table(pos, dim)
        t[0, :, 0:NNAT] = c
        t[1, :, 0:NNAT] = s
        t[0, :, NNAT:] = 1.0
    return t128, t64


def _na_bias_index(half):
    ri = np.zeros((8, 8, 128, 512), np.int64)
    ci = np.zeros((8, 8, 128, 512), np.int64)
    valid = np.zeros((8, 8, 128, 512), bool)
    for jl in range(8):
        s_, j = jl // 4, jl % 4
        qhalf = half if s_ == 0 else 1 - half
        q_rows = qhalf * 32 + 8 * j + np.arange(8)
        qr = np.repeat(q_rows, 64)
        qc = np.tile(np.arange(64), 8)
        r_start = np.clip(qr - 4, 0, 64 - 8)
        c_start = np.clip(qc - 8, 0, 64 - 16)
        for i, t in enumerate(na_tiles(jl)):
            thalf = half if t < 16 else 1 - half
            rows = thalf * 32 + 2 * (t % 16) + np.arange(2)
            kr = np.repeat(rows, 64)
            kc = np.tile(np.arange(64), 2)
            v = ((kr[:, None] >= r_start[None, :]) & (kr[:, None] < r_start[None, :] + 8) &
                 (kc[:, None] >= c_start[None, :]) & (kc[:, None] < c_start[None, :] + 16))
            ri[jl, i] = np.clip(kr[:, None] - qr[None, :] + 7, 0, 14)
            ci[jl, i] = np.clip(kc[:, None] - qc[None, :] + 15, 0, 30)
            valid[jl, i] = v
    return ri, ci, valid


_CONST_CACHE = {}


def _consts(half):
    if half not in _CONST_CACHE:
        t128, t64 = _tables(half)
        _CONST_CACHE[half] = dict(tab128=t128, tab64=t64, nabidx=_na_bias_index(half))
    return _CONST_CACHE[half]


def _fm(v, nch):
    return np.ascontiguousarray(np.asarray(v, np.float32).reshape(nch, 128).T)


def prep_layer_shared(inp, l, with_moe=True):
    sh = {}
    sh["w_ada"] = np.ascontiguousarray(inp["w_ada"][l])
    sh["b_ada"] = _fm(inp["b_ada"][l], 96)
    sh["w_in"] = np.ascontiguousarray(inp["w_in"][l])
    sh["gq"] = np.ascontiguousarray(inp["gqa_q_norm"][l].reshape(128, 1))
    sh["gk"] = np.ascontiguousarray(inp["gqa_k_norm"][l].reshape(128, 1))
    sh["mlaq_g"] = _fm(inp["mla_q_norm"][l], 4)
    sh["mlakv_g"] = _fm(inp["mla_kv_norm"][l], 2)
    sh["w_uq"] = np.ascontiguousarray(inp["mla_w_uq"][l])
    sh["w_ukv"] = np.ascontiguousarray(inp["mla_w_ukv"][l])
    sh["w_ba"] = np.ascontiguousarray(inp["w_branch_a"][l])
    sh["w_bb"] = np.ascontiguousarray(inp["w_branch_b"][l])
    sh["w_bc"] = np.ascontiguousarray(inp["w_branch_c"][l])
    sh["w_out"] = np.ascontiguousarray(inp["w_out"][l])
    sh["lnp"] = np.ascontiguousarray(np.stack([_fm(inp[n][l], 16) for n in ("ln1_g", "ln1_b", "ln2_g", "ln2_b")], axis=1))
    sh["w_r"] = np.ascontiguousarray(np.concatenate([inp["w_router_group"][l], inp["w_router_expert"][l]], axis=1))
    br = np.concatenate([inp["b_router_group"][l], inp["b_router_expert"][l]])[None, :]
    sh["b_r"] = np.ascontiguousarray(np.broadcast_to(br, (128, 72)).astype(np.float32))
    if with_moe:
        sh["w_eg"] = np.ascontiguousarray(inp["w_expert_gate"][l])
        sh["w_eu"] = np.ascontiguousarray(inp["w_expert_up"][l])
        sh["w_ed"] = np.ascontiguousarray(inp["w_expert_down"][l])
    return {k + str(l): v for k, v in sh.items()}


def prep_core(inp, core, shared, nlayers=2):
    b, half = core // 2, core % 2
    cst = _consts(half)
    m = dict(shared)
    xb = np.asarray(inp["x"][b], np.float32)
    pos = _local_pos(half)
    m["xt"] = np.ascontiguousarray(np.concatenate([xb[pos].T, np.asarray(inp["ctx"][b], np.float32).T], axis=1))
    cond = np.stack([inp["c"][b], inp["c_ctx"]], axis=1)
    m["cond"] = np.ascontiguousarray(cond.reshape(KC, 128, 2).transpose(1, 0, 2).reshape(128, 32).astype(np.float32))
    m["tab128"] = cst["tab128"]
    m["tab64"] = cst["tab64"]
    m["rt128"] = _rot_T(128)
    m["rt64"] = _rot_T(64)
    m["ident"] = np.eye(128, dtype=np.float32)
    ri, ci, valid = cst["nabidx"]
    for l in range(nlayers):
        nq = 8 if l == 0 else 4
        rpb = np.asarray(inp["na_rpb"][l], np.float32)
        m["nab" + str(l)] = np.ascontiguousarray(
            np.where(valid[None, :nq], rpb[:, ri[:nq], ci[:nq]], np.float32(-30000.0)).astype(np.float32))
    return m


def proj_blocks(src, layer):
    blocks = []
    for i in range(8):
        q = (i * 512) if (layer == 0 or i < 4) else None
        blocks.append(dict(src=src[:, i * 512:(i + 1) * 512], ntok=512, modcol=0, tabcol=i * 512, qcol=q, kd=i * 512, kn=i * 512))
    blocks.append(dict(src=src[:, NNAT:TL], ntok=NCTX, modcol=1, tabcol=NNAT, qcol=(NNAT if layer == 0 else None), kd=NNAT, kn=NNAT))
    return blocks


def na_tiles(jl):
    s_, j = jl // 4, jl % 4
    base = 16 * s_
    obase = 16 * (1 - s_)
    tiles = []
    for i in range(8):
        lt = 4 * j - 2 + i
        if 0 <= lt < 16:
            tiles.append(base + lt)
        elif lt < 0:
            tiles.append(obase + 16 + lt)
        else:
            tiles.append(obase + lt - 16)
    return tiles


def q_blocks(layer):
    qb = [(i * 512, 512, i) for i in range(8 if layer == 0 else 4)]
    if layer == 0:
        qb.append((NNAT, NCTX, None))
    return qb


def phase_attn(C, W, kind, qblocks):
    nc = C.nc
    ph = Phase(nc, C.sems, "att" + kind)
    ps = C.ps
    nkeys = NKEY
    nkt = nkeys // 128
    with ExitStack() as st:
        sb = lambda name, shape, dt: st.enter_context(nc.sbuf_tensor(uname(name), shape, dt))
        kt_r = Ring(nc, st, "t_k", [128, nkeys], BF16, 2)
        v_r = Ring(nc, st, "t_v", [128, nkt, 128], BF16, 2)
        q_r = Ring(nc, st, "t_q", [128, 512], BF16, 2)
        p_r = Ring(nc, st, "t_p", [128, 512], BF16, 3)
        rd_r = Ring(nc, st, "t_rd", [128, 512], F32, 2)
        o_r = Ring(nc, st, "t_o", [128, 512], BF16, 2)
        if kind == "b":
            kr = sb("t_kr", [64, nkeys], BF16)
            qr_r = Ring(nc, st, "t_qr", [64, 512], BF16, 2)
            ph.add(SP, e_dma(kr[:], C.KBR), writes=["kr"], dma="kr")
        if kind == "c":
            b_r = Ring(nc, st, "t_b", [128, 512], BF16, 3)
        s_i = [0]
        blk_i = [0]
        nheads = {"a": 6, "b": 5, "c": 5}[kind]
        Ksrc = {"a": C.KA, "b": C.KBN, "c": C.KCs}[kind]
        Vsrc = {"a": C.VA, "b": C.VB, "c": C.VC}[kind]
        Qsrc = {"a": C.QA, "b": C.QBN, "c": C.QC}[kind]
        Odst = {"a": C.OA, "b": C.OB, "c": C.OC}[kind]
        Vv = Vsrc.rearrange("(t p) c -> p t c", p=128)
        kT = vv = None
        for h in range(nheads):
            kvh = h // 3 if kind == "a" else h
            if kind != "a" or h % 3 == 0:
                kT, kTk = kt_r.next()
                ph.add(SP, e_dma(kT[:], Ksrc[kvh]), writes=[kTk], dma=kTk)
                vv, vk = v_r.next()
                ph.add(SP, e_dma(vv[:], Vv[:, :, kvh * 128:(kvh + 1) * 128]), writes=[vk], dma=vk)
            for (qcol, n, j) in qblocks:
                q, qk = q_r.next()
                ph.add(SP, e_dma(q[:, 0:n], Qsrc[h, :, qcol:qcol + n]), writes=[qk], dma=qk)
                rkeys = [qk, kTk]
                if kind == "b":
                    qr, qrk = qr_r.next()
                    ph.add(SP, e_dma(qr[:, 0:n], C.QBR[h, :, qcol:qcol + n]), writes=[qrk], dma=qrk)
                    rkeys += [qrk, "kr"]
                if j is None:
                    tiles = [(nkt - 2, None), (nkt - 1, None)]
                elif kind == "c":
                    tiles = [(loc, i) for i, loc in enumerate(na_tiles(j))]
                    tiles += [(nkt - 2, None), (nkt - 1, None)]
                else:
                    tiles = [(t, None) for t in range(nkt)]
                bo = 3 + (blk_i[0] % 2)
                bd = 5 + (blk_i[0] % 2)
                blk_i[0] += 1
                nt = len(tiles)
                pend = {}

                def emit_s(idx):
                    t, bi = tiles[idx]
                    sbk = s_i[0] % 3
                    s_i[0] += 1
                    items = [(ps[sbk][:, 0:n], kT[:, t * 128:(t + 1) * 128], q[:, 0:n], True, (kind == "a") or (kind == "c" and bi is None))]
                    rk = list(rkeys)
                    if kind == "b":
                        items.append((ps[sbk][:, 0:n], kr[:, t * 128:(t + 1) * 128], qr[:, 0:n], False, True))
                    if kind == "c" and bi is not None:
                        bt, btk = b_r.next()
                        ph.add(POOL, e_dma(bt[:, :], W.nab[h, j, bi]), writes=[btk], dma=btk)
                        items.append((ps[sbk][:, 0:n], C.ident_bf[:], bt[:, 0:n], False, True))
                        rk.append(btk)
                    ph.add(PE, e_mm(items), reads=rk, writes=[("ps", sbk)])
                    pend[idx] = sbk

                emit_s(0)
                for idx in range(nt):
                    if idx + 1 < nt:
                        emit_s(idx + 1)
                    sbk = pend.pop(idx)
                    p, pk = p_r.next()
                    ph.add(ACT, e_act(p[:, 0:n], ps[sbk][:, 0:n], AF.Exp), reads=[("ps", sbk)], writes=[pk])
                    t, _ = tiles[idx]
                    ph.add(PE, e_mm([(ps[bo][:, 0:n], vv[:, t, :], p[:, 0:n], idx == 0, idx == nt - 1),
                                     (ps[bd][:, 0:n], C.ones_bf[:], p[:, 0:n], idx == 0, idx == nt - 1)]),
                           reads=[pk, vk, "g"], writes=[("ps", bo), ("ps", bd)], acc=True)
                rd, rdk = rd_r.next()
                ph.add(DVE, lambda e, rd=rd, bd=bd, n=n: e.reciprocal(out=rd[:, 0:n], in_=ps[bd][:, 0:n]), reads=[("ps", bd)], writes=[rdk])
                o, ok = o_r.next()
                ph.add(DVE, e_tt(o[:, 0:n], ps[bo][:, 0:n], rd[:, 0:n], ALU.mult), reads=[("ps", bo), rdk], writes=[ok])
                ph.add(SP, e_dma(Odst[h, :, qcol:qcol + n], o[:, 0:n]), reads=[ok], dma=ok)
        nops = ph.finalize()
    return nops


def ln_block(ph, C, rings, v, vkey, n, col, gi, dst, dstkey_prefix, off=0):
    ps = C.ps
    sq_r, st_r, o_r = rings["sq"], rings["stat"], rings["out"]
    bs, bq = 6, 7
    for m in range(KC):
        sq, sqk = sq_r.next()
        ph.add(ACT, e_act(sq[:, 0:n], v[:, m, off:off + n], AF.Square), reads=[(vkey, m)], writes=[sqk])
        ph.add(PE, e_mm([(ps[bs][:, 0:n], C.ones_f[:], v[:, m, off:off + n], m == 0, m == KC - 1)]),
               reads=[(vkey, m), "g"], writes=[("ps", bs)], acc=True)
        ph.add(PE, e_mm([(ps[bq][:, 0:n], C.ones_f[:], sq[:, 0:n], m == 0, m == KC - 1)]),
               reads=[sqk, "g"], writes=[("ps", bq)], acc=True)
    mean, meank = st_r.next()
    ph.add(DVE, e_ts(mean[:, 0:n], ps[bs][:, 0:n], 1.0 / D, ALU.mult), reads=[("ps", bs)], writes=[meank])
    msq, msqk = st_r.next()
    ph.add(DVE, e_tt(msq[:, 0:n], mean[:, 0:n], mean[:, 0:n], ALU.mult), reads=[meank], writes=[msqk])
    var, vark = st_r.next()
    ph.add(DVE, lambda e, var=var, msq=msq: e.scalar_tensor_tensor(out=var[:, 0:n], in0=ps[bq][:, 0:n], scalar=1.0 / D, in1=msq[:, 0:n],
                                                                     op0=ALU.mult, op1=ALU.subtract),
           reads=[("ps", bq), msqk], writes=[vark])
    lnv, lnk = st_r.next()
    ph.add(ACT, e_act(lnv[:, 0:n], var[:, 0:n], AF.Ln, bias=EPS), reads=[vark], writes=[lnk])
    rstd, rstdk = st_r.next()
    ph.add(ACT, e_act(rstd[:, 0:n], lnv[:, 0:n], AF.Exp, scale=-0.5), reads=[lnk], writes=[rstdk])
    for m in range(KC):
        t, tk = sq_r.next()
        ph.add(DVE, e_tt(t[:, 0:n], v[:, m, off:off + n], mean[:, 0:n], ALU.subtract), reads=[(vkey, m), meank], writes=[tk])
        t2, t2k = sq_r.next()
        ph.add(DVE, e_tt(t2[:, 0:n], t[:, 0:n], rstd[:, 0:n], ALU.mult), reads=[tk, rstdk], writes=[t2k])
        o, ok = o_r.next()
        ph.add(ACT, e_act(o[:, 0:n], t2[:, 0:n], AF.Identity, scale=C.lnps[:, gi, m:m + 1], bias=C.lnps[:, gi + 1, m:m + 1]),
               reads=[t2k, "g"], writes=[ok])
        ph.add(SP, e_dma(dst[m * 128:(m + 1) * 128, :], o[:, 0:n]), reads=[ok], dma=ok)


def phase_merge(C, W, xsrc, layer):
    nc = C.nc
    ph = Phase(nc, C.sems, "merge")
    ps = C.ps
    NB = 256
    with ExitStack() as st:
        sb = lambda name, shape, dt: st.enter_context(nc.sbuf_tensor(uname(name), shape, dt))
        wba = sb("m_wba", [128, 6, D], BF16)
        wbb = sb("m_wbb", [128, 5, D], BF16)
        wbc = sb("m_wbc", [128, 5, D], BF16)
        ph.add(POOL, e_dma(wba[:], W.w_ba.rearrange("(k p) n -> p k n", p=128)), writes=["wba"], dma="wba")
        ph.add(POOL, e_dma(wbb[:], W.w_bb.rearrange("(k p) n -> p k n", p=128)), writes=["wbb"], dma="wbb")
        ph.add(POOL, e_dma(wbc[:], W.w_bc.rearrange("(k p) n -> p k n", p=128)), writes=["wbc"], dma="wbc")
        wo_r = Ring(nc, st, "m_wo", [128, KC, 512], BF16, 2)
        o_in = Ring(nc, st, "m_oin", [128, 16, NB], BF16, 2)
        g_r = Ring(nc, st, "m_g", [128, 3, NB], BF16, 3)
        ta_r = Ring(nc, st, "m_ta", [128, NB], F32, 6)
        mix = sb("m_mix", [128, KC, NB], BF16)
        v = sb("m_v", [128, KC, NB], F32)
        x_r = Ring(nc, st, "m_x", [128, NB], F32, 3)
        xa_r = Ring(nc, st, "m_xa", [128, NB], F32, 3)
        rings = dict(sq=Ring(nc, st, "m_sq", [128, NB], F32, 4), stat=Ring(nc, st, "m_st", [128, NB], F32, 5),
                     out=Ring(nc, st, "m_out", [128, NB], F32, 3))
        wov = W.w_out.rearrange("(kc p) n -> p kc n", p=128)
        Gv = C.G.rearrange("(b m) p t -> m p b t", b=3)
        blocks = [(i * NB, NB, 0) for i in range((NNAT if layer == 0 else NOWN) // NB)]
        if layer == 0:
            blocks.append((NNAT, NCTX, 1))
        pi = [0]
        for (qcol, n, col) in blocks:
            oin, oink = o_in.next()
            ph.add(SP, e_dma(oin[:, 0:6, 0:n], C.OA[:, :, qcol:qcol + n].rearrange("h p t -> p h t")), writes=[(oink, 0)], dma=(oink, 0))
            ph.add(SP, e_dma(oin[:, 6:11, 0:n], C.OB[:, :, qcol:qcol + n].rearrange("h p t -> p h t")), writes=[(oink, 1)], dma=(oink, 1))
            ph.add(SP, e_dma(oin[:, 11:16, 0:n], C.OC[:, :, qcol:qcol + n].rearrange("h p t -> p h t")), writes=[(oink, 2)], dma=(oink, 2))
            for m in range(KC):
                g, gk = g_r.next()
                ph.add(SP, e_dma(g[:, :, 0:n], Gv[m, :, :, qcol:qcol + n]), writes=[gk], dma=gk)
                b3 = [(pi[0] * 3 + i) % 6 for i in range(3)]
                pi[0] += 1
                ms = slice(m * 128, (m + 1) * 128)
                ph.add(PE, e_mm([(ps[b3[0]][:, 0:n], wba[:, k, ms], oin[:, k, 0:n], k == 0, k == 5) for k in range(6)]),
                       reads=["wba", (oink, 0)], writes=[("ps", b3[0])])
                ph.add(PE, e_mm([(ps[b3[1]][:, 0:n], wbb[:, k, ms], oin[:, 6 + k, 0:n], k == 0, k == 4) for k in range(5)]),
                       reads=["wbb", (oink, 1)], writes=[("ps", b3[1])])
                ph.add(PE, e_mm([(ps[b3[2]][:, 0:n], wbc[:, k, ms], oin[:, 11 + k, 0:n], k == 0, k == 4) for k in range(5)]),
                       reads=["wbc", (oink, 2)], writes=[("ps", b3[2])])
                ta, tak = ta_r.next()
                ph.add(DVE, e_tt(ta[:, 0:n], ps[b3[0]][:, 0:n], g[:, 0, 0:n], ALU.mult), reads=[("ps", b3[0]), gk], writes=[tak])
                tb, tbk = ta_r.next()
                ph.add(DVE, e_tt(tb[:, 0:n], ps[b3[1]][:, 0:n], g[:, 1, 0:n], ALU.mult), reads=[("ps", b3[1]), gk], writes=[tbk])
                tcc, tck = ta_r.next()
                ph.add(DVE, e_tt(tcc[:, 0:n], ps[b3[2]][:, 0:n], g[:, 2, 0:n], ALU.mult), reads=[("ps", b3[2]), gk], writes=[tck])
                ph.add(DVE, e_tt(ta[:, 0:n], ta[:, 0:n], tb[:, 0:n], ALU.add), reads=[tak, tbk], writes=[tak])
                ph.add(DVE, e_tt(mix[:, m, 0:n], ta[:, 0:n], tcc[:, 0:n], ALU.add), reads=[tak, tck], writes=[("mix", m)])
            mixkeys = [("mix", m) for m in range(KC)]
            for m in range(KC):
                if m % 4 == 0:
                    wo, wok = wo_r.next()
                    ph.add(POOL, e_dma(wo[:], wov[:, :, m * 128:m * 128 + 512]), writes=[wok], dma=wok)
                by = 6 + (m % 2)
                ph.add(PE, e_mm([(ps[by][:, 0:n], wo[:, k, (m % 4) * 128:(m % 4 + 1) * 128], mix[:, k, 0:n], k == 0, k == KC - 1) for k in range(KC)]),
                       reads=[wok] + mixkeys, writes=[("ps", by)])
                x, xk = x_r.next()
                ph.add(SP, e_dma(x[:, 0:n], xsrc[m * 128:(m + 1) * 128, qcol:qcol + n]), writes=[xk], dma=xk)
                xa, xak = xa_r.next()
                ph.add(ACT, e_act(xa[:, 0:n], x[:, 0:n], AF.Identity, scale=ALPHA), reads=[xk], writes=[xak])
                ph.add(DVE, lambda e, m=m, by=by, xa=xa, n=n, col=col: e.scalar_tensor_tensor(
                    out=v[:, m, 0:n], in0=ps[by][:, 0:n], scalar=C.mod[:, col, 32 + m:33 + m], in1=xa[:, 0:n], op0=ALU.mult, op1=ALU.add),
                    reads=[("ps", by), xak, "g"], writes=[("v", m)])
            ln_block(ph, C, rings, v, "v", n, col, 0, C.X1[:, qcol:qcol + n], "x1")
        nops = ph.finalize()
    return nops


def phase_moe(C, W, layer, dst, n_exp=NEXP):
    nc = C.nc
    ph = Phase(nc, C.sems, "moe")
    ps = C.ps
    NB = 512
    BIG = 1.0e30
    with ExitStack() as st:
        sb = lambda name, shape, dt: st.enter_context(nc.sbuf_tensor(uname(name), shape, dt))
        wr = sb("e_wr", [128, KC, 72], F32)
        br = sb("e_br", [128, 72], F32)
        ph.add(SP, e_dma(wr[:], W.w_r.rearrange("(kc p) n -> p kc n", p=128)), writes=["wr"], dma="wr")
        ph.add(SP, e_dma(br[:], W.b_r), writes=["br"], dma="br")
        nops = ph.finalize()
        xs_r = Ring(nc, st, "e_xs", [128, 4, NB], F32, 1)
        u32_r = Ring(nc, st, "e_u32", [128, 4, NB], F32, 2)
        u16 = sb("e_u16", [128, KC, NB], BF16)
        w_r = Ring(nc, st, "e_w", [128, 8192], BF16, 4)
        mx = sb("e_mx", [128, KC, NB], F32)
        hh_r = Ring(nc, st, "e_hh", [128, NB], F32, 2)
        sg_r = Ring(nc, st, "e_sg", [128, NB], F32, 2)
        H_r = Ring(nc, st, "e_H", [128, 4, NB], BF16, 2)
        wgtT = sb("e_wgtT", [64, NB], F32)
        wm_r = Ring(nc, st, "e_wm", [64, NB], F32, 2)
        lg = sb("e_lg", [128, 72], F32)
        r8 = sb("e_r8", [128, 8], F32)
        goh = sb("e_goh", [128, 8], F32)
        pen = sb("e_pen", [128, 8], F32)
        ex8 = sb("e_ex8", [128, 8], F32)
        lm = sb("e_lm", [128, 64], F32)
        lm2 = sb("e_lm2", [128, 64], F32)
        oh1 = sb("e_oh1", [128, 64], F32)
        oh2 = sb("e_oh2", [128, 64], F32)
        sc = sb("e_sc", [128, 16], F32)
        wgt = sb("e_wgt", [128, 64], F32)
        rings = dict(sq=Ring(nc, st, "e_sq", [128, 256], F32, 4), stat=Ring(nc, st, "e_st", [128, 256], F32, 5),
                     out=Ring(nc, st, "e_out", [128, 256], F32, 3))
        x_r = Ring(nc, st, "e_x", [128, NB], F32, 2)
        xa_r = Ring(nc, st, "e_xa", [128, NB], F32, 2)
        blocks = [(i * NB, NB, 0) for i in range((NNAT if layer == 0 else NOWN) // NB)]
        if layer == 0:
            blocks.append((NNAT, NCTX, 1))
        gi = [0]
        GRP = 3
        for bi_, (qcol, n, col) in enumerate(blocks):
            if bi_ % GRP == 0:
                ph = Phase(nc, C.sems, "moe_b")
            ntt = n // 128
            srcv = C.X1[:, qcol:qcol + n].rearrange("(kc p) t -> p kc t", p=128)
            for q4 in range(4):
                xs, xk = xs_r.next()
                ph.add(SP, e_dma(xs[:, :, 0:n], srcv[:, q4 * 4:(q4 + 1) * 4, :]), writes=[xk], dma=xk)
                u32, u32k = u32_r.next()
                for kk in range(4):
                    k = q4 * 4 + kk
                    ph.add(ACT, e_act(u32[:, kk, 0:n], xs[:, kk, 0:n], AF.Identity, scale=C.sc1[:, col, 1, k:k + 1],
                                      bias=C.mod[:, col, 48 + k:49 + k]), reads=[xk, "g"], writes=[(u32k, kk)])
                    ph.add(POOL, e_copy(u16[:, k, 0:n], u32[:, kk, 0:n]), reads=[(u32k, kk)], writes=[("u16", k)])
                    for tt in range(ntt):
                        ph.add(PE, e_mm([(ps[tt][:, 0:72], u32[:, kk, tt * 128:(tt + 1) * 128], wr[:, k, :], k == 0, k == KC - 1)]),
                               reads=[(u32k, kk), "wr"], writes=[("ps", tt)], acc=True)
            for tt in range(ntt):
                A = lambda o, a, b, op: ph.add(DVE, e_tt(o, a, b, op), reads=["rt"], writes=["rt"])
                S = lambda o, a, s1, op0, s2=None, op1=None: ph.add(DVE, e_ts(o, a, s1, op0, s2, op1), reads=["rt"], writes=["rt"])
                ph.add(DVE, e_tt(lg[:], ps[tt][:, 0:72], br[:], ALU.add), reads=[("ps", tt), "br", "rt"], writes=["rt"])
                ph.add(DVE, lambda e: e.tensor_reduce(out=sc[:, 0:1], in_=lg[:, 0:8], axis=AX.X, op=ALU.max), reads=["rt"], writes=["rt"])
                S(goh[:], lg[:, 0:8], sc[:, 0:1], ALU.is_equal)
                S(sc[:, 1:2], sc[:, 0:1], -1.0, ALU.mult)
                ph.add(ACT, e_act(ex8[:], lg[:, 0:8], AF.Exp, bias=sc[:, 1:2]), reads=["rt"], writes=["rt"])
                ph.add(DVE, lambda e: e.tensor_reduce(out=sc[:, 2:3], in_=ex8[:], axis=AX.X, op=ALU.add), reads=["rt"], writes=["rt"])
                ph.add(DVE, lambda e: e.reciprocal(out=sc[:, 3:4], in_=sc[:, 2:3]), reads=["rt"], writes=["rt"])
                S(pen[:], goh[:], -1.0, ALU.add, BIG, ALU.mult)
                for g in range(8):
                    S(lm[:, g * 8:(g + 1) * 8], lg[:, 8 + g * 8:16 + g * 8], pen[:, g:g + 1], ALU.add)
                ph.add(DVE, lambda e: e.tensor_reduce(out=sc[:, 4:5], in_=lm[:], axis=AX.X, op=ALU.max), reads=["rt"], writes=["rt"])
                S(oh1[:], lm[:], sc[:, 4:5], ALU.is_equal)
                S(lm2[:], oh1[:], -BIG, ALU.mult)
                A(lm2[:], lm2[:], lm[:], ALU.add)
                ph.add(DVE, lambda e: e.tensor_reduce(out=sc[:, 5:6], in_=lm2[:], axis=AX.X, op=ALU.max), reads=["rt"], writes=["rt"])
                S(oh2[:], lm2[:], sc[:, 5:6], ALU.is_equal)
                A(sc[:, 6:7], sc[:, 5:6], sc[:, 4:5], ALU.subtract)
                ph.add(ACT, e_act(sc[:, 7:8], sc[:, 6:7], AF.Exp), reads=["rt"], writes=["rt"])
                S(sc[:, 8:9], sc[:, 7:8], 1.0, ALU.add)
                ph.add(DVE, lambda e: e.reciprocal(out=sc[:, 9:10], in_=sc[:, 8:9]), reads=["rt"], writes=["rt"])
                A(sc[:, 10:11], sc[:, 7:8], sc[:, 9:10], ALU.mult)
                A(sc[:, 11:12], sc[:, 9:10], sc[:, 3:4], ALU.mult)
                A(sc[:, 12:13], sc[:, 10:11], sc[:, 3:4], ALU.mult)
                S(wgt[:], oh1[:], sc[:, 11:12], ALU.mult)
                S(oh2[:], oh2[:], sc[:, 12:13], ALU.mult)
                A(wgt[:], wgt[:], oh2[:], ALU.add)
                ph.add(PE, lambda e: e.transpose(out=ps[4][0:64, 0:128], in_=wgt[:], identity=C.ident_f[:]),
                       reads=["rt", "g"], writes=[("ps", 4)])
                ph.add(DVE, e_copy(wgtT[:, tt * 128:(tt + 1) * 128], ps[4][0:64, 0:128]), reads=[("ps", 4), "rt"], writes=["wgtT", "rt"])
            u16keys = [("u16", k) for k in range(KC)]
            for e_i in range(n_exp):
                wg, wgk = w_r.next()
                ph.add(POOL, e_dma(wg[:].rearrange("p (k n) -> p k n", k=KC), W.w_eg[e_i].rearrange("(kc p) n -> p kc n", p=128)), writes=[wgk], dma=wgk)
                wu, wuk = w_r.next()
                ph.add(POOL, e_dma(wu[:].rearrange("p (k n) -> p k n", k=KC), W.w_eu[e_i].rearrange("(kc p) n -> p kc n", p=128)), writes=[wuk], dma=wuk)
                wd, wdk = w_r.next()
                ph.add(POOL, e_dma(wd[:].rearrange("p (k n) -> p k n", k=4), W.w_ed[e_i].rearrange("(kc p) n -> p kc n", p=128)), writes=[wdk], dma=wdk)
                wgv = wg[:].rearrange("p (k n) -> p k n", k=KC)
                wuv = wu[:].rearrange("p (k n) -> p k n", k=KC)
                wdv = wd[:].rearrange("p (k n) -> p k n", k=4)
                def emit_wm(ei):
                    wm, wmk = wm_r.next()
                    ph.add(DVE, e_ts(wm[:, 0:n], wgtT[:, 0:n], C.ident_f[0:64, ei:ei + 1], ALU.mult), reads=["wgtT", "g"], writes=[wmk])
                    return wm, wmk

                def emit_bc(ei, wm, wmk):
                    bb_ = 4 + (ei % 2)
                    ph.add(PE, e_mm([(ps[bb_][:, 0:n], C.ones_f[0:64, :], wm[:, 0:n], True, True)]), reads=[wmk, "g"], writes=[("ps", bb_)])

                if e_i == 0:
                    wm_c = emit_wm(0)
                    emit_bc(0, *wm_c)
                bb = 4 + (e_i % 2)
                wm_n = None
                H, Hk = H_r.next()
                for c in range(4):
                    if c == 2 and e_i + 1 < n_exp:
                        wm_n = emit_wm(e_i + 1)
                    bg = (gi[0] % 2) * 2
                    gi[0] += 1
                    cs = slice(c * 128, (c + 1) * 128)
                    ph.add(PE, e_mm([(ps[bg][:, 0:n], wgv[:, k, cs], u16[:, k, 0:n], k == 0, k == KC - 1) for k in range(KC)]),
                           reads=[wgk] + u16keys, writes=[("ps", bg)])
                    ph.add(PE, e_mm([(ps[bg + 1][:, 0:n], wuv[:, k, cs], u16[:, k, 0:n], k == 0, k == KC - 1) for k in range(KC)]),
                           reads=[wuk] + u16keys, writes=[("ps", bg + 1)])
                    sg, sgk = sg_r.next()
                    ph.add(ACT, e_act(sg[:, 0:n], ps[bg][:, 0:n], AF.Silu), reads=[("ps", bg)], writes=[sgk])
                    hh, hhk = hh_r.next()
                    ph.add(DVE, e_tt(hh[:, 0:n], ps[bg + 1][:, 0:n], sg[:, 0:n], ALU.mult), reads=[("ps", bg + 1), sgk], writes=[hhk])
                    ph.add(DVE, e_tt(H[:, c, 0:n], ps[bb][:, 0:n], hh[:, 0:n], ALU.mult), reads=[("ps", bb), hhk], writes=[(Hk, c)])
                if wm_n is not None:
                    emit_bc(e_i + 1, *wm_n)
                Hkeys = [(Hk, c) for c in range(4)]
                for m in range(KC):
                    by = 6 + (m % 2)
                    ph.add(PE, e_mm([(ps[by][:, 0:n], wdv[:, c, m * 128:(m + 1) * 128], H[:, c, 0:n], c == 0, c == 3) for c in range(4)]),
                           reads=[wdk] + Hkeys, writes=[("ps", by)])
                    if e_i == 0:
                        ph.add(DVE, e_copy(mx[:, m, 0:n], ps[by][:, 0:n]), reads=[("ps", by)], writes=[("mx", m)])
                    else:
                        ph.add(DVE, e_tt(mx[:, m, 0:n], ps[by][:, 0:n], mx[:, m, 0:n], ALU.add), reads=[("ps", by), ("mx", m)], writes=[("mx", m)])
            for m in range(KC):
                x, xk = x_r.next()
                ph.add(SP, e_dma(x[:, 0:n], C.X1[m * 128:(m + 1) * 128, qcol:qcol + n]), writes=[xk], dma=xk)
                xa, xak = xa_r.next()
                ph.add(ACT, e_act(xa[:, 0:n], x[:, 0:n], AF.Identity, scale=ALPHA), reads=[xk], writes=[xak])
                ph.add(DVE, lambda e, m=m, xa=xa, n=n, col=col: e.scalar_tensor_tensor(
                    out=mx[:, m, 0:n], in0=mx[:, m, 0:n], scalar=C.mod[:, col, 80 + m:81 + m], in1=xa[:, 0:n], op0=ALU.mult, op1=ALU.add),
                    reads=[("mx", m), xak, "g"], writes=[("mx", m)])
            for off in range(0, n, 256):
                ln_block(ph, C, rings, mx, "mx", 256, col, 2, dst[:, qcol + off:qcol + off + 256], "xo", off=off)
            if bi_ % GRP == GRP - 1 or bi_ == len(blocks) - 1:
                nops += ph.finalize()
    return nops


def build_program(nlayers=2, debug=False, with_moe=True, stop_after=None, n_exp=NEXP):
    nc = bass.Bass("TRN2", target_bir_lowering=False)
    C = declare_io(nc, nlayers=nlayers, debug=debug, with_moe=with_moe)
    n = 0
    with ExitStack() as st:
        alloc_globals(C, st)
        for l in range(nlayers):
            W = C.L[l]
            src = C.xt if l == 0 else C.XO0
            lay = 0 if l < nlayers - 1 or nlayers == 1 else 1
            if nlayers == 1:
                lay = 0
            n += phase_ada(C, W)
            if stop_after == "ada":
                break
            n += phase_proj(C, W, proj_blocks(src, lay))
            if stop_after == "proj":
                break
            for k in "abc":
                n += phase_attn(C, W, k, q_blocks(lay))
            if stop_after == "att":
                break
            n += phase_merge(C, W, src, lay)
            if stop_after == "merge":
                break
            if with_moe:
                n += phase_moe(C, W, lay, C.XO0 if lay == 0 else C.xo, n_exp=n_exp)
    return nc, n


_PROG = {}


def kernel(**inputs):
    inp = {k: np.asarray(v) for k, v in inputs.items()}
    if "nc" not in _PROG:
        _PROG["nc"] = build_program(2)[0]
    nc = _PROG["nc"]
    shared = {}
    for l in range(2):
        shared.update(prep_layer_shared(inp, l))
    in_maps = [prep_core(inp, c, shared) for c in range(8)]
    res = run_bass_kernel_spmd(nc, in_maps, core_ids=list(range(8)))
    del in_maps
    x = np.asarray(inp["x"])
    out = np.empty(x.shape, np.float32)
    for c in range(8):
        xo = np.asarray(res.results[c]["xo"])
        b, half = c // 2, c % 2
        out[b, half * NOWN:(half + 1) * NOWN] = xo.T
    return out
```
